# Optimizing a Trainium2 kernel written in Bass

```python
import math
import jax
import jax.numpy as jnp
from jax import lax
import numpy as np

D_MODEL = 1024
BATCH = 4
SEQ = 4096
DEPTH = 1

MIX_WIDTH = D_MODEL
HEAD_DIM = 64
ATTN_WIDTH = MIX_WIDTH // 2
N_Q_HEADS = ATTN_WIDTH // HEAD_DIM
N_KV_HEADS = max(1, N_Q_HEADS // 4)
Q_PER_KV = N_Q_HEADS // N_KV_HEADS
KV_WIDTH = N_KV_HEADS * HEAD_DIM
WINDOW = 128
ATTN_BLOCK = 128
HALO = -(-WINDOW // ATTN_BLOCK)
ROPE_THETA = 10000.0
MASK_VALUE = -1e30

SSM_WIDTH = MIX_WIDTH - ATTN_WIDTH
SSM_CH_PER_GROUP = 16
SSM_GROUPS = SSM_WIDTH // SSM_CH_PER_GROUP
SSM_STATE = 64
DT_MIN = 0.001
DT_MAX = 0.1

IN_WIDTH = ATTN_WIDTH + 2 * KV_WIDTH + SSM_WIDTH

N_EXPERTS = 32
TOP_K = 4
D_FF = D_MODEL
SWIGLU_LIMIT = 7.0
SWIGLU_ALPHA = 1.702
MOE_BLOCK = 512

RMS_EPS = 1e-6

kernel_name = "hymba_swa_s5_moe_adaln_layer"


def rms_norm(x, g):
    xf = x.astype(jnp.float32)
    y = xf * lax.rsqrt(jnp.mean(xf * xf, axis=-1, keepdims=True) + RMS_EPS)
    return (y * g.astype(jnp.float32)).astype(x.dtype)


def rope(t, pos):
    half = HEAD_DIM // 2
    inv_freq = ROPE_THETA ** (-jnp.arange(half, dtype=jnp.float32) * 2.0 / HEAD_DIM)
    ang = pos.astype(jnp.float32)[:, None] * inv_freq[None, :]
    cos = jnp.cos(ang)[None, :, None, :]
    sin = jnp.sin(ang)[None, :, None, :]
    tf = t.astype(jnp.float32)
    t1, t2 = tf[..., :half], tf[..., half:]
    return jnp.concatenate([t1 * cos - t2 * sin, t2 * cos + t1 * sin], axis=-1).astype(t.dtype)


def windowed_gqa(q, k, v, sink):
    bsz, s, _ = q.shape
    nb = s // ATTN_BLOCK
    kb_len = (2 * HALO + 1) * ATTN_BLOCK
    pos = jnp.arange(s, dtype=jnp.int32)
    q = rope(q.reshape(bsz, s, N_Q_HEADS, HEAD_DIM), pos)
    k = rope(k.reshape(bsz, s, N_KV_HEADS, HEAD_DIM), pos)
    v = v.reshape(bsz, s, N_KV_HEADS, HEAD_DIM)
    pad = HALO * ATTN_BLOCK
    kp = jnp.pad(k, ((0, 0), (pad, pad), (0, 0), (0, 0))).reshape(bsz, nb + 2 * HALO, ATTN_BLOCK, N_KV_HEADS, HEAD_DIM)
    vp = jnp.pad(v, ((0, 0), (pad, pad), (0, 0), (0, 0))).reshape(bsz, nb + 2 * HALO, ATTN_BLOCK, N_KV_HEADS, HEAD_DIM)
    kb = jnp.concatenate([kp[:, o:o + nb] for o in range(2 * HALO + 1)], axis=2)
    vb = jnp.concatenate([vp[:, o:o + nb] for o in range(2 * HALO + 1)], axis=2)
    qb = q.reshape(bsz, nb, ATTN_BLOCK, N_KV_HEADS, Q_PER_KV, HEAD_DIM)
    scores = jnp.einsum('bnqhgd,bnkhd->bnhgqk', qb, kb).astype(jnp.float32) * (HEAD_DIM ** -0.5)
    blk = jnp.arange(nb, dtype=jnp.int32)[:, None]
    qpos = blk * ATTN_BLOCK + jnp.arange(ATTN_BLOCK, dtype=jnp.int32)[None, :]
    kpos = (blk - HALO) * ATTN_BLOCK + jnp.arange(kb_len, dtype=jnp.int32)[None, :]
    valid = ((jnp.abs(qpos[:, :, None] - kpos[:, None, :]) <= WINDOW)
             & (kpos >= 0)[:, None, :] & (kpos < s)[:, None, :])
    scores = jnp.where(valid[None, :, None, None], scores, MASK_VALUE)
    sink_l = sink.astype(jnp.float32).reshape(1, 1, N_KV_HEADS, Q_PER_KV, 1, 1)
    m = jnp.maximum(jnp.max(scores, axis=-1, keepdims=True), sink_l)
    p = jnp.exp(scores - m)
    denom = jnp.sum(p, axis=-1, keepdims=True) + jnp.exp(sink_l - m)
    probs = (p / denom).astype(v.dtype)
    out = jnp.einsum('bnhgqk,bnkhd->bnqhgd', probs, vb)
    return out.reshape(bsz, s, ATTN_WIDTH)


def _linear_recurrence_combine(e1, e2):
    a1r, a1i, b1r, b1i = e1
    a2r, a2i, b2r, b2i = e2
    ar = a1r * a2r - a1i * a2i
    ai = a1r * a2i + a1i * a2r
    br = a2r * b1r - a2i * b1i + b2r
    bi = a2r * b1i + a2i * b1r + b2i
    return (ar, ai, br, bi)


def _s5_direction(uf, a_re, a_im, log_dt, b_re, b_im, c_re, c_im, reverse):
    dt = jnp.exp(log_dt)[:, None]
    lr = a_re * dt
    li = a_im * dt
    mag = jnp.exp(lr)
    abar_r = mag * jnp.cos(li)
    abar_i = mag * jnp.sin(li)
    den = a_re * a_re + a_im * a_im
    nr = abar_r - 1.0
    ni = abar_i
    f_r = (nr * a_re + ni * a_im) / den
    f_i = (ni * a_re - nr * a_im) / den
    bb_r = f_r[..., None] * b_re - f_i[..., None] * b_im
    bb_i = f_r[..., None] * b_im + f_i[..., None] * b_re
    bu_r = jnp.einsum('bsgh,gph->bsgp', uf, bb_r)
    bu_i = jnp.einsum('bsgh,gph->bsgp', uf, bb_i)
    a_r = jnp.broadcast_to(abar_r, bu_r.shape)
    a_i = jnp.broadcast_to(abar_i, bu_i.shape)
    _, _, s_r, s_i = lax.associative_scan(_linear_recurrence_combine, (a_r, a_i, bu_r, bu_i), axis=1, reverse=reverse)
    return jnp.einsum('bsgp,ghp->bsgh', s_r, c_re) - jnp.einsum('bsgp,ghp->bsgh', s_i, c_im)


def s5_bidirectional(u, a_re, a_im, log_dt, b_re, b_im, c_re, c_im, d_skip):
    bsz, s, _ = u.shape
    uf = u.astype(jnp.float32).reshape(bsz, s, SSM_GROUPS, SSM_CH_PER_GROUP)
    y = d_skip.astype(jnp.float32).reshape(SSM_GROUPS, SSM_CH_PER_GROUP) * uf
    for direction in range(2):
        y = y + _s5_direction(uf,
                              a_re[direction].astype(jnp.float32), a_im[direction].astype(jnp.float32),
                              log_dt[direction].astype(jnp.float32),
                              b_re[direction].astype(jnp.float32), b_im[direction].astype(jnp.float32),
                              c_re[direction].astype(jnp.float32), c_im[direction].astype(jnp.float32),
                              direction == 1)
    return y.reshape(bsz, s, SSM_WIDTH)


def hybrid_mixer(h, w_in, attn_sink, ssm_a_re, ssm_a_im, ssm_log_dt, ssm_b_re, ssm_b_im,
                 ssm_c_re, ssm_c_im, ssm_d, w_glu, b_glu, g_attn_out, g_ssm_out, w_out):
    proj = h @ w_in
    q = proj[..., :ATTN_WIDTH]
    k = proj[..., ATTN_WIDTH:ATTN_WIDTH + KV_WIDTH]
    v = proj[..., ATTN_WIDTH + KV_WIDTH:ATTN_WIDTH + 2 * KV_WIDTH]
    u = proj[..., ATTN_WIDTH + 2 * KV_WIDTH:]
    y_attn = windowed_gqa(q, k, v, attn_sink)
    y_ssm = s5_bidirectional(u, ssm_a_re, ssm_a_im, ssm_log_dt, ssm_b_re, ssm_b_im, ssm_c_re, ssm_c_im, ssm_d)
    g = jax.nn.gelu(y_ssm).astype(h.dtype)
    y_ssm = g * jax.nn.sigmoid(g @ w_glu + b_glu)
    y = jnp.concatenate([rms_norm(y_attn, g_attn_out), rms_norm(y_ssm, g_ssm_out)], axis=-1)
    return y @ w_out


def moe_ffn(h, w_router, b_router, w_gate_up, b_gate_up, w_down, b_down):
    bsz, s, d = h.shape
    n_tok = bsz * s
    n_assign = n_tok * TOP_K
    hf = h.reshape(n_tok, d)
    logits = (hf @ w_router + b_router).astype(jnp.float32)
    top_logits, top_idx = lax.top_k(logits, TOP_K)
    gates = jax.nn.softmax(top_logits, axis=-1)
    expert_flat = top_idx.reshape(-1)
    gate_flat = gates.reshape(-1)
    token_flat = jnp.arange(n_assign, dtype=jnp.int32) // TOP_K
    order = jnp.argsort(expert_flat)
    sorted_expert = expert_flat[order]
    counts = jnp.bincount(expert_flat, length=N_EXPERTS).astype(jnp.int32)
    padded = (counts + MOE_BLOCK - 1) // MOE_BLOCK * MOE_BLOCK
    ends = jnp.cumsum(counts)
    starts = ends - counts
    pends = jnp.cumsum(padded)
    pstarts = pends - padded
    rank = jnp.arange(n_assign, dtype=jnp.int32) - starts[sorted_expert]
    dest = pstarts[sorted_expert] + rank
    n_rows = (-(-n_assign // MOE_BLOCK) + N_EXPERTS) * MOE_BLOCK
    n_blocks = n_rows // MOE_BLOCK
    row_token = jnp.zeros((n_rows,), jnp.int32).at[dest].set(token_flat[order])
    row_gate = jnp.zeros((n_rows,), jnp.float32).at[dest].set(gate_flat[order])
    block_start = jnp.arange(n_blocks, dtype=jnp.int32) * MOE_BLOCK
    block_expert = jnp.minimum(jnp.searchsorted(pends, block_start, side='right'), N_EXPERTS - 1)
    xs = hf[row_token].reshape(n_blocks, MOE_BLOCK, d)

    def expert_block(args):
        xb, e = args
        gu = xb @ w_gate_up[e] + b_gate_up[e]
        glu, lin = gu[:, :D_FF], gu[:, D_FF:]
        glu = jnp.minimum(glu, SWIGLU_LIMIT)
        lin = jnp.clip(lin, -SWIGLU_LIMIT, SWIGLU_LIMIT)
        act = glu * jax.nn.sigmoid(SWIGLU_ALPHA * glu) * (lin + 1.0)
        return act @ w_down[e] + b_down[e]

    ys = lax.map(expert_block, (xs, block_expert)).reshape(n_rows, d)
    out = jnp.zeros((n_tok, d), ys.dtype).at[row_token].add(ys * row_gate[:, None].astype(ys.dtype))
    return out.reshape(bsz, s, d)


def setup_inputs(seed: int = 0) -> dict:
    key = jax.random.key(seed)
    ks = jax.random.split(key, 32)
    L = DEPTH
    G, P, H = SSM_GROUPS, SSM_STATE, SSM_CH_PER_GROUP

    def nrm(k, shape, scale):
        return jax.random.normal(k, shape, jnp.float32) * scale

    x = nrm(ks[0], (BATCH, SEQ, D_MODEL), 1.0)
    c = nrm(ks[1], (BATCH, D_MODEL), 1.0)
    w_ada = nrm(ks[2], (L, D_MODEL, 6 * D_MODEL), 0.5 * D_MODEL ** -0.5)
    b_ada = nrm(ks[3], (L, 6 * D_MODEL), 0.02)
    g_pre_mix = 1.0 + nrm(ks[4], (L, D_MODEL), 0.05)
    w_in = nrm(ks[5], (L, D_MODEL, IN_WIDTH), D_MODEL ** -0.5)
    attn_sink = nrm(ks[6], (L, N_Q_HEADS), 1.0)
    ssm_a_re = -0.5 + nrm(ks[7], (L, 2, G, P), 0.01)
    ssm_a_im = math.pi * jnp.arange(P, dtype=jnp.float32) + nrm(ks[8], (L, 2, G, P), 0.01)
    ssm_log_dt = math.log(DT_MIN) + jax.random.uniform(ks[9], (L, 2, G), jnp.float32) * (math.log(DT_MAX) - math.log(DT_MIN))
    ssm_b_re = nrm(ks[10], (L, 2, G, P, H), (2 * H) ** -0.5)
    ssm_b_im = nrm(ks[11], (L, 2, G, P, H), (2 * H) ** -0.5)
    ssm_c_re = nrm(ks[12], (L, 2, G, H, P), P ** -0.5)
    ssm_c_im = nrm(ks[13], (L, 2, G, H, P), P ** -0.5)
    ssm_d = nrm(ks[14], (L, SSM_WIDTH), 0.5)
    w_glu = nrm(ks[15], (L, SSM_WIDTH, SSM_WIDTH), SSM_WIDTH ** -0.5)
    b_glu = nrm(ks[16], (L, SSM_WIDTH), 0.02)
    g_attn_out = 1.0 + nrm(ks[17], (L, ATTN_WIDTH), 0.05)
    g_ssm_out = 1.0 + nrm(ks[18], (L, SSM_WIDTH), 0.05)
    w_out = nrm(ks[19], (L, MIX_WIDTH, D_MODEL), MIX_WIDTH ** -0.5)
    g_post_mix = 1.0 + nrm(ks[20], (L, D_MODEL), 0.05)
    g_pre_ffn = 1.0 + nrm(ks[21], (L, D_MODEL), 0.05)
    w_router = nrm(ks[22], (L, D_MODEL, N_EXPERTS), D_MODEL ** -0.5)
    b_router = nrm(ks[23], (L, N_EXPERTS), 0.01)
    w_gate_up = nrm(ks[24], (L, N_EXPERTS, D_MODEL, 2 * D_FF), D_MODEL ** -0.5)
    b_gate_up = nrm(ks[25], (L, N_EXPERTS, 2 * D_FF), 0.02)
    w_down = nrm(ks[26], (L, N_EXPERTS, D_FF, D_MODEL), D_FF ** -0.5)
    b_down = nrm(ks[27], (L, N_EXPERTS, D_MODEL), 0.02)
    g_post_ffn = 1.0 + nrm(ks[28], (L, D_MODEL), 0.05)
    return {"x": x, "c": c, "w_ada": w_ada, "b_ada": b_ada, "g_pre_mix": g_pre_mix,
            "w_in": w_in, "attn_sink": attn_sink, "ssm_a_re": ssm_a_re, "ssm_a_im": ssm_a_im,
            "ssm_log_dt": ssm_log_dt, "ssm_b_re": ssm_b_re, "ssm_b_im": ssm_b_im,
            "ssm_c_re": ssm_c_re, "ssm_c_im": ssm_c_im, "ssm_d": ssm_d, "w_glu": w_glu,
            "b_glu": b_glu, "g_attn_out": g_attn_out, "g_ssm_out": g_ssm_out, "w_out": w_out,
            "g_post_mix": g_post_mix, "g_pre_ffn": g_pre_ffn, "w_router": w_router,
            "b_router": b_router, "w_gate_up": w_gate_up, "b_gate_up": b_gate_up,
            "w_down": w_down, "b_down": b_down, "g_post_ffn": g_post_ffn}


def reference(x, c, w_ada, b_ada, g_pre_mix, w_in, attn_sink, ssm_a_re, ssm_a_im, ssm_log_dt,
              ssm_b_re, ssm_b_im, ssm_c_re, ssm_c_im, ssm_d, w_glu, b_glu, g_attn_out, g_ssm_out,
              w_out, g_post_mix, g_pre_ffn, w_router, b_router, w_gate_up, b_gate_up, w_down,
              b_down, g_post_ffn):
    c_act = jax.nn.silu(c)
    for l in range(DEPTH):
        mods = c_act @ w_ada[l] + b_ada[l]
        sh1, sc1, gt1, sh2, sc2, gt2 = [m[:, None, :] for m in jnp.split(mods, 6, axis=-1)]
        h = rms_norm(x, g_pre_mix[l]) * (1.0 + sc1) + sh1
        y = hybrid_mixer(h, w_in[l], attn_sink[l], ssm_a_re[l], ssm_a_im[l], ssm_log_dt[l],
                         ssm_b_re[l], ssm_b_im[l], ssm_c_re[l], ssm_c_im[l], ssm_d[l],
                         w_glu[l], b_glu[l], g_attn_out[l], g_ssm_out[l], w_out[l])
        x = x + gt1 * rms_norm(y, g_post_mix[l])
        h = rms_norm(x, g_pre_ffn[l]) * (1.0 + sc2) + sh2
        y = moe_ffn(h, w_router[l], b_router[l], w_gate_up[l], b_gate_up[l], w_down[l], b_down[l])
        x = x + gt2 * rms_norm(y, g_post_ffn[l])
    return x
```

```python
import math
import os
import numpy as np
import concourse.bass as bass
import concourse.mybir as mybir
from concourse.bass_utils import run_bass_kernel_spmd
from contextlib import ExitStack

F32 = mybir.dt.float32
BF16 = mybir.dt.bfloat16
U8 = mybir.dt.uint8
I32 = mybir.dt.int32
ALU = mybir.AluOpType
AF = mybir.ActivationFunctionType
PI = math.pi

D = 1024
T_OWN = 2048
NT = 16
NE = 32
WEXT = 2176
QO, QSO, KO, KSO, VO, UO = 0, 512, 1024, 1280, 1536, 1664
CH = 512


class Reg:
    __slots__ = ("lw", "rd", "name")

    def __init__(self, name=""):
        self.lw = None
        self.rd = {}
        self.name = name


class Sched:
    CE = ("pe", "act", "dve", "pool")

    def __init__(self, nc, es, nslots=8):
        self.nc = nc
        self.sem, self.cnt, self.prog, self.waited = {}, {}, {}, {}
        for e in ("pe", "act", "dve", "pool", "sp"):
            self.prog[e] = []
            self.waited[e] = {}
        for e in self.CE:
            self.sem[e] = es.enter_context(nc.semaphore("sem_" + e))
            self.cnt[e] = 0
        self.ns = nslots
        self.dcount, self.dnext = {}, {}
        for q in ("sp", "pool", "bg"):
            self.dnext[q] = 0
            for s in range(nslots):
                k = ("d", q, s)
                self.sem[k] = es.enter_context(nc.semaphore("sd_%s_%d" % (q, s)))
                self.dcount[k] = 0

    def _deps(self, eng, reads, writes, skip_same=True):
        need = {}
        raw_same = 0

        def add(tok):
            if tok is None:
                return
            k, v = tok
            if need.get(k, 0) < v:
                need[k] = v

        for r in reads:
            add(r.lw)
            if r.lw is not None and r.lw[0] == eng and r.lw[1] > raw_same:
                raw_same = r.lw[1]
        for w in writes:
            add(w.lw)
            for k, v in w.rd.items():
                add((k, v))
        waits = []
        for k, v in need.items():
            if skip_same and k == eng:
                if eng == "pe" or raw_same == 0:
                    continue
                v = raw_same
            if self.waited[eng].get(k, 0) >= v:
                continue
            self.waited[eng][k] = v
            waits.append((k, v))
        return waits

    def op(self, eng, fn, reads=(), writes=()):
        waits = self._deps(eng, reads, writes)
        self.cnt[eng] += 1
        tok = (eng, self.cnt[eng])
        self.prog[eng].append((waits, fn, self.sem[eng], 1))
        for r in reads:
            if r.rd.get(eng, 0) < tok[1]:
                r.rd[eng] = tok[1]
        for w in writes:
            w.lw = tok
            w.rd = {}
        return tok

    def dma(self, q, out_ap, in_ap, reads=(), writes=(), ring=None):
        waits = self._deps(q, reads, writes, skip_same=False)
        ring = ring or q
        s = self.dnext[ring] % self.ns
        self.dnext[ring] += 1
        k = ("d", ring, s)
        c = self.dcount[k]
        if c > 0 and self.waited[q].get(k, 0) < 16 * c:
            self.waited[q][k] = 16 * c
            waits.append((k, 16 * c))
        self.dcount[k] = c + 1
        tok = (k, 16 * (c + 1))
        self.prog[q].append((waits, (lambda e: e.dma_start(out=out_ap, in_=in_ap)), self.sem[k], 16))
        for r in reads:
            if r.rd.get(k, 0) < tok[1]:
                r.rd[k] = tok[1]
        for w in writes:
            w.lw = tok
            w.rd = {}
        return tok

    def idma(self, q, out_ap, out_off, in_ap, in_off, bounds, reads=(), writes=()):
        waits = self._deps(q, reads, writes, skip_same=False)
        s = self.dnext[q] % self.ns
        self.dnext[q] += 1
        k = ("d", q, s)
        c = self.dcount[k]
        if c > 0 and self.waited[q].get(k, 0) < 16 * c:
            self.waited[q][k] = 16 * c
            waits.append((k, 16 * c))
        self.dcount[k] = c + 1
        tok = (k, 16 * (c + 1))
        self.prog[q].append((waits, (lambda e: e.indirect_dma_start(out=out_ap, out_offset=out_off, in_=in_ap, in_offset=in_off,
                                                                    bounds_check=None)), self.sem[k], 16))
        for r in reads:
            if r.rd.get(k, 0) < tok[1]:
                r.rd[k] = tok[1]
        for w in writes:
            w.lw = tok
            w.rd = {}
        return tok

    def barrier(self):
        cur = {e: self.cnt[e] for e in self.CE}
        for k, c in self.dcount.items():
            cur[k] = 16 * c
        for e in ("pe", "act", "dve", "pool", "sp"):
            waits = []
            for k, v in cur.items():
                if k == e or v == 0:
                    continue
                if self.waited[e].get(k, 0) >= v:
                    continue
                self.waited[e][k] = v
                waits.append((k, v))
            self.prog[e].append((waits, None, None, 0))

    def finish(self):
        waits = []
        for k, c in self.dcount.items():
            if c > 0:
                waits.append((k, 16 * c))
        self.prog["sp"].append((waits, None, None, 0))

    def emit(self):
        nc = self.nc

        def run(name):
            def f(e):
                for waits, fn, sem, inc in self.prog[name]:
                    for k, v in waits:
                        e.wait_ge(self.sem[k], v)
                    if fn is not None:
                        ins = fn(e)
                        ins.then_inc(sem, inc)
            return f

        with nc.Block() as block:
            block.tensor(run("pe"))
            block.scalar(run("act"))
            block.vector(run("dve"))
            block.gpsimd(run("pool"))
            block.sync(run("sp"))


DTS = {F32: 4, BF16: 2, U8: 1, I32: 4}


class Arena:
    def __init__(self, nc, nbytes):
        self.t = nc.alloc_sbuf_tensor("arena", [128, nbytes], U8)
        self.top = 0
        self.n = nbytes
        self.peak = 0
        self.hi = nbytes

    def alloc_top(self, free_shape, dt):
        size = int(np.prod(free_shape)) * DTS[dt]
        off = (self.hi - size) // 32 * 32
        self.hi = off
        ap = self.t[:, off:off + size].bitcast(dt)
        if len(free_shape) == 2:
            ap = ap.rearrange("p (a b) -> p a b", a=free_shape[0])
        return ap

    def alloc(self, free_shape, dt):
        size = int(np.prod(free_shape)) * DTS[dt]
        off = (self.top + 31) // 32 * 32
        self.top = off + size
        self.peak = max(self.peak, self.top)
        assert self.top <= self.hi, ("SBUF arena overflow", self.top, self.hi)
        ap = self.t[:, off:off + size].bitcast(dt)
        if len(free_shape) == 2:
            ap = ap.rearrange("p (a b) -> p a b", a=free_shape[0])
        elif len(free_shape) == 3:
            ap = ap.rearrange("p (a b c) -> p a b c", a=free_shape[0], b=free_shape[1])
        elif len(free_shape) == 4:
            ap = ap.rearrange("p (a b c d) -> p a b c d", a=free_shape[0], b=free_shape[1], c=free_shape[2])
        return ap


def bc_last(ap, m):
    return bass.AP(tensor=ap.tensor, offset=ap.offset, ap=[list(x) for x in ap.ap] + [[0, m]])


def rev(ap):
    dims = [list(x) for x in ap.ap]
    st, n = dims[-1]
    dims[-1] = [-st, n]
    return bass.AP(tensor=ap.tensor, offset=ap.offset + st * (n - 1), ap=dims)


def build(stage=99):
    nc = bass.Bass("TRN2", target_bir_lowering=False)
    es = ExitStack()

    def din(name, shape, dt=F32):
        return nc.dram_tensor(name, list(shape), dt, kind="ExternalInput").ap()

    x_core = din("x_core", [4096, D])
    c_col = din("c_col", [128, 8])
    w_ada = din("w_ada", [D, 6 * D])
    b_ada_rep = din("b_ada_rep", [128, 6 * D])
    gvecs = din("gvecs", [4, 128, D])
    w_ext = din("w_ext", [D, WEXT])
    rope_cos = din("rope_cos", [128, 2176])
    rope_sin = din("rope_sin", [128, 2176])
    sink_rep = din("sink_rep", [128, 8])
    mask_mid = din("mask_mid", [128, 384])
    ident_in = din("ident", [128, 128])
    iota1 = din("iota1", [128, CH])
    a_re_p = din("a_re_p", [128, 32])
    a_im_p = din("a_im_p", [128, 32])
    logdt_p = din("logdt_p", [128, 32])
    bpad_re = din("bpad_re", [32, 128, 128])
    bpad_im = din("bpad_im", [32, 128, 128])
    cpad_re = din("cpad_re", [32, 128, 128])
    cpad_im = din("cpad_im", [32, 128, 128])
    dskip_col = din("dskip_col", [128, 4])
    w_glu = din("w_glu", [512, 512])
    bglu_col = din("bglu_col", [128, 4])
    gcat_col = din("gcat_col", [128, 8])
    w_out = din("w_out", [D, D])
    w_router = din("w_router", [D, NE])
    b_router_rep = din("b_router_rep", [128, NE])
    wgu_rows = din("wgu_rows", [NE * 128 * 2, 4 * 2 * D])
    bgu_rows_in = din("bgu_rows", [NE * 128, 16])
    ltri_in = din("ltri", [128, 128])
    jb512_in = din("jb512", [128, 54])
    kp_iota_in = din("kp_iota", [128, 2])
    p_iota_in = din("p_iota", [128, 1])
    wdn_rows = din("wdn_rows", [NE * 128, 8 * D])
    b_down = din("b_down", [NE, D])
    y_out = nc.dram_tensor("y_out", [T_OWN, D], F32, kind="ExternalOutput").ap()
    dbg_outs = {}

    S = Sched(nc, es, nslots=12)
    A = Arena(nc, 204000)
    pst = [nc.alloc_psum_tensor("ps%d" % i, [128, 512], F32) for i in range(8)]
    PS = [t[:, :] for t in pst]
    PSB = [t[:, :].bitcast(BF16) for t in pst]
    PR = [Reg("ps%d" % i) for i in range(8)]
    bank_ctr = [0]

    def bank():
        i = bank_ctr[0] % 8
        bank_ctr[0] += 1
        return i

    def dbg(name, ap, shape, reg):
        t = nc.dram_tensor("dbg_" + name, list(shape), ap.dtype, kind="ExternalOutput").ap()
        dbg_outs[name] = t
        S.dma("sp", t, ap, reads=[reg])

    def ACT(out, in_, func, bias=None, scale=None, accum=None):
        kw = {}
        if bias is not None:
            kw["bias"] = bias
        if scale is not None:
            kw["scale"] = scale
        if accum is not None:
            kw["accum_out"] = accum
        return lambda e: e.activation(out=out, in_=in_, func=func, **kw)

    def TS(out, in0, s1, s2, op0, op1=None, accum=None):
        kw = {}
        if op1 is not None:
            kw["op1"] = op1
        if accum is not None:
            kw["accum_out"] = accum
        return lambda e: e.tensor_scalar(out=out, in0=in0, scalar1=s1, scalar2=s2, op0=op0, **kw)

    def TT(out, in0, in1, op):
        return lambda e: e.tensor_tensor(out=out, in0=in0, in1=in1, op=op)

    def STT(out, in0, sc, in1, op0, op1, accum=None):
        kw = {}
        if accum is not None:
            kw["accum_out"] = accum
        return lambda e: e.scalar_tensor_tensor(out=out, in0=in0, scalar=sc, in1=in1, op0=op0, op1=op1, **kw)

    def CP(out, in_):
        return lambda e: e.tensor_copy(out=out, in_=in_)

    def MM(groups):
        def f(e):
            ins = None
            for (o, l, r, st, sp_) in groups:
                ins = e.matmul(o, lhsT=l, rhs=r, start=st, stop=sp_)
            return ins
        return f

    def TR(pairs, ident):
        def f(e):
            ins = None
            for (o, i) in pairs:
                ins = e.transpose(o, i, ident)
            return ins
        return f


    def ACOPY(out, in_):
        return lambda e: e.copy(out=out, in_=in_)

    def AMUL(out, in_, m):
        return lambda e: e.mul(out=out, in_=in_, mul=m)

    def RECIP(out, in_):
        return lambda e: e.reciprocal(out=out, in_=in_)

    def MEMSET(ap, v):
        return lambda e: e.memset(ap, v)

    def SCAN(out, d0, d1, init):
        return lambda e: e.tensor_tensor_scan(out=out, data0=d0, data1=d1, initial=init, op0=ALU.mult, op1=ALU.add)

    ident = A.alloc([128], BF16)
    R_ident = Reg()
    S.dma("pool", ident, ident_in, writes=[R_ident])
    ones_col = A.alloc([16], BF16)
    R_ones = Reg()
    S.op("dve", MEMSET(ones_col, 1.0), writes=[R_ones])

    eps_col = A.alloc([1], F32)
    R_eps = Reg()
    S.op("dve", MEMSET(eps_col, 1e-6), writes=[R_eps])
    negpi = A.alloc([1], F32)
    S.op("dve", MEMSET(negpi, -PI), writes=[R_eps])
    stat = A.alloc([64], F32)
    R_stat = Reg()
    sqj = A.alloc([D], BF16)
    R_sqj = Reg()
    gtg2 = A.alloc([D], F32)
    mark_core = A.top
    mods5 = A.alloc([5, D], F32)
    R_mods = [Reg("mods%d" % i) for i in range(6)]

    def modsl(slot):
        return gtg2 if slot == 5 else mods5[:, slot, :]

    def modsl_s(slot, sl):
        return gtg2[:, sl] if slot == 5 else mods5[:, slot, sl]
    SEGMAP = {0: 1, 1: 0, 2: 2, 3: 4, 4: 3, 5: 5}
    mark_persist = A.top

    ccol = A.alloc([8], F32)
    R_cc = Reg()
    S.dma("sp", ccol, c_col, writes=[R_cc])
    csil = A.alloc([8], F32)
    R_cs = Reg()
    S.op("act", ACT(csil, ccol, AF.Silu), reads=[R_cc], writes=[R_cs])
    cl = A.alloc([8, 128], BF16)
    R_cl = Reg()
    for k in range(8):
        S.op("dve", CP(cl[:, k, :], csil[:, k:k + 1].to_broadcast([128, 128])), reads=[R_cs], writes=[R_cl])
    wada_buf = [A.alloc([8, 512], BF16) for _ in range(2)]
    R_wada = [Reg(), Reg()]
    bada_buf = [A.alloc([512], F32) for _ in range(2)]
    R_bada = [Reg(), Reg()]
    w_ada_v = w_ada.rearrange("(k p) n -> p k n", p=128)
    for j in range(12):
        bi = j % 2
        S.dma("pool", wada_buf[bi], w_ada_v[:, :, j * 512:(j + 1) * 512], writes=[R_wada[bi]])
        S.dma("sp", bada_buf[bi], b_ada_rep[:, j * 512:(j + 1) * 512], writes=[R_bada[bi]])
        b = bank()
        S.op("pe", MM([(PS[b], cl[:, k, :], wada_buf[bi][:, k, :], k == 0, k == 7) for k in range(8)]),
             reads=[R_cl, R_wada[bi]], writes=[PR[b]])
        slot = SEGMAP[j // 2]
        S.op("dve", TT(modsl_s(slot, slice((j % 2) * 512, (j % 2) * 512 + 512)), PS[b], bada_buf[bi], ALU.add),
             reads=[PR[b], R_bada[bi]], writes=[R_mods[slot]])
    gtmp = A.alloc([D], F32)
    R_gt = Reg()
    for (gi, slot, plus1) in ((0, 0, True), (1, 2, False), (2, 3, True), (3, 5, False)):
        S.dma("sp", gtmp, gvecs[gi], writes=[R_gt])
        if plus1:
            S.op("dve", STT(modsl(slot), modsl(slot), 1.0, gtmp, ALU.add, ALU.mult),
                 reads=[R_gt, R_mods[slot]], writes=[R_mods[slot]])
        else:
            S.op("dve", TT(modsl(slot), modsl(slot), gtmp, ALU.mult),
                 reads=[R_gt, R_mods[slot]], writes=[R_mods[slot]])
    if stage == 0:
        dbg("mods", mods5, [128, 5, D], R_mods[0])
        dbg("gtg2", gtg2, [128, D], R_mods[5])
        S.finish(); S.emit(); return nc, dbg_outs
    S.barrier()
    A.top = mark_persist

    wgu_bf = nc.dram_tensor("wgu_bf", [NE * 256, 8192], BF16, kind="Internal").ap()
    wdn_bf = nc.dram_tensor("wdn_bf", [NE * 128, 8192], BF16, kind="Internal").ap()
    bg_list = []
    for e_ in range(NE):
        bg_list.append((wgu_bf[e_ * 256:e_ * 256 + 128, :], wgu_rows[e_ * 256:e_ * 256 + 128, :]))
        bg_list.append((wgu_bf[e_ * 256 + 128:e_ * 256 + 256, :], wgu_rows[e_ * 256 + 128:e_ * 256 + 256, :]))
        bg_list.append((wdn_bf[e_ * 128:(e_ + 1) * 128, :], wdn_rows[e_ * 128:(e_ + 1) * 128, :]))
    bg_pos = [0]

    def bg_step(n=1):
        for _ in range(n):
            if bg_pos[0] < len(bg_list):
                o_, i_ = bg_list[bg_pos[0]]
                bg_pos[0] += 1
                S.dma("pool", o_, i_, ring="bg")

    uT = A.alloc([4, 4096], BF16)
    R_u = [Reg() for _ in range(8)]
    attnT = A.alloc([4, T_OWN], BF16)
    R_attnT = [Reg() for _ in range(NT)]
    rattn = A.alloc([NT], F32)
    R_rattn = Reg()
    mark_B = A.top
    qT = A.alloc([4, T_OWN], BF16)
    kT = A.alloc([2, 2176], BF16)
    vaug = A.alloc([17, 2, 65], BF16)
    maskb = A.alloc([384], BF16)
    R_mask = Reg()
    S.dma("pool", maskb, mask_mid, writes=[R_mask])
    R_q = [Reg() for _ in range(4)]
    R_k = [Reg() for _ in range(5)]
    R_v = [Reg() for _ in range(17)]
    S.op("pool", MEMSET(vaug, 1.0), writes=R_v)
    mark_C = A.top

    W = {}

    def alloc_norm_bufs():
        W["xt"] = [A.alloc([D], F32) for _ in range(2)]
        W["R_xt"] = [Reg(), Reg()]
        W["t1"] = A.alloc([D], F32)
        W["R_t1"] = Reg()
        W["hb"] = [A.alloc([D], BF16) for _ in range(2)]
        W["R_hb"] = [Reg(), Reg()]

    alloc_norm_bufs()
    wext = A.alloc([8, WEXT], BF16)
    R_wext = Reg()
    w_ext_v = w_ext.rearrange("(k p) n -> p k n", p=128)
    for k in range(0, 8, 2):
        S.dma("pool", wext[:, k:k + 2, :], w_ext_v[:, k:k + 2, :], writes=[R_wext])
    rcos = A.alloc([2176], BF16)
    rsin = A.alloc([2176], BF16)
    R_rope = Reg()
    S.dma("pool", rcos, rope_cos, writes=[R_rope])
    S.dma("pool", rsin, rope_sin, writes=[R_rope])
    hT = [A.alloc([8, 512], BF16) for _ in range(2)]
    R_hT = [Reg(), Reg()]
    ropet = [A.alloc([512], F32) for _ in range(4)]
    R_ropet = [Reg() for _ in range(4)]

    def prenorm_tile(src_ap, xbuf, R_x, gslot, shslot, dst_ap, R_dst, tcnt, load=True):
        bi = tcnt % 2
        t1, R_t1 = W["t1"], W["R_t1"]
        hb, R_hb = W["hb"][bi], W["R_hb"][bi]
        if load:
            S.dma("sp", xbuf, src_ap, writes=[R_x])
        S.op("act", ACT(sqj, xbuf, AF.Square, accum=stat[:, 0:1]), reads=[R_x], writes=[R_sqj, R_stat])
        S.op("act", ACT(stat[:, 1:2], stat[:, 0:1], AF.Sqrt, bias=eps_col, scale=1.0 / D), reads=[R_stat, R_eps], writes=[R_stat])
        S.op("dve", RECIP(stat[:, 2:3], stat[:, 1:2]), reads=[R_stat], writes=[R_stat])
        S.op("dve", STT(t1, xbuf, stat[:, 2:3], modsl(gslot), ALU.mult, ALU.mult),
             reads=[R_x, R_stat, R_mods[gslot]], writes=[R_t1])
        S.op("pool", TT(hb, t1, modsl(shslot), ALU.add), reads=[R_t1, R_mods[shslot]], writes=[R_hb])
        b = bank()
        S.op("pe", TR([(PSB[b][:, k * 128:(k + 1) * 128], hb[:, k * 128:(k + 1) * 128]) for k in range(8)], ident),
             reads=[R_hb, R_ident], writes=[PR[b]])
        S.op("act", ACOPY(dst_ap, PSB[b].rearrange("p (k c) -> p k c", k=8)), reads=[PR[b]], writes=[R_dst])

    def proj_fm(colblk_off, hTb, R_h, ntok_):
        b = bank()
        S.op("pe", MM([(PS[b][:, :ntok_], wext[:, k, colblk_off:colblk_off + 128], hTb[:, k, :ntok_], k == 0, k == 7)
                       for k in range(8)]), reads=[R_wext, R_h], writes=[PR[b]])
        return b

    tcnt = 0
    nblk = 8 if stage >= 3 else 5
    for blk in range(nblk):
        own = blk < 4
        ntile = 4 if (own or stage >= 3) else 1
        hb_i = blk % 2
        for tl in range(ntile):
            tile_i = blk * 4 + tl
            xi = tcnt % 2
            prenorm_tile(x_core[tile_i * 128:(tile_i + 1) * 128, :], W["xt"][xi], W["R_xt"][xi], 0, 1,
                         hT[hb_i][:, :, tl * 128:(tl + 1) * 128], R_hT[hb_i], tcnt)
            tcnt += 1
            bg_step(1)
        ntok = ntile * 128
        tok0 = blk * 512
        if own or blk == 4:
            kt = 512 if own else 128
            plist = []
            if own:
                plist += [("q", cb, QO + cb * 128, QSO + cb * 128) for cb in range(4)]
            plist += [("k", kb, KO + kb * 128, KSO + kb * 128) for kb in range(2)]
            for pi, (kind, cb, o1, o2) in enumerate(plist):
                ba = proj_fm(o1, hT[hb_i], R_hT[hb_i], kt)
                bb = proj_fm(o2, hT[hb_i], R_hT[hb_i], kt)
                ra, rb = ropet[(2 * pi) % 4], ropet[(2 * pi + 1) % 4]
                Ra, Rb = R_ropet[(2 * pi) % 4], R_ropet[(2 * pi + 1) % 4]
                S.op("dve", TT(ra[:, :kt], PS[ba][:, :kt], rcos[:, tok0:tok0 + kt], ALU.mult), reads=[PR[ba], R_rope], writes=[Ra])
                S.op("dve", TT(rb[:, :kt], PS[bb][:, :kt], rsin[:, tok0:tok0 + kt], ALU.mult), reads=[PR[bb], R_rope], writes=[Rb])
                if kind == "q":
                    S.op("pool", TT(qT[:, cb, tok0:tok0 + kt], ra[:, :kt], rb[:, :kt], ALU.add), reads=[Ra, Rb], writes=[R_q[blk]])
                else:
                    S.op("pool", TT(kT[:, cb, tok0:tok0 + kt], ra[:, :kt], rb[:, :kt], ALU.add), reads=[Ra, Rb], writes=[R_k[blk]])
            for tl in range(4 if own else 1):
                tile_i = blk * 4 + tl
                b = bank()
                S.op("pe", MM([(PS[b][:, :128], hT[hb_i][:, k, tl * 128:(tl + 1) * 128], wext[:, k, VO:VO + 128], k == 0, k == 7)
                               for k in range(8)]), reads=[R_wext, R_hT[hb_i]], writes=[PR[b]])
                S.op("act", ACOPY(vaug[:, tile_i, :, 0:64], PS[b][:, :128].rearrange("p (h c) -> p h c", h=2)),
                     reads=[PR[b]], writes=[R_v[tile_i]])
        for cb in range(4):
            b = proj_fm(UO + cb * 128, hT[hb_i], R_hT[hb_i], ntok)
            S.op("act", ACOPY(uT[:, cb, tok0:tok0 + ntok], PS[b][:, :ntok]), reads=[PR[b]], writes=[R_u[blk]])
    if stage == 1:
        dbg("qT", qT, [128, 4, T_OWN], R_q[3])
        dbg("kT", kT, [128, 2, 2176], R_k[4])
        dbg("vaug", vaug, [128, 17, 2, 65], R_v[16])
        dbg("uT", uT, [128, 4, 4096], R_u[4])
        S.finish(); S.emit(); return nc, dbg_outs
    S.barrier()
    A.top = mark_C

    esink = A.alloc([8], F32)
    R_es = Reg()
    S.dma("sp", esink, sink_rep, writes=[R_es])
    S.op("act", ACT(esink, esink, AF.Exp), reads=[R_es], writes=[R_es])
    pT = A.alloc([4, 8, 384], BF16)
    R_pT = [[Reg() for _ in range(8)] for _ in range(4)]
    attn_o = [A.alloc([512], BF16) for _ in range(2)]
    R_ao = [Reg(), Reg()]
    den = A.alloc([16], F32)
    R_den = Reg()

    def kq_range(j):
        return max(j - 1, 0), min(j + 2, 16)

    def pv_block(n):
        bo = [bank(), bank()]
        ai = n % 2
        for hh in range(2):
            groups, regs = [], []
            for h4 in range(4):
                h = hh * 4 + h4
                kvh = h // 4
                js = [j for j in (n - 1, n, n + 1) if 0 <= j <= 16]
                for ji, j in enumerate(js):
                    qb0, _ = kq_range(j)
                    c0 = (n - qb0) * 128
                    groups.append((PS[bo[hh]][:, h4 * 65:(h4 + 1) * 65], pT[:, j % 4, h, c0:c0 + 128], vaug[:, j, kvh, :],
                                   ji == 0, ji == len(js) - 1))
                    regs.append(R_pT[j % 4][h])
                    regs.append(R_v[j])
            S.op("pe", MM(groups), reads=regs, writes=[PR[bo[hh]]])
        for hh in range(2):
            pv = PS[bo[hh]][:, 0:260].rearrange("p (h c) -> p h c", h=4)
            S.op("dve", TT(den[:, hh * 4:hh * 4 + 4], pv[:, :, 64], esink[:, hh * 4:hh * 4 + 4], ALU.add),
                 reads=[PR[bo[hh]], R_es], writes=[R_den])
        S.op("dve", RECIP(den[:, 8:16], den[:, 0:8]), reads=[R_den], writes=[R_den])
        for hh in range(2):
            pv = PS[bo[hh]][:, 0:260].rearrange("p (h c) -> p h c", h=4)
            S.op("dve", TT(attn_o[ai][:, hh * 256:(hh + 1) * 256].rearrange("p (h c) -> p h c", h=4), pv[:, :, 0:64],
                           bc_last(den[:, 8 + hh * 4:8 + hh * 4 + 4], 64), ALU.mult),
                 reads=[PR[bo[hh]], R_den], writes=[R_ao[ai]])
        S.op("act", ACT(sqj[:, 0:512], attn_o[ai], AF.Square, accum=stat[:, 8:9]), reads=[R_ao[ai]], writes=[R_sqj, R_stat])
        S.op("act", ACT(stat[:, 9:10], stat[:, 8:9], AF.Sqrt, bias=eps_col, scale=1.0 / 512), reads=[R_stat, R_eps], writes=[R_stat])
        S.op("dve", RECIP(rattn[:, n:n + 1], stat[:, 9:10]), reads=[R_stat], writes=[R_rattn])
        b = bank()
        S.op("pe", TR([(PSB[b][:, k * 128:(k + 1) * 128], attn_o[ai][:, k * 128:(k + 1) * 128]) for k in range(4)], ident),
             reads=[R_ao[ai], R_ident], writes=[PR[b]])
        S.op("act", ACOPY(attnT[:, :, n * 128:(n + 1) * 128], PSB[b][:, 0:512].rearrange("p (k c) -> p k c", k=4)),
             reads=[PR[b]], writes=[R_attnT[n]])

    for j in range(17):
        qb0, qb1 = kq_range(j)
        ncol = (qb1 - qb0) * 128
        moff = 128 if j == 0 else 0
        for h in range(8):
            kvh, qblk, pr = h // 4, h // 2, (h % 2) * 64
            b = bank()
            S.op("pe", MM([(PS[b][:, :ncol], kT[pr:pr + 64, kvh, j * 128:(j + 1) * 128], qT[pr:pr + 64, qblk, qb0 * 128:qb1 * 128], True, False),
                           (PS[b][:, :ncol], ident, maskb[:, moff:moff + ncol], False, True)]),
                 reads=[R_k[min(j // 4, 4)], R_q[0], R_q[1], R_q[2], R_q[3], R_ident, R_mask], writes=[PR[b]])
            S.op("act", ACT(pT[:, j % 4, h, :ncol], PS[b][:, :ncol], AF.Exp, scale=0.125), reads=[PR[b]], writes=[R_pT[j % 4][h]])
        if j >= 1:
            pv_block(j - 1)
        bg_step(1)
    if stage == 2:
        dbg("attnT", attnT, [128, 4, T_OWN], R_attnT[15])
        dbg("rattn", rattn, [128, NT], R_rattn)
        S.finish(); S.emit(); return nc, dbg_outs
    S.barrier()
    A.top = mark_B

    y2T = A.alloc([4, T_OWN], BF16)
    R_y2T = [Reg() for _ in range(4)]
    rssm = A.alloc([NT], F32)
    R_rssm = Reg()
    mark_D = A.top
    are = A.alloc([32], F32); aim = A.alloc([32], F32); ldt = A.alloc([32], F32)
    R_sc = Reg()
    S.dma("sp", are, a_re_p, writes=[R_sc])
    S.dma("sp", aim, a_im_p, writes=[R_sc])
    S.dma("sp", ldt, logdt_p, writes=[R_sc])
    dtv = A.alloc([32], F32); lr = A.alloc([32], F32); li = A.alloc([32], F32); rmag = A.alloc([32], F32)
    tmpa = A.alloc([32], F32); tmpb = A.alloc([32], F32); cosl = A.alloc([32], F32); sinl = A.alloc([32], F32)
    fr = A.alloc([32], F32); fi = A.alloc([32], F32); dnm = A.alloc([32], F32)
    dsk = A.alloc([4], F32)
    kis = A.alloc([32], I32)
    S.dma("sp", dsk, dskip_col, writes=[R_sc])
    seq = [
        ("act", ACT(dtv, ldt, AF.Exp)),
        ("dve", TT(lr, are, dtv, ALU.mult)),
        ("dve", TT(li, aim, dtv, ALU.mult)),
        ("act", ACT(rmag, lr, AF.Exp)),
        ("dve", TS(kis, li, 1.0 / (2 * PI), None, ALU.mult)),
        ("dve", CP(tmpa, kis)),
        ("dve", STT(tmpa, tmpa, -2 * PI, li, ALU.mult, ALU.add)),
        ("dve", TS(tmpa, tmpa, 3.1415925, -3.1415925, ALU.min, ALU.max)),
        ("act", ACT(sinl, tmpa, AF.Sin)),
        ("dve", TS(tmpb, li, PI / 2, None, ALU.add)),
        ("dve", TS(kis, tmpb, 1.0 / (2 * PI), None, ALU.mult)),
        ("dve", CP(tmpa, kis)),
        ("dve", STT(tmpa, tmpa, -2 * PI, tmpb, ALU.mult, ALU.add)),
        ("dve", TS(tmpa, tmpa, 3.1415925, -3.1415925, ALU.min, ALU.max)),
        ("act", ACT(cosl, tmpa, AF.Sin)),
        ("dve", TT(cosl, cosl, rmag, ALU.mult)),
        ("dve", TT(sinl, sinl, rmag, ALU.mult)),
        ("dve", TS(cosl, cosl, -1.0, None, ALU.add)),
        ("dve", TT(dnm, are, are, ALU.mult)),
        ("dve", TT(tmpa, aim, aim, ALU.mult)),
        ("dve", TT(dnm, dnm, tmpa, ALU.add)),
        ("dve", RECIP(dnm, dnm)),
        ("dve", TT(tmpa, cosl, are, ALU.mult)),
        ("dve", TT(tmpb, sinl, aim, ALU.mult)),
        ("dve", TT(fr, tmpa, tmpb, ALU.add)),
        ("dve", TT(fr, fr, dnm, ALU.mult)),
        ("dve", TT(tmpa, sinl, are, ALU.mult)),
        ("dve", TT(tmpb, cosl, aim, ALU.mult)),
        ("dve", TT(fi, tmpa, tmpb, ALU.subtract)),
        ("dve", TT(fi, fi, dnm, ALU.mult)),
    ]
    for eng, fn in seq:
        S.op(eng, fn, reads=[R_sc, R_eps], writes=[R_sc])
    iot = A.alloc([CH], F32)
    R_iot = Reg()
    S.dma("sp", iot, iota1, writes=[R_iot])

    NG = 4
    wl = [[A.alloc([128], BF16) for _ in range(2)] for _ in range(NG)]
    cm = [[A.alloc([128], BF16) for _ in range(2)] for _ in range(NG)]
    tabC = [A.alloc([CH], F32) for _ in range(NG)]
    tabS = [A.alloc([CH], F32) for _ in range(NG)]
    R_gc = [Reg() for _ in range(NG)]
    ldb = [A.alloc([128], F32) for _ in range(4)]
    R_ldb = [Reg() for _ in range(4)]
    bbf = [A.alloc([128], BF16) for _ in range(2)]
    R_bbf = [Reg(), Reg()]
    NW = 4
    mt = [[A.alloc([CH], F32) for _ in range(4)] for _ in range(NW)]
    qt = [[A.alloc([CH], BF16) for _ in range(2)] for _ in range(NW)]
    R_mt = [[Reg() for _ in range(4)] for _ in range(NW)]
    R_qt = [Reg() for _ in range(NW)]
    mtmp = mt
    R_w = [R_mt[0][0], R_mt[1][0]]
    argt, argk = mt[3][0], mt[3][1]
    kint = mt[3][2].bitcast(I32)
    R_argt = R_mt[3][0]
    R_argk = R_mt[3][1]
    R_kint = R_mt[3][2]
    sro = [[A.alloc([CH], BF16) for _ in range(2)] for _ in range(NG)]
    R_sro = [Reg() for _ in range(NG)]
    sprev = [A.alloc([2], F32) for _ in range(NG)]
    R_sprev = [Reg() for _ in range(NG)]
    cs5 = [A.alloc([4], F32) for _ in range(NG)]
    ltmp = [A.alloc([2], F32) for _ in range(NW)]
    ysum = A.alloc([T_OWN], F32)
    R_ys = [Reg() for _ in range(4)]
    gact = A.alloc([4, T_OWN], BF16)
    R_gact = [Reg() for _ in range(4)]
    wcount = [0]

    for cb in range(4):
        for d in range(2):
            for gs in range(NG):
                G = cb * 4 + gs
                dg = d * 16 + G
                S.dma("sp", ldb[0], bpad_re[dg], writes=[R_ldb[0]])
                S.dma("sp", ldb[1], bpad_im[dg], writes=[R_ldb[1]])
                S.dma("sp", ldb[2], cpad_re[dg], writes=[R_ldb[2]])
                S.dma("sp", ldb[3], cpad_im[dg], writes=[R_ldb[3]])
                frc, fic = fr[:, dg:dg + 1], fi[:, dg:dg + 1]
                ta, tb_ = mt[0][0][:, :128], mt[0][1][:, :128]
                S.op("dve", TS(ta, ldb[1], fic, None, ALU.mult), reads=[R_ldb[1], R_sc], writes=[R_mt[0][0]])
                S.op("dve", STT(bbf[0], ldb[0], frc, ta, ALU.mult, ALU.subtract), reads=[R_ldb[0], R_sc, R_mt[0][0]], writes=[R_bbf[0]])
                S.op("dve", TS(tb_, ldb[0], fic, None, ALU.mult), reads=[R_ldb[0], R_sc], writes=[R_mt[0][1]])
                S.op("dve", STT(bbf[1], ldb[1], frc, tb_, ALU.mult, ALU.add), reads=[R_ldb[1], R_sc, R_mt[0][1]], writes=[R_bbf[1]])
                b = bank()
                S.op("pe", TR([(PSB[b][:, 0:128], bbf[0]), (PSB[b][:, 128:256], bbf[1])], ident),
                     reads=[R_bbf[0], R_bbf[1], R_ident], writes=[PR[b]])
                S.op("act", ACOPY(wl[gs][0], PSB[b][:, 0:128]), reads=[PR[b]], writes=[R_gc[gs]])
                S.op("act", ACOPY(wl[gs][1], PSB[b][:, 128:256]), reads=[PR[b]], writes=[R_gc[gs]])
                S.op("act", ACOPY(cm[gs][0], ldb[2]), reads=[R_ldb[2]], writes=[R_gc[gs]])
                S.op("act", AMUL(cm[gs][1], ldb[3], -1.0), reads=[R_ldb[3]], writes=[R_gc[gs]])
                lic = li[:, dg:dg + 1]
                S.op("dve", TS(argt, iot, lic, None, ALU.mult), reads=[R_iot, R_sc], writes=[R_argt])
                for (tab, shift) in ((tabS[gs], False), (tabC[gs], True)):
                    if shift:
                        S.op("dve", TS(argt, argt, PI / 2, None, ALU.add), reads=[R_argt], writes=[R_argt])
                    S.op("dve", TS(kint, argt, 1.0 / (2 * PI), None, ALU.mult), reads=[R_argt], writes=[R_kint])
                    S.op("dve", CP(argk, kint), reads=[R_kint], writes=[R_argk])
                    S.op("dve", STT(argk, argk, -2 * PI, argt, ALU.mult, ALU.add), reads=[R_argt, R_argk], writes=[R_argk])
                    S.op("dve", TS(argk, argk, 3.1415925, -3.1415925, ALU.min, ALU.max), reads=[R_argk], writes=[R_argk])
                    S.op("act", ACT(tab, argk, AF.Sin), reads=[R_argk], writes=[R_gc[gs]])
                S.op("dve", CP(cs5[gs][:, 0:1], tabC[gs][:, CH - 1:CH]), reads=[R_gc[gs]], writes=[R_sprev[gs]])
                S.op("dve", CP(cs5[gs][:, 1:2], tabS[gs][:, CH - 1:CH]), reads=[R_gc[gs]], writes=[R_sprev[gs]])
                S.op("dve", TS(cs5[gs][:, 2:3], tabS[gs][:, CH - 1:CH], -1.0, None, ALU.mult), reads=[R_gc[gs]], writes=[R_sprev[gs]])
                S.op("dve", CP(cs5[gs][:, 3:4], tabC[gs][:, CH - 1:CH]), reads=[R_gc[gs]], writes=[R_sprev[gs]])
                S.op("dve", MEMSET(sprev[gs], 0.0), writes=[R_sprev[gs]])
            chunks = [0, 1, 2, 3] if d == 0 else [7, 6, 5, 4, 3, 2, 1, 0]
            for c in chunks:
                is_own = c < 4
                t0 = c * CH
                for gs in range(NG):
                    G = cb * 4 + gs
                    dg = d * 16 + G
                    wi = wcount[0] % NW
                    wcount[0] += 1
                    if wcount[0] % 3 == 0:
                        bg_step(1)
                    m, Rm = mt[wi], R_mt[wi]
                    ba, bb = bank(), bank()
                    S.op("pe", MM([(PS[ba], wl[gs][0], uT[:, cb, t0:t0 + CH], True, True)]), reads=[R_gc[gs], R_u[c]], writes=[PR[ba]])
                    S.op("pe", MM([(PS[bb], wl[gs][1], uT[:, cb, t0:t0 + CH], True, True)]), reads=[R_gc[gs], R_u[c]], writes=[PR[bb]])
                    tC = tabC[gs] if d == 0 else rev(tabC[gs])
                    tS = tabS[gs] if d == 0 else rev(tabS[gs])
                    S.op("dve", TT(m[0], PS[ba], tC, ALU.mult), reads=[PR[ba], R_gc[gs]], writes=[Rm[0]])
                    S.op("dve", TT(m[1], PS[bb], tS, ALU.mult), reads=[PR[bb], R_gc[gs]], writes=[Rm[1]])
                    S.op("dve", TT(m[2], PS[bb], tC, ALU.mult), reads=[PR[bb], R_gc[gs]], writes=[Rm[2]])
                    S.op("dve", TT(m[3], PS[ba], tS, ALU.mult), reads=[PR[ba], R_gc[gs]], writes=[Rm[3]])
                    S.op("pool", TT(m[0], m[0], m[1], ALU.add), reads=[Rm[0], Rm[1]], writes=[Rm[0]])
                    S.op("pool", TT(m[2], m[2], m[3], ALU.subtract), reads=[Rm[2], Rm[3]], writes=[Rm[2]])
                    rcol = rmag[:, dg:dg + 1]
                    rb = bass.AP(tensor=rcol.tensor, offset=rcol.offset, ap=[list(rcol.ap[0]), [0, CH]])
                    for ri, (src, dst) in enumerate(((0, 1), (2, 3))):
                        o = m[dst] if d == 0 else rev(m[dst])
                        i1 = m[src] if d == 0 else rev(m[src])
                        S.op("dve", SCAN(o, rb, i1, sprev[gs][:, ri:ri + 1]),
                             reads=[Rm[src], R_sprev[gs], R_sc], writes=[Rm[dst]])
                    lc = CH - 1 if d == 0 else 0
                    sr_l, si_l = m[1][:, lc:lc + 1], m[3][:, lc:lc + 1]
                    S.op("dve", TS(ltmp[wi], cs5[gs][:, 2:4], si_l, None, ALU.mult), reads=[Rm[3], R_sprev[gs]], writes=[R_qt[wi]])
                    S.op("dve", STT(sprev[gs], cs5[gs][:, 0:2], sr_l, ltmp[wi], ALU.mult, ALU.add), reads=[Rm[1], R_qt[wi], R_sprev[gs]], writes=[R_sprev[gs]])
                    if is_own:
                        S.op("dve", TT(m[0], m[1], tC, ALU.mult), reads=[Rm[1], R_gc[gs]], writes=[Rm[0]])
                        S.op("dve", TT(m[2], m[3], tS, ALU.mult), reads=[Rm[3], R_gc[gs]], writes=[Rm[2]])
                        S.op("pool", TT(qt[wi][0], m[1], tS, ALU.mult), reads=[Rm[1], R_gc[gs]], writes=[R_qt[wi]])
                        S.op("pool", TT(qt[wi][1], m[3], tC, ALU.mult), reads=[Rm[3], R_gc[gs]], writes=[R_qt[wi]])
                        S.op("dve", TT(sro[gs][0], m[0], m[2], ALU.subtract), reads=[Rm[0], Rm[2]], writes=[R_sro[gs]])
                        S.op("dve", TT(sro[gs][1], qt[wi][0], qt[wi][1], ALU.add), reads=[R_qt[wi]], writes=[R_sro[gs]])
                if is_own:
                    b = bank()
                    groups = []
                    for gs in range(NG):
                        groups.append((PS[b], cm[gs][0], sro[gs][0], gs == 0, False))
                        groups.append((PS[b], cm[gs][1], sro[gs][1], False, gs == NG - 1))
                    S.op("pe", MM(groups), reads=R_gc + R_sro, writes=[PR[b]])
                    ys = ysum[:, t0:t0 + CH]
                    if d == 0:
                        S.op("dve", STT(ys, uT[:, cb, t0:t0 + CH], dsk[:, cb:cb + 1], PS[b], ALU.mult, ALU.add),
                             reads=[PR[b], R_u[c], R_sc], writes=[R_ys[c]])
                    else:
                        S.op("dve", TT(ys, ys, PS[b], ALU.add), reads=[PR[b], R_ys[c]], writes=[R_ys[c]])
        for c in range(4):
            ys = ysum[:, c * CH:(c + 1) * CH]
            g1, g2 = mt[c % 2][0], mt[c % 2][1]
            Rg1, Rg2 = R_mt[c % 2][0], R_mt[c % 2][1]
            S.op("act", ACT(g1, ys, AF.Square), reads=[R_ys[c]], writes=[Rg1])
            S.op("dve", TS(g1, g1, 0.044715, 1.0, ALU.mult, ALU.add), reads=[Rg1], writes=[Rg1])
            S.op("dve", TT(g1, g1, ys, ALU.mult), reads=[Rg1, R_ys[c]], writes=[Rg1])
            S.op("act", ACT(g2, g1, AF.Sigmoid, scale=1.5957691216057308), reads=[Rg1], writes=[Rg2])
            S.op("pool", TT(gact[:, cb, c * CH:(c + 1) * CH], ys, g2, ALU.mult), reads=[Rg2, R_ys[c]], writes=[R_gact[cb]])
    if stage == 3:
        dbg("gact", gact, [128, 4, T_OWN], R_gact[3])
        S.finish(); S.emit(); return nc, dbg_outs

    wglu = A.alloc([4, 512], BF16)
    R_wglu = Reg()
    S.dma("pool", wglu, w_glu.rearrange("(k p) n -> p k n", p=128), writes=[R_wglu])
    bglu = A.alloc([4], F32)
    S.dma("sp", bglu, bglu_col, writes=[R_wglu])
    ysq = [A.alloc([512], BF16) for _ in range(4)]
    R_ysq = [Reg() for _ in range(4)]
    sgt = [mt[2][0], mt[2][1]]
    R_sgt = [R_mt[2][0], R_mt[2][1]]
    ci = 0
    for tb in range(4):
        tsl = slice(tb * 512, (tb + 1) * 512)
        for cbo in range(4):
            b = bank()
            S.op("pe", MM([(PS[b], wglu[:, k, cbo * 128:(cbo + 1) * 128], gact[:, k, tsl], k == 0, k == 3) for k in range(4)]),
                 reads=[R_wglu] + R_gact, writes=[PR[b]])
            si = ci % 2
            ci += 1
            S.op("act", ACT(sgt[si], PS[b], AF.Sigmoid, bias=bglu[:, cbo:cbo + 1]), reads=[PR[b], R_wglu], writes=[R_sgt[si]])
            S.op("dve", TT(y2T[:, cbo, tsl], gact[:, cbo, tsl], sgt[si], ALU.mult),
                 reads=[R_sgt[si], R_gact[cbo]], writes=[R_y2T[cbo]])
            S.op("pool", TT(ysq[cbo], y2T[:, cbo, tsl], y2T[:, cbo, tsl], ALU.mult),
                 reads=[R_y2T[cbo]], writes=[R_ysq[cbo]])
        bss = bank()
        groups = []
        for tl in range(4):
            for cbo in range(4):
                groups.append((PS[bss][:, tl * 16:(tl + 1) * 16], ysq[cbo][:, tl * 128:(tl + 1) * 128], ones_col, cbo == 0, cbo == 3))
        S.op("pe", MM(groups), reads=R_ysq + [R_ones], writes=[PR[bss]])
        S.op("act", ACT(stat[:, 10:14], PS[bss][:, 0:64].rearrange("p (t c) -> p t c", c=16)[:, :, 0], AF.Sqrt, bias=eps_col, scale=1.0 / 512), reads=[PR[bss], R_eps], writes=[R_stat])
        S.op("dve", RECIP(rssm[:, tb * 4:tb * 4 + 4], stat[:, 10:14]), reads=[R_stat], writes=[R_rssm])
    if stage == 4:
        dbg("y2T", y2T, [128, 4, T_OWN], R_y2T[3])
        dbg("rssm", rssm, [128, NT], R_rssm)
        S.finish(); S.emit(); return nc, dbg_outs
    S.barrier()
    A.top = mark_D

    BR = 384
    NBLK = 54
    NTB = BR // 128
    NROWS = NBLK * BR
    xs_d = nc.dram_tensor("xs_scr", [NROWS, D], BF16, kind="Internal").ap()
    ys_d = nc.dram_tensor("ys_scr", [NROWS, D], F32, kind="Internal").ap()
    gates = A.alloc_top([NT, NE], F32)
    R_gates = [Reg() for _ in range(NT)]
    maskall = A.alloc_top([NT, NE], BF16)
    R_maskall = [Reg() for _ in range(NT)]
    dest4i = A.alloc_top([NT, 4], I32)
    gate4 = A.alloc_top([NT, 4], F32)
    widx = A.alloc_top([NBLK, 2], I32)
    bidx = A.alloc_top([NBLK], I32)
    eidx = A.alloc_top([NBLK], I32)
    R_meta = Reg()
    mark_top_meta = A.hi
    alloc_norm_bufs()
    h2tm = A.alloc([NT, D], BF16)
    R_h2tm = [Reg() for _ in range(NT)]
    h2Tt = [A.alloc([8, 128], BF16) for _ in range(2)]
    R_h2Tt = [Reg(), Reg()]
    wout = A.alloc([8, D], BF16)
    R_wout = Reg()
    w_out_v = w_out.rearrange("(k p) n -> p k n", p=128)
    S.dma("pool", wout[:, 0:4, :], w_out_v[:, 0:4, :], writes=[R_wout])
    S.dma("pool", wout[:, 4:8, :], w_out_v[:, 4:8, :], writes=[R_wout])
    gcat = A.alloc([8], F32)
    S.dma("sp", gcat, gcat_col, writes=[R_wout])
    for k in range(8):
        S.op("dve", TS(wout[:, k, :], wout[:, k, :], gcat[:, k:k + 1], None, ALU.mult), reads=[R_wout], writes=[R_wout])
    wr = A.alloc([8, NE], BF16)
    R_wr = Reg()
    S.dma("pool", wr, w_router.rearrange("(k p) n -> p k n", p=128), writes=[R_wr])
    brt = A.alloc([NE], F32)
    S.dma("sp", brt, b_router_rep, writes=[R_wr])
    ym = [A.alloc([D], F32) for _ in range(2)]
    R_ym = [Reg(), Reg()]
    xn = [A.alloc([D], F32) for _ in range(2)]
    R_xn = [Reg(), Reg()]
    lg = A.alloc([NE], F32)
    top8 = A.alloc([8], F32)
    eg = A.alloc([NE], F32)
    msk = A.alloc([NE], F32)
    R_lg = Reg()
    R_xmid = [Reg() for _ in range(NT)]

    def prenorm2_tile(xbuf, R_x, n):
        t1, R_t1 = W["t1"], W["R_t1"]
        S.op("act", ACT(sqj, xbuf, AF.Square, accum=stat[:, 0:1]), reads=[R_x], writes=[R_sqj, R_stat])
        S.op("act", ACT(stat[:, 1:2], stat[:, 0:1], AF.Sqrt, bias=eps_col, scale=1.0 / D), reads=[R_stat, R_eps], writes=[R_stat])
        S.op("dve", RECIP(stat[:, 2:3], stat[:, 1:2]), reads=[R_stat], writes=[R_stat])
        S.op("dve", STT(t1, xbuf, stat[:, 2:3], modsl(3), ALU.mult, ALU.mult), reads=[R_x, R_stat, R_mods[3]], writes=[R_t1])
        S.op("dve", TT(h2tm[:, n, :], t1, modsl(4), ALU.add), reads=[R_t1, R_mods[4]], writes=[R_h2tm[n]])
        b = bank()
        S.op("pe", TR([(PSB[b][:, k * 128:(k + 1) * 128], h2tm[:, n, k * 128:(k + 1) * 128]) for k in range(8)], ident),
             reads=[R_h2tm[n], R_ident], writes=[PR[b]])
        S.op("act", ACOPY(h2Tt[n % 2], PSB[b].rearrange("p (k c) -> p k c", k=8)), reads=[PR[b]], writes=[R_h2Tt[n % 2]])

    for n in range(NT):
        i2 = n % 2
        nsl = slice(n * 128, (n + 1) * 128)
        bA = [bank(), bank()]
        bS = [bank(), bank()]
        for half in range(2):
            hs = slice(half * 512, (half + 1) * 512)
            S.op("pe", MM([(PS[bA[half]], attnT[:, k, nsl], wout[:, k, hs], k == 0, k == 3) for k in range(4)]),
                 reads=[R_attnT[n], R_wout], writes=[PR[bA[half]]])
            S.op("pe", MM([(PS[bS[half]], y2T[:, k, nsl], wout[:, 4 + k, hs], k == 0, k == 3) for k in range(4)]),
                 reads=R_y2T + [R_wout], writes=[PR[bS[half]]])
        for half in range(2):
            hs = slice(half * 512, (half + 1) * 512)
            S.op("dve", TS(ym[i2][:, hs], PS[bA[half]], rattn[:, n:n + 1], None, ALU.mult), reads=[PR[bA[half]], R_rattn], writes=[R_ym[i2]])
            S.op("dve", STT(ym[i2][:, hs], PS[bS[half]], rssm[:, n:n + 1], ym[i2][:, hs], ALU.mult, ALU.add),
                 reads=[PR[bS[half]], R_rssm, R_ym[i2]], writes=[R_ym[i2]])
        xi = n % 2
        xtb, R_xtb = W["xt"][xi], W["R_xt"][xi]
        S.dma("sp", xtb, x_core[nsl, :], writes=[R_xtb])
        S.op("act", ACT(sqj, ym[i2], AF.Square, accum=stat[:, 16:17]), reads=[R_ym[i2]], writes=[R_sqj, R_stat])
        S.op("act", ACT(stat[:, 17:18], stat[:, 16:17], AF.Sqrt, bias=eps_col, scale=1.0 / D), reads=[R_stat, R_eps], writes=[R_stat])
        S.op("dve", RECIP(stat[:, 18:19], stat[:, 17:18]), reads=[R_stat], writes=[R_stat])
        S.op("dve", STT(ym[i2], ym[i2], stat[:, 18:19], modsl(2), ALU.mult, ALU.mult), reads=[R_ym[i2], R_stat, R_mods[2]], writes=[R_ym[i2]])
        S.op("dve", TT(xn[i2], ym[i2], xtb, ALU.add), reads=[R_ym[i2], R_xtb], writes=[R_xn[i2]])
        S.dma("sp", y_out[nsl, :], xn[i2], reads=[R_xn[i2]], writes=[R_xmid[n]])
        prenorm2_tile(xn[i2], R_xn[i2], n)
        b = bank()
        S.op("pe", MM([(PS[b][:, 0:NE], h2Tt[n % 2][:, k, :], wr[:, k, :], k == 0, k == 7) for k in range(8)]),
             reads=[R_h2Tt[n % 2], R_wr], writes=[PR[b]])
        S.op("dve", TT(lg, PS[b][:, 0:NE], brt, ALU.add), reads=[PR[b], R_wr], writes=[R_lg])
        S.op("dve", (lambda e: e.max(out=top8, in_=lg)), reads=[R_lg], writes=[R_lg])
        S.op("dve", TS(msk, lg, top8[:, 3:4], None, ALU.is_ge), reads=[R_lg], writes=[R_lg])
        S.op("dve", CP(maskall[:, n, :], msk), reads=[R_lg], writes=[R_maskall[n]])
        S.op("dve", TS(stat[:, 20:21], top8[:, 0:1], -1.0, None, ALU.mult), reads=[R_lg], writes=[R_stat])
        S.op("act", ACT(eg, lg, AF.Exp, bias=stat[:, 20:21]), reads=[R_lg, R_stat], writes=[R_lg])
        S.op("dve", TT(eg, eg, msk, ALU.mult), reads=[R_lg], writes=[R_lg])
        S.op("dve", (lambda e: e.reduce_sum(out=stat[:, 21:22], in_=eg, axis=mybir.AxisListType.X)), reads=[R_lg], writes=[R_stat])
        S.op("dve", RECIP(stat[:, 22:23], stat[:, 21:22]), reads=[R_stat], writes=[R_stat])
        S.op("dve", TS(gates[:, n, :], eg, stat[:, 22:23], None, ALU.mult), reads=[R_lg, R_stat], writes=[R_gates[n]])

    ones128 = A.alloc([128], BF16)
    ltri = A.alloc([128], BF16)
    R_cst = Reg()
    S.op("dve", MEMSET(ones128, 1.0), writes=[R_cst])
    S.dma("pool", ltri, ltri_in, writes=[R_cst])
    jb = A.alloc([NBLK], F32)
    kpi = A.alloc([2], F32)
    pio = A.alloc([1], F32)
    S.dma("sp", jb, jb512_in, writes=[R_cst])
    S.dma("sp", kpi, kp_iota_in, writes=[R_cst])
    S.dma("sp", pio, p_iota_in, writes=[R_cst])
    rank = A.alloc([NT, NE], F32)
    R_rank = Reg()
    for n in range(NT):
        b = bank()
        groups = [(PS[b][:, 0:NE], ones128, maskall[:, t, :], t == 0, False) for t in range(n)]
        groups.append((PS[b][:, 0:NE], ltri, maskall[:, n, :], n == 0, True))
        S.op("pe", MM(groups), reads=R_maskall[:n + 1] + [R_cst], writes=[PR[b]])
        S.op("act", ACOPY(rank[:, n, :], PS[b][:, 0:NE]), reads=[PR[b]], writes=[R_rank])
    cnt = A.alloc([NE], F32)
    b = bank()
    S.op("pe", MM([(PS[b][:, 0:NE], ones128, maskall[:, t, :], t == 0, t == NT - 1) for t in range(NT)]),
         reads=R_maskall + [R_cst], writes=[PR[b]])
    kint32 = A.alloc([NE], I32)
    padded = A.alloc([NE], F32)
    pend = A.alloc([NE], F32)
    pstart = A.alloc([NE], F32)
    ones32 = A.alloc([NE], F32)
    destm = A.alloc([NT, NE], F32)
    selt = A.alloc([NT, NE], F32)
    d8 = A.alloc([NT, 8], F32)
    cmpt = A.alloc([NBLK, NE], F32)
    ejf = A.alloc([NBLK], F32)
    R_m = Reg()
    S.op("dve", MEMSET(ones32, 1.0), writes=[R_m])
    S.op("dve", TS(kint32, PS[b][:, 0:NE], 1.0 / BR, (BR / 2 - 0.5) / BR, ALU.mult, ALU.add), reads=[PR[b]], writes=[R_m])
    S.op("dve", CP(cnt, kint32), reads=[R_m], writes=[R_m])
    S.op("dve", TS(padded, cnt, float(BR), None, ALU.mult), reads=[R_m], writes=[R_m])
    S.op("dve", SCAN(pend, ones32, padded, 0.0), reads=[R_m], writes=[R_m])
    S.op("dve", TT(pstart, pend, padded, ALU.subtract), reads=[R_m], writes=[R_m])
    pstart_bc = bass.AP(tensor=pstart.tensor, offset=pstart.offset, ap=[list(pstart.ap[0]), [0, NT], list(pstart.ap[1])])
    S.op("dve", TT(destm, rank, pstart_bc, ALU.add), reads=[R_m, R_rank], writes=[R_m])
    S.op("dve", STT(destm, destm, 1.0, maskall, ALU.add, ALU.mult), reads=[R_m] + R_maskall, writes=[R_m])
    S.op("dve", TS(destm, destm, -1.0, None, ALU.add), reads=[R_m], writes=[R_m])
    for n in range(NT):
        S.op("dve", (lambda e, n=n: e.max(out=d8[:, n, :], in_=destm[:, n, :])), reads=[R_m], writes=[R_m])
    S.op("dve", CP(dest4i, d8[:, :, 0:4]), reads=[R_m], writes=[R_meta])
    for k in range(4):
        S.op("dve", TT(selt, destm, bc_last(d8[:, :, k], NE), ALU.is_equal), reads=[R_m], writes=[R_m])
        S.op("dve", TT(selt, selt, gates, ALU.mult), reads=[R_m] + R_gates, writes=[R_m])
        S.op("dve", (lambda e, k=k: e.reduce_sum(out=gate4[:, :, k], in_=selt, axis=mybir.AxisListType.X)), reads=[R_m], writes=[R_meta])
    pend_bc = bass.AP(tensor=pend.tensor, offset=pend.offset, ap=[list(pend.ap[0]), [0, NBLK], list(pend.ap[1])])
    S.op("dve", TT(cmpt, pend_bc, bc_last(jb, NE), ALU.is_le), reads=[R_m, R_cst], writes=[R_m])
    S.op("dve", (lambda e: e.reduce_sum(out=ejf, in_=cmpt, axis=mybir.AxisListType.X)), reads=[R_m], writes=[R_m])
    S.op("dve", TS(ejf, ejf, float(NE - 1), None, ALU.min), reads=[R_m], writes=[R_m])
    kpi_bc = bass.AP(tensor=kpi.tensor, offset=kpi.offset, ap=[list(kpi.ap[0]), [0, NBLK], list(kpi.ap[1])])
    S.op("dve", STT(widx, bc_last(ejf, 2), 256.0, kpi_bc, ALU.mult, ALU.add), reads=[R_m, R_cst], writes=[R_meta])
    S.op("dve", STT(bidx, ejf, 128.0, pio[:, 0:1].to_broadcast([128, NBLK]), ALU.mult, ALU.add), reads=[R_m, R_cst], writes=[R_meta])
    S.op("dve", CP(eidx, ejf), reads=[R_m], writes=[R_meta])
    if stage == 5:
        dbg("gates", gates, [128, NT, NE], R_gates[15])
        dbg("dest4i", dest4i, [128, NT, 4], R_meta)
        dbg("gate4", gate4, [128, NT, 4], R_meta)
        dbg("eidx", eidx, [128, NBLK], R_meta)
        dbg("h2tm", h2tm, [128, NT, D], R_h2tm[15])
        S.finish(); S.emit(); return nc, dbg_outs
    bg_step(1000)
    for n in range(NT):
        for k in range(4):
            S.idma("pool", xs_d, bass.IndirectOffsetOnAxis(ap=dest4i[:, n, k:k + 1], axis=0), h2tm[:, n, :], None, NROWS - 1,
                   reads=[R_meta, R_h2tm[n]], writes=[])
    S.barrier()
    A.top = mark_core

    wgu = [A.alloc([8, 2 * D], BF16) for _ in range(2)]
    wdn = [A.alloc([8, D], BF16) for _ in range(1)]
    R_wgu = [[Reg() for _ in range(2)] for _ in range(2)]
    R_wdn = [[Reg()]]
    bgub = [A.alloc([16], F32) for _ in range(2)]
    bgub1 = [A.alloc([8], F32) for _ in range(2)]
    bdb = [A.alloc([D], F32) for _ in range(2)]
    R_bb = [Reg(), Reg()]
    R_bd = [Reg(), Reg()]
    xb = [A.alloc([NTB, D], BF16) for _ in range(2)]
    R_xb = [Reg(), Reg()]
    xT = [A.alloc([8, BR], BF16) for _ in range(2)]
    R_xT = [Reg(), Reg()]
    actT = A.alloc([8, BR], BF16)
    R_actT = Reg()
    ea = [A.alloc([BR], F32) for _ in range(2)]
    es_ = [A.alloc([BR], BF16) for _ in range(2)]
    eb = [A.alloc([BR], F32) for _ in range(2)]
    R_e = [Reg(), Reg()]
    ysb = [A.alloc([D], F32) for _ in range(2)]
    R_ysb = [Reg(), Reg()]
    ysct = 0

    def load_blk(j):
        bi = j % 2
        for kh in range(2):
            S.idma("pool", wgu[bi][:, kh * 4:(kh + 1) * 4, :].rearrange("p a b -> p (a b)"), None, wgu_bf,
                   bass.IndirectOffsetOnAxis(ap=widx[:, j, kh:kh + 1], axis=0), None, reads=[R_meta], writes=[R_wgu[bi][kh]])
        S.idma("pool", bgub[bi], None, bgu_rows_in, bass.IndirectOffsetOnAxis(ap=bidx[:, j:j + 1], axis=0), NE * 128 - 1,
               reads=[R_meta], writes=[R_bb[bi]])
        S.idma("pool", bdb[bi], None, b_down, bass.IndirectOffsetOnAxis(ap=eidx[:, j:j + 1], axis=0), NE - 1,
               reads=[R_meta], writes=[R_bd[bi]])
        S.op("dve", TS(bgub1[bi], bgub[bi][:, 8:16], 1.0, None, ALU.add), reads=[R_bb[bi]], writes=[R_bb[bi]])
        S.dma("sp", xb[bi], xs_d[j * BR:(j + 1) * BR, :].rearrange("(a p) c -> p a c", p=128), writes=[R_xb[bi]])

    def load_wdn(j):
        S.idma("pool", wdn[0].rearrange("p a b -> p (a b)"), None, wdn_bf, bass.IndirectOffsetOnAxis(ap=bidx[:, j:j + 1], axis=0), None,
               reads=[R_meta], writes=[R_wdn[0][0]])

    nblk_run = NBLK if stage >= 7 else 3
    load_blk(0)
    for j in range(nblk_run):
        bi = j % 2
        load_wdn(j)
        for a in range(NTB):
            b = bank()
            S.op("pe", TR([(PSB[b][:, k * 128:(k + 1) * 128], xb[bi][:, a, k * 128:(k + 1) * 128]) for k in range(8)], ident),
                 reads=[R_xb[bi], R_ident], writes=[PR[b]])
            S.op("act", ACOPY(xT[bi][:, :, a * 128:(a + 1) * 128], PSB[b].rearrange("p (k c) -> p k c", k=8)), reads=[PR[b]], writes=[R_xT[bi]])
        if j + 1 < nblk_run:
            load_blk(j + 1)
        for fb in range(8):
            bg, bl = bank(), bank()
            S.op("pe", MM([(PS[bg][:, :BR], wgu[bi][:, k, fb * 128:(fb + 1) * 128], xT[bi][:, k, :], k == 0, k == 7) for k in range(8)]),
                 reads=R_wgu[bi] + [R_xT[bi]], writes=[PR[bg]])
            S.op("pe", MM([(PS[bl][:, :BR], wgu[bi][:, k, D + fb * 128:D + (fb + 1) * 128], xT[bi][:, k, :], k == 0, k == 7) for k in range(8)]),
                 reads=R_wgu[bi] + [R_xT[bi]], writes=[PR[bl]])
            wi = fb % 2
            S.op("dve", TS(ea[wi], PS[bg][:, :BR], bgub[bi][:, fb:fb + 1], 7.0, ALU.add, ALU.min), reads=[PR[bg], R_bb[bi]], writes=[R_e[wi]])
            S.op("act", ACT(es_[wi], ea[wi], AF.Sigmoid, scale=1.702), reads=[R_e[wi]], writes=[R_e[wi]])
            S.op("dve", TS(eb[wi], PS[bl][:, :BR], bgub1[bi][:, fb:fb + 1], 8.0, ALU.add, ALU.min), reads=[PR[bl], R_bb[bi]], writes=[R_e[wi]])
            S.op("dve", STT(eb[wi], eb[wi], -6.0, ea[wi], ALU.max, ALU.mult), reads=[R_e[wi]], writes=[R_e[wi]])
            S.op("dve", TT(actT[:, fb, :], eb[wi], es_[wi], ALU.mult), reads=[R_e[wi]], writes=[R_actT])
        for a in range(NTB):
            yi = ysct % 2
            ysct += 1
            for half in range(2):
                hs = slice(half * 512, (half + 1) * 512)
                b = bank()
                S.op("pe", MM([(PS[b], actT[:, fb, a * 128:(a + 1) * 128], wdn[0][:, fb, hs], fb == 0, fb == 7) for fb in range(8)]),
                     reads=[R_actT] + R_wdn[0], writes=[PR[b]])
                S.op("dve", TT(ysb[yi][:, hs], PS[b], bdb[bi][:, hs], ALU.add), reads=[PR[b], R_bd[bi]], writes=[R_ysb[yi]])
            r0 = j * BR + a * 128
            S.dma("sp", ys_d[r0:r0 + 128, :], ysb[yi], reads=[R_ysb[yi]])
    S.barrier()
    A.top = mark_core

    yk = [A.alloc([D], F32) for _ in range(4)]
    R_yk = [Reg() for _ in range(4)]
    acc = [A.alloc([D], F32) for _ in range(2)]
    R_acc = [Reg(), Reg()]
    xf = [A.alloc([D], F32) for _ in range(2)]
    R_xf = [Reg(), Reg()]
    kc = 0
    for n in range(NT):
        nsl = slice(n * 128, (n + 1) * 128)
        ai = n % 2
        S.dma("sp", xf[ai], y_out[nsl, :], reads=[R_xmid[n]], writes=[R_xf[ai]])
        for k in range(4):
            ki = kc % 4
            kc += 1
            S.idma("pool", yk[ki], None, ys_d, bass.IndirectOffsetOnAxis(ap=dest4i[:, n, k:k + 1], axis=0), NROWS - 1,
                   reads=[R_meta], writes=[R_yk[ki]])
            if k == 0:
                S.op("dve", TS(acc[ai], yk[ki], gate4[:, n, k:k + 1], None, ALU.mult), reads=[R_yk[ki], R_meta], writes=[R_acc[ai]])
            else:
                S.op("dve", STT(acc[ai], yk[ki], gate4[:, n, k:k + 1], acc[ai], ALU.mult, ALU.add), reads=[R_yk[ki], R_meta, R_acc[ai]], writes=[R_acc[ai]])
        S.op("act", ACT(sqj, acc[ai], AF.Square, accum=stat[:, 24:25]), reads=[R_acc[ai]], writes=[R_sqj, R_stat])
        S.op("act", ACT(stat[:, 25:26], stat[:, 24:25], AF.Sqrt, bias=eps_col, scale=1.0 / D), reads=[R_stat, R_eps], writes=[R_stat])
        S.op("dve", RECIP(stat[:, 26:27], stat[:, 25:26]), reads=[R_stat], writes=[R_stat])
        S.op("dve", STT(acc[ai], acc[ai], stat[:, 26:27], gtg2, ALU.mult, ALU.mult), reads=[R_acc[ai], R_stat, R_mods[5]], writes=[R_acc[ai]])
        S.op("pool", TT(acc[ai], acc[ai], xf[ai], ALU.add), reads=[R_acc[ai], R_xf[ai]], writes=[R_acc[ai]])
        S.dma("sp", y_out[nsl, :], acc[ai], reads=[R_acc[ai]], writes=[R_xmid[n]])
    S.finish()
    S.emit()
    return nc, dbg_outs


def _prep_inputs(inp):
    f = lambda a: np.ascontiguousarray(np.asarray(a, dtype=np.float32))
    x = f(inp["x"]); c = f(inp["c"])
    L = 0
    w_in = f(inp["w_in"][L])
    q = w_in[:, 0:512].reshape(1024, 8, 64)
    qsw = np.concatenate([q[:, :, 32:], q[:, :, :32]], axis=2).reshape(1024, 512)
    k = w_in[:, 512:640].reshape(1024, 2, 64)
    ksw = np.concatenate([k[:, :, 32:], k[:, :, :32]], axis=2)
    kdup = np.concatenate([k[:, 0], k[:, 0], k[:, 1], k[:, 1]], axis=1)
    kswdup = np.concatenate([ksw[:, 0], ksw[:, 0], ksw[:, 1], ksw[:, 1]], axis=1)
    w_ext = f(np.concatenate([w_in[:, 0:512], qsw, kdup, kswdup, w_in[:, 640:768], w_in[:, 768:1280]], axis=1))
    assert w_ext.shape == (1024, WEXT)
    rep = lambda v: f(np.broadcast_to(np.asarray(v, np.float32).reshape(1, -1), (128, np.asarray(v).size)))
    col = lambda v, nk: f(np.asarray(v, np.float32).reshape(nk, 128).T)
    gvecs = f(np.stack([rep(inp["g_pre_mix"][L]), rep(inp["g_post_mix"][L]), rep(inp["g_pre_ffn"][L]), rep(inp["g_post_ffn"][L])]))
    a = np.arange(128)[:, None]; b = np.arange(128)[None, :]
    m_prev = np.where(a <= b, 0.0, -30000.0)
    m_next = np.where(b <= a, 0.0, -30000.0)
    mask_mid = f(np.concatenate([m_prev, np.zeros((128, 128)), m_next], axis=1))
    inv_freq = (np.float32(10000.0) ** (-np.arange(32, dtype=np.float32) * np.float32(2.0) / np.float32(64))).astype(np.float32)
    shared = dict(
        w_ada=f(inp["w_ada"][L]), b_ada_rep=rep(inp["b_ada"][L]), gvecs=gvecs, w_ext=w_ext,
        sink_rep=rep(inp["attn_sink"][L]), mask_mid=mask_mid, ident=f(np.eye(128)),
        iota1=rep(np.arange(1, CH + 1)), dskip_col=col(inp["ssm_d"][L], 4),
        w_glu=f(inp["w_glu"][L]), bglu_col=col(inp["b_glu"][L], 4),
        gcat_col=col(np.concatenate([inp["g_attn_out"][L], inp["g_ssm_out"][L]]), 8),
        w_out=f(inp["w_out"][L]), w_router=f(inp["w_router"][L]), b_router_rep=rep(inp["b_router"][L]),
        wgu_rows=f(np.asarray(inp["w_gate_up"][L], np.float32).reshape(32, 8, 128, 2048).transpose(0, 2, 1, 3).reshape(32 * 128 * 2, 4 * 2048)),
        wdn_rows=f(np.asarray(inp["w_down"][L], np.float32).reshape(32, 8, 128, 1024).transpose(0, 2, 1, 3).reshape(32 * 128, 8 * 1024)),
        b_down=f(inp["b_down"][L]),
        bgu_rows=f(np.asarray(inp["b_gate_up"][L], np.float32).reshape(32, 16, 128).transpose(0, 2, 1).reshape(32 * 128, 16)),
        ltri=f(np.triu(np.ones((128, 128)), 1)),
        jb512=rep(np.arange(54) * 384.0),
        kp_iota=f(np.arange(2)[None, :] * 1.0 + 2.0 * np.arange(128)[:, None]),
        p_iota=f(np.arange(128).reshape(128, 1)),
    )
    are, aim, ldt = f(inp["ssm_a_re"][L]), f(inp["ssm_a_im"][L]), f(inp["ssm_log_dt"][L])
    bre, bim, cre, cim = f(inp["ssm_b_re"][L]), f(inp["ssm_b_im"][L]), f(inp["ssm_c_re"][L]), f(inp["ssm_c_im"][L])
    ssm_by_flip = {}
    for flip in (0, 1):
        dd = [1, 0] if flip else [0, 1]
        sc = {}
        for nm, arr in (("a_re_p", are), ("a_im_p", aim)):
            v = arr[dd].reshape(2, 16, 2, 64)
            sc[nm] = f(v.transpose(2, 3, 0, 1).reshape(128, 32))
        v = np.broadcast_to(ldt[dd].reshape(2, 16, 2, 1), (2, 16, 2, 64))
        sc["logdt_p"] = f(v.transpose(2, 3, 0, 1).reshape(128, 32))
        for nm, arr, is_c in (("bpad_re", bre, False), ("bpad_im", bim, False), ("cpad_re", cre, True), ("cpad_im", cim, True)):
            out = np.zeros((2, 16, 128, 128), np.float32)
            for d in range(2):
                for G in range(16):
                    for g2 in range(2):
                        g = 2 * G + g2
                        blk = arr[dd[d], g].T if is_c else arr[dd[d], g]
                        c0 = (G % 4) * 32 + g2 * 16
                        out[d, G, g2 * 64:(g2 + 1) * 64, c0:c0 + 16] = blk
            sc[nm] = out.reshape(32, 128, 128)
        ssm_by_flip[flip] = sc
    in_maps = []
    for core in range(8):
        bidx, hf = core // 2, core % 2
        seq = x[bidx]
        pos = np.arange(4096, dtype=np.float32)
        if hf == 1:
            seq = seq[::-1]
            pos = pos[::-1]
        pos = pos[:2176]
        ang = (pos[:, None] * inv_freq[None, :]).astype(np.float32)
        cs, sn = np.cos(ang).astype(np.float32), np.sin(ang).astype(np.float32)
        cos64 = np.concatenate([cs, cs], axis=1).T
        sin64 = np.concatenate([-sn, sn], axis=1).T
        m = dict(shared)
        m.update(ssm_by_flip[hf])
        m["x_core"] = f(seq)
        m["c_col"] = col(c[bidx], 8)
        m["rope_cos"] = f(np.concatenate([cos64, cos64], axis=0))
        m["rope_sin"] = f(np.concatenate([sin64, sin64], axis=0))
        in_maps.append(m)
    return in_maps


_CACHE = {}


def kernel(**inputs):
    in_maps = _prep_inputs(inputs)
    if "nc" not in _CACHE:
        _CACHE["nc"] = build()[0]
    res = run_bass_kernel_spmd(_CACHE["nc"], in_maps, core_ids=list(range(8)))
    out = np.zeros((4, 4096, 1024), np.float32)
    for core in range(8):
        bidx, hf = core // 2, core % 2
        y = np.asarray(res.results[core]["y_out"], np.float32)
        if hf == 0:
            out[bidx, :2048] = y
        else:
            out[bidx, 2048:] = y[::-1]
    return out
```

```python
import math
import os
import numpy as np
import concourse.bass as bass
import concourse.mybir as mybir
from concourse.bass_utils import run_bass_kernel_spmd
from contextlib import ExitStack

F32 = mybir.dt.float32
BF16 = mybir.dt.bfloat16
U8 = mybir.dt.uint8
I32 = mybir.dt.int32
ALU = mybir.AluOpType
AF = mybir.ActivationFunctionType
PI = math.pi

D = 1024
T_OWN = 2048
NT = 16
NE = 32
WEXT = 2176
QO, QSO, KO, KSO, VO, UO = 0, 512, 1024, 1280, 1536, 1664
CH = 512


class Reg:
    __slots__ = ("lw", "rd", "name")

    def __init__(self, name=""):
        self.lw = None
        self.rd = {}
        self.name = name


class Sched:
    CE = ("pe", "act", "dve", "pool")

    def __init__(self, nc, es, nslots=8):
        self.nc = nc
        self.sem, self.cnt, self.prog, self.waited = {}, {}, {}, {}
        for e in ("pe", "act", "dve", "pool", "sp"):
            self.prog[e] = []
            self.waited[e] = {}
        for e in self.CE:
            self.sem[e] = es.enter_context(nc.semaphore("sem_" + e))
            self.cnt[e] = 0
        self.ns = nslots
        self.dcount, self.dnext = {}, {}
        for q in ("sp", "pool", "bg"):
            self.dnext[q] = 0
            for s in range(nslots):
                k = ("d", q, s)
                self.sem[k] = es.enter_context(nc.semaphore("sd_%s_%d" % (q, s)))
                self.dcount[k] = 0

    def _deps(self, eng, reads, writes, skip_same=True):
        need = {}
        raw_same = 0

        def add(tok):
            if tok is None:
                return
            k, v = tok
            if need.get(k, 0) < v:
                need[k] = v

        for r in reads:
            add(r.lw)
            if r.lw is not None and r.lw[0] == eng and r.lw[1] > raw_same:
                raw_same = r.lw[1]
        for w in writes:
            add(w.lw)
            if w.lw is not None and w.lw[0] == eng and w.lw[1] > raw_same:
                raw_same = w.lw[1]
            if w.rd.get(eng, 0) > raw_same:
                raw_same = w.rd[eng]
            for k, v in w.rd.items():
                add((k, v))
        waits = []
        for k, v in need.items():
            if skip_same and k == eng:
                if eng == "pe" or raw_same == 0:
                    continue
                v = raw_same
            if self.waited[eng].get(k, 0) >= v:
                continue
            self.waited[eng][k] = v
            waits.append((k, v))
        return waits

    def op(self, eng, fn, reads=(), writes=()):
        waits = self._deps(eng, reads, writes)
        self.cnt[eng] += 1
        tok = (eng, self.cnt[eng])
        self.prog[eng].append((waits, fn, self.sem[eng], 1))
        for r in reads:
            if r.rd.get(eng, 0) < tok[1]:
                r.rd[eng] = tok[1]
        for w in writes:
            w.lw = tok
            w.rd = {}
        return tok

    def dma(self, q, out_ap, in_ap, reads=(), writes=(), ring=None):
        waits = self._deps(q, reads, writes, skip_same=False)
        ring = ring or q
        s = self.dnext[ring] % self.ns
        self.dnext[ring] += 1
        k = ("d", ring, s)
        c = self.dcount[k]
        if c > 0 and self.waited[q].get(k, 0) < 16 * c:
            self.waited[q][k] = 16 * c
            waits.append((k, 16 * c))
        self.dcount[k] = c + 1
        tok = (k, 16 * (c + 1))
        self.prog[q].append((waits, (lambda e: e.dma_start(out=out_ap, in_=in_ap)), self.sem[k], 16))
        for r in reads:
            if r.rd.get(k, 0) < tok[1]:
                r.rd[k] = tok[1]
        for w in writes:
            w.lw = tok
            w.rd = {}
        return tok

    def idma(self, q, out_ap, out_off, in_ap, in_off, bounds, reads=(), writes=()):
        waits = self._deps(q, reads, writes, skip_same=False)
        s = self.dnext[q] % self.ns
        self.dnext[q] += 1
        k = ("d", q, s)
        c = self.dcount[k]
        if c > 0 and self.waited[q].get(k, 0) < 16 * c:
            self.waited[q][k] = 16 * c
            waits.append((k, 16 * c))
        self.dcount[k] = c + 1
        tok = (k, 16 * (c + 1))
        self.prog[q].append((waits, (lambda e: e.indirect_dma_start(out=out_ap, out_offset=out_off, in_=in_ap, in_offset=in_off,
                                                                    bounds_check=None)), self.sem[k], 16))
        for r in reads:
            if r.rd.get(k, 0) < tok[1]:
                r.rd[k] = tok[1]
        for w in writes:
            w.lw = tok
            w.rd = {}
        return tok

    def barrier(self):
        cur = {e: self.cnt[e] for e in self.CE}
        for k, c in self.dcount.items():
            cur[k] = 16 * c
        for e in ("pe", "act", "dve", "pool", "sp"):
            waits = []
            for k, v in cur.items():
                if k == e or v == 0:
                    continue
                if self.waited[e].get(k, 0) >= v:
                    continue
                self.waited[e][k] = v
                waits.append((k, v))
            self.prog[e].append((waits, None, None, 0))

    def finish(self):
        waits = []
        for k, c in self.dcount.items():
            if c > 0:
                waits.append((k, 16 * c))
        self.prog["sp"].append((waits, None, None, 0))

    def emit(self):
        nc = self.nc

        def run(name):
            def f(e):
                for waits, fn, sem, inc in self.prog[name]:
                    for k, v in waits:
                        e.wait_ge(self.sem[k], v)
                    if fn is not None:
                        ins = fn(e)
                        ins.then_inc(sem, inc)
            return f

        with nc.Block() as block:
            block.tensor(run("pe"))
            block.scalar(run("act"))
            block.vector(run("dve"))
            block.gpsimd(run("pool"))
            block.sync(run("sp"))


DTS = {F32: 4, BF16: 2, U8: 1, I32: 4}


class Arena:
    def __init__(self, nc, nbytes):
        self.t = nc.alloc_sbuf_tensor("arena", [128, nbytes], U8)
        self.top = 0
        self.n = nbytes
        self.peak = 0
        self.hi = nbytes

    def alloc_top(self, free_shape, dt):
        size = int(np.prod(free_shape)) * DTS[dt]
        off = (self.hi - size) // 32 * 32
        self.hi = off
        ap = self.t[:, off:off + size].bitcast(dt)
        if len(free_shape) == 2:
            ap = ap.rearrange("p (a b) -> p a b", a=free_shape[0])
        return ap

    def alloc(self, free_shape, dt):
        size = int(np.prod(free_shape)) * DTS[dt]
        off = (self.top + 31) // 32 * 32
        self.top = off + size
        self.peak = max(self.peak, self.top)
        assert self.top <= self.hi, ("SBUF arena overflow", self.top, self.hi)
        ap = self.t[:, off:off + size].bitcast(dt)
        if len(free_shape) == 2:
            ap = ap.rearrange("p (a b) -> p a b", a=free_shape[0])
        elif len(free_shape) == 3:
            ap = ap.rearrange("p (a b c) -> p a b c", a=free_shape[0], b=free_shape[1])
        elif len(free_shape) == 4:
            ap = ap.rearrange("p (a b c d) -> p a b c d", a=free_shape[0], b=free_shape[1], c=free_shape[2])
        return ap


def bc_last(ap, m):
    return bass.AP(tensor=ap.tensor, offset=ap.offset, ap=[list(x) for x in ap.ap] + [[0, m]])


def rev(ap):
    dims = [list(x) for x in ap.ap]
    st, n = dims[-1]
    dims[-1] = [-st, n]
    return bass.AP(tensor=ap.tensor, offset=ap.offset + st * (n - 1), ap=dims)


def build(stage=99):
    nc = bass.Bass("TRN2", target_bir_lowering=False)
    es = ExitStack()

    def din(name, shape, dt=F32):
        return nc.dram_tensor(name, list(shape), dt, kind="ExternalInput").ap()

    x_core = din("x_core", [4096, D])
    c_col = din("c_col", [128, 8])
    w_ada = din("w_ada", [D, 6 * D])
    b_ada_rep = din("b_ada_rep", [128, 6 * D])
    gvecs = din("gvecs", [4, 128, D])
    w_ext = din("w_ext", [D, WEXT])
    rope_cos = din("rope_cos", [128, 2176])
    rope_sin = din("rope_sin", [128, 2176])
    sink_rep = din("sink_rep", [128, 8])
    mask_mid = din("mask_mid", [128, 384])
    ident_in = din("ident", [128, 128])
    iota1 = din("iota1", [128, CH])
    a_re_p = din("a_re_p", [128, 32])
    a_im_p = din("a_im_p", [128, 32])
    logdt_p = din("logdt_p", [128, 32])
    bpad_re = din("bpad_re", [32, 128, 128])
    bpad_im = din("bpad_im", [32, 128, 128])
    cpad_re = din("cpad_re", [32, 128, 128])
    cpad_im = din("cpad_im", [32, 128, 128])
    dskip_col = din("dskip_col", [128, 4])
    w_glu = din("w_glu", [512, 512])
    bglu_col = din("bglu_col", [128, 4])
    gcat_col = din("gcat_col", [128, 8])
    w_out = din("w_out", [D, D])
    w_router = din("w_router", [D, NE])
    b_router_rep = din("b_router_rep", [128, NE])
    wgu_rows = din("wgu_rows", [NE * 128 * 2, 4 * 2 * D])
    bgu_rows_in = din("bgu_rows", [NE * 128, 16])
    ltri_in = din("ltri", [128, 128])
    jb512_in = din("jb512", [128, 54])
    kp_iota_in = din("kp_iota", [128, 2])
    p_iota_in = din("p_iota", [128, 1])
    wdn_rows = din("wdn_rows", [NE * 128, 8 * D])
    b_down = din("b_down", [NE, D])
    y_out = nc.dram_tensor("y_out", [T_OWN, D], F32, kind="ExternalOutput").ap()
    dbg_outs = {}

    S = Sched(nc, es, nslots=12)
    A = Arena(nc, 204000)
    pst = [nc.alloc_psum_tensor("ps%d" % i, [128, 512], F32) for i in range(8)]
    PS = [t[:, :] for t in pst]
    PSB = [t[:, :].bitcast(BF16) for t in pst]
    PR = [Reg("ps%d" % i) for i in range(8)]
    bank_ctr = [0]

    def bank():
        i = bank_ctr[0] % 8
        bank_ctr[0] += 1
        return i

    def dbg(name, ap, shape, reg):
        t = nc.dram_tensor("dbg_" + name, list(shape), ap.dtype, kind="ExternalOutput").ap()
        dbg_outs[name] = t
        S.dma("sp", t, ap, reads=[reg])

    def ACT(out, in_, func, bias=None, scale=None, accum=None):
        kw = {}
        if bias is not None:
            kw["bias"] = bias
        if scale is not None:
            kw["scale"] = scale
        if accum is not None:
            kw["accum_out"] = accum
        return lambda e: e.activation(out=out, in_=in_, func=func, **kw)

    def TS(out, in0, s1, s2, op0, op1=None, accum=None):
        kw = {}
        if op1 is not None:
            kw["op1"] = op1
        if accum is not None:
            kw["accum_out"] = accum
        return lambda e: e.tensor_scalar(out=out, in0=in0, scalar1=s1, scalar2=s2, op0=op0, **kw)

    def TT(out, in0, in1, op):
        return lambda e: e.tensor_tensor(out=out, in0=in0, in1=in1, op=op)

    def STT(out, in0, sc, in1, op0, op1, accum=None):
        kw = {}
        if accum is not None:
            kw["accum_out"] = accum
        return lambda e: e.scalar_tensor_tensor(out=out, in0=in0, scalar=sc, in1=in1, op0=op0, op1=op1, **kw)

    def CP(out, in_):
        return lambda e: e.tensor_copy(out=out, in_=in_)

    def MM(groups):
        def f(e):
            ins = None
            for (o, l, r, st, sp_) in groups:
                ins = e.matmul(o, lhsT=l, rhs=r, start=st, stop=sp_)
            return ins
        return f

    def TR(pairs, ident):
        def f(e):
            ins = None
            for (o, i) in pairs:
                ins = e.transpose(o, i, ident)
            return ins
        return f


    def ACOPY(out, in_):
        return lambda e: e.copy(out=out, in_=in_)

    def AMUL(out, in_, m):
        return lambda e: e.mul(out=out, in_=in_, mul=m)

    def RECIP(out, in_):
        return lambda e: e.reciprocal(out=out, in_=in_)

    def MEMSET(ap, v):
        return lambda e: e.memset(ap, v)

    def SCAN(out, d0, d1, init):
        return lambda e: e.tensor_tensor_scan(out=out, data0=d0, data1=d1, initial=init, op0=ALU.mult, op1=ALU.add)

    ident = A.alloc([128], BF16)
    R_ident = Reg()
    S.dma("pool", ident, ident_in, writes=[R_ident])
    ones_col = A.alloc([16], BF16)
    R_ones = Reg()
    S.op("dve", MEMSET(ones_col, 1.0), writes=[R_ones])

    eps_col = A.alloc([1], F32)
    R_eps = Reg()
    S.op("dve", MEMSET(eps_col, 1e-6), writes=[R_eps])
    negpi = A.alloc([1], F32)
    S.op("dve", MEMSET(negpi, -PI), writes=[R_eps])
    stat = A.alloc([64], F32)
    R_stat = Reg()
    sqj = A.alloc([D], BF16)
    R_sqj = Reg()
    gtg2 = A.alloc([D], F32)
    mark_core = A.top
    mods5 = A.alloc([5, D], F32)
    R_mods = [Reg("mods%d" % i) for i in range(6)]

    def modsl(slot):
        return gtg2 if slot == 5 else mods5[:, slot, :]

    def modsl_s(slot, sl):
        return gtg2[:, sl] if slot == 5 else mods5[:, slot, sl]
    SEGMAP = {0: 1, 1: 0, 2: 2, 3: 4, 4: 3, 5: 5}
    mark_persist = A.top

    ccol = A.alloc([8], F32)
    R_cc = Reg()
    S.dma("sp", ccol, c_col, writes=[R_cc])
    csil = A.alloc([8], F32)
    R_cs = Reg()
    S.op("act", ACT(csil, ccol, AF.Silu), reads=[R_cc], writes=[R_cs])
    cl = A.alloc([8, 128], BF16)
    R_cl = Reg()
    for k in range(8):
        S.op("dve", CP(cl[:, k, :], csil[:, k:k + 1].to_broadcast([128, 128])), reads=[R_cs], writes=[R_cl])
    wada_buf = [A.alloc([8, 512], BF16) for _ in range(2)]
    R_wada = [Reg(), Reg()]
    bada_buf = [A.alloc([512], F32) for _ in range(2)]
    R_bada = [Reg(), Reg()]
    w_ada_v = w_ada.rearrange("(k p) n -> p k n", p=128)
    for j in range(12):
        bi = j % 2
        S.dma("pool", wada_buf[bi], w_ada_v[:, :, j * 512:(j + 1) * 512], writes=[R_wada[bi]])
        S.dma("sp", bada_buf[bi], b_ada_rep[:, j * 512:(j + 1) * 512], writes=[R_bada[bi]])
        b = bank()
        S.op("pe", MM([(PS[b], cl[:, k, :], wada_buf[bi][:, k, :], k == 0, k == 7) for k in range(8)]),
             reads=[R_cl, R_wada[bi]], writes=[PR[b]])
        slot = SEGMAP[j // 2]
        S.op("dve", TT(modsl_s(slot, slice((j % 2) * 512, (j % 2) * 512 + 512)), PS[b], bada_buf[bi], ALU.add),
             reads=[PR[b], R_bada[bi]], writes=[R_mods[slot]])
    gtmp = A.alloc([D], F32)
    R_gt = Reg()
    for (gi, slot, plus1) in ((0, 0, True), (1, 2, False), (2, 3, True), (3, 5, False)):
        S.dma("sp", gtmp, gvecs[gi], writes=[R_gt])
        if plus1:
            S.op("dve", STT(modsl(slot), modsl(slot), 1.0, gtmp, ALU.add, ALU.mult),
                 reads=[R_gt, R_mods[slot]], writes=[R_mods[slot]])
        else:
            S.op("dve", TT(modsl(slot), modsl(slot), gtmp, ALU.mult),
                 reads=[R_gt, R_mods[slot]], writes=[R_mods[slot]])
    if stage == 0:
        dbg("mods", mods5, [128, 5, D], R_mods[0])
        dbg("gtg2", gtg2, [128, D], R_mods[5])
        S.finish(); S.emit(); return nc, dbg_outs
    S.barrier()
    A.top = mark_persist

    wgu_bf = nc.dram_tensor("wgu_bf", [NE * 256, 8192], BF16, kind="Internal").ap()
    wdn_bf = nc.dram_tensor("wdn_bf", [NE * 128, 8192], BF16, kind="Internal").ap()
    bg_list = []
    for e_ in range(NE):
        bg_list.append((wgu_bf[e_ * 256:e_ * 256 + 128, :], wgu_rows[e_ * 256:e_ * 256 + 128, :]))
        bg_list.append((wgu_bf[e_ * 256 + 128:e_ * 256 + 256, :], wgu_rows[e_ * 256 + 128:e_ * 256 + 256, :]))
        bg_list.append((wdn_bf[e_ * 128:(e_ + 1) * 128, :], wdn_rows[e_ * 128:(e_ + 1) * 128, :]))
    bg_pos = [0]

    def bg_step(n=1):
        for _ in range(n):
            if bg_pos[0] < len(bg_list):
                o_, i_ = bg_list[bg_pos[0]]
                bg_pos[0] += 1
                S.dma("pool", o_, i_, ring="bg")

    uT = A.alloc([4, 4096], BF16)
    R_u = [Reg() for _ in range(8)]
    attnT = A.alloc([4, T_OWN], BF16)
    R_attnT = [Reg() for _ in range(NT)]
    rattn = A.alloc([NT], F32)
    R_rattn = Reg()
    mark_B = A.top
    qT = A.alloc([4, T_OWN], BF16)
    kT = A.alloc([2, 2176], BF16)
    vaug = A.alloc([17, 2, 65], BF16)
    maskb = A.alloc([384], BF16)
    R_mask = Reg()
    S.dma("pool", maskb, mask_mid, writes=[R_mask])
    R_q = [Reg() for _ in range(4)]
    R_k = [Reg() for _ in range(5)]
    R_v = [Reg() for _ in range(17)]
    S.op("pool", MEMSET(vaug, 1.0), writes=R_v)
    mark_C = A.top

    W = {}

    def alloc_norm_bufs():
        W["xt"] = [A.alloc([D], F32) for _ in range(2)]
        W["R_xt"] = [Reg(), Reg()]
        W["t1"] = A.alloc([D], F32)
        W["R_t1"] = Reg()
        W["hb"] = [A.alloc([D], BF16) for _ in range(2)]
        W["R_hb"] = [Reg(), Reg()]

    alloc_norm_bufs()
    wext = A.alloc([8, WEXT], BF16)
    R_wext = Reg()
    w_ext_v = w_ext.rearrange("(k p) n -> p k n", p=128)
    for k in range(0, 8, 2):
        S.dma("pool", wext[:, k:k + 2, :], w_ext_v[:, k:k + 2, :], writes=[R_wext])
    rcos = A.alloc([2176], BF16)
    rsin = A.alloc([2176], BF16)
    R_rope = Reg()
    S.dma("pool", rcos, rope_cos, writes=[R_rope])
    S.dma("pool", rsin, rope_sin, writes=[R_rope])
    hT = [A.alloc([8, 512], BF16) for _ in range(2)]
    R_hT = [Reg(), Reg()]
    ropet = [A.alloc([512], F32) for _ in range(4)]
    R_ropet = [Reg() for _ in range(4)]

    def prenorm_tile(src_ap, xbuf, R_x, gslot, shslot, dst_ap, R_dst, tcnt, load=True):
        bi = tcnt % 2
        t1, R_t1 = W["t1"], W["R_t1"]
        hb, R_hb = W["hb"][bi], W["R_hb"][bi]
        if load:
            S.dma("sp", xbuf, src_ap, writes=[R_x])
        S.op("act", ACT(sqj, xbuf, AF.Square, accum=stat[:, 0:1]), reads=[R_x], writes=[R_sqj, R_stat])
        S.op("act", ACT(stat[:, 1:2], stat[:, 0:1], AF.Sqrt, bias=eps_col, scale=1.0 / D), reads=[R_stat, R_eps], writes=[R_stat])
        S.op("dve", RECIP(stat[:, 2:3], stat[:, 1:2]), reads=[R_stat], writes=[R_stat])
        S.op("dve", STT(t1, xbuf, stat[:, 2:3], modsl(gslot), ALU.mult, ALU.mult),
             reads=[R_x, R_stat, R_mods[gslot]], writes=[R_t1])
        S.op("pool", TT(hb, t1, modsl(shslot), ALU.add), reads=[R_t1, R_mods[shslot]], writes=[R_hb])
        b = bank()
        S.op("pe", TR([(PSB[b][:, k * 128:(k + 1) * 128], hb[:, k * 128:(k + 1) * 128]) for k in range(8)], ident),
             reads=[R_hb, R_ident], writes=[PR[b]])
        S.op("act", ACOPY(dst_ap, PSB[b].rearrange("p (k c) -> p k c", k=8)), reads=[PR[b]], writes=[R_dst])

    def proj_fm(colblk_off, hTb, R_h, ntok_):
        b = bank()
        S.op("pe", MM([(PS[b][:, :ntok_], wext[:, k, colblk_off:colblk_off + 128], hTb[:, k, :ntok_], k == 0, k == 7)
                       for k in range(8)]), reads=[R_wext, R_h], writes=[PR[b]])
        return b

    tcnt = 0
    nblk = 8 if stage >= 3 else 5
    for blk in range(nblk):
        own = blk < 4
        ntile = 4 if (own or stage >= 3) else 1
        hb_i = blk % 2
        for tl in range(ntile):
            tile_i = blk * 4 + tl
            xi = tcnt % 2
            prenorm_tile(x_core[tile_i * 128:(tile_i + 1) * 128, :], W["xt"][xi], W["R_xt"][xi], 0, 1,
                         hT[hb_i][:, :, tl * 128:(tl + 1) * 128], R_hT[hb_i], tcnt)
            tcnt += 1
            bg_step(1)
        ntok = ntile * 128
        tok0 = blk * 512
        if own or blk == 4:
            kt = 512 if own else 128
            plist = []
            if own:
                plist += [("q", cb, QO + cb * 128, QSO + cb * 128) for cb in range(4)]
            plist += [("k", kb, KO + kb * 128, KSO + kb * 128) for kb in range(2)]
            for pi, (kind, cb, o1, o2) in enumerate(plist):
                ba = proj_fm(o1, hT[hb_i], R_hT[hb_i], kt)
                bb = proj_fm(o2, hT[hb_i], R_hT[hb_i], kt)
                ra, rb = ropet[(2 * pi) % 4], ropet[(2 * pi + 1) % 4]
                Ra, Rb = R_ropet[(2 * pi) % 4], R_ropet[(2 * pi + 1) % 4]
                S.op("dve", TT(ra[:, :kt], PS[ba][:, :kt], rcos[:, tok0:tok0 + kt], ALU.mult), reads=[PR[ba], R_rope], writes=[Ra])
                S.op("dve", TT(rb[:, :kt], PS[bb][:, :kt], rsin[:, tok0:tok0 + kt], ALU.mult), reads=[PR[bb], R_rope], writes=[Rb])
                if kind == "q":
                    S.op("pool", TT(qT[:, cb, tok0:tok0 + kt], ra[:, :kt], rb[:, :kt], ALU.add), reads=[Ra, Rb], writes=[R_q[blk]])
                else:
                    S.op("pool", TT(kT[:, cb, tok0:tok0 + kt], ra[:, :kt], rb[:, :kt], ALU.add), reads=[Ra, Rb], writes=[R_k[blk]])
            for tl in range(4 if own else 1):
                tile_i = blk * 4 + tl
                b = bank()
                S.op("pe", MM([(PS[b][:, :128], hT[hb_i][:, k, tl * 128:(tl + 1) * 128], wext[:, k, VO:VO + 128], k == 0, k == 7)
                               for k in range(8)]), reads=[R_wext, R_hT[hb_i]], writes=[PR[b]])
                S.op("act", ACOPY(vaug[:, tile_i, :, 0:64], PS[b][:, :128].rearrange("p (h c) -> p h c", h=2)),
                     reads=[PR[b]], writes=[R_v[tile_i]])
        for cb in range(4):
            b = proj_fm(UO + cb * 128, hT[hb_i], R_hT[hb_i], ntok)
            S.op("act", ACOPY(uT[:, cb, tok0:tok0 + ntok], PS[b][:, :ntok]), reads=[PR[b]], writes=[R_u[blk]])
    if stage == 1:
        dbg("qT", qT, [128, 4, T_OWN], R_q[3])
        dbg("kT", kT, [128, 2, 2176], R_k[4])
        dbg("vaug", vaug, [128, 17, 2, 65], R_v[16])
        dbg("uT", uT, [128, 4, 4096], R_u[4])
        S.finish(); S.emit(); return nc, dbg_outs
    S.barrier()
    A.top = mark_C

    esink = A.alloc([8], F32)
    R_es = Reg()
    S.dma("sp", esink, sink_rep, writes=[R_es])
    S.op("act", ACT(esink, esink, AF.Exp), reads=[R_es], writes=[R_es])
    pT = A.alloc([4, 8, 384], BF16)
    R_pT = [[Reg() for _ in range(8)] for _ in range(4)]
    attn_o = [A.alloc([512], BF16) for _ in range(2)]
    R_ao = [Reg(), Reg()]
    den = A.alloc([16], F32)
    R_den = Reg()

    def kq_range(j):
        return max(j - 1, 0), min(j + 2, 16)

    def pv_block(n):
        bo = [bank(), bank()]
        ai = n % 2
        for hh in range(2):
            groups, regs = [], []
            for h4 in range(4):
                h = hh * 4 + h4
                kvh = h // 4
                js = [j for j in (n - 1, n, n + 1) if 0 <= j <= 16]
                for ji, j in enumerate(js):
                    qb0, _ = kq_range(j)
                    c0 = (n - qb0) * 128
                    groups.append((PS[bo[hh]][:, h4 * 65:(h4 + 1) * 65], pT[:, j % 4, h, c0:c0 + 128], vaug[:, j, kvh, :],
                                   ji == 0, ji == len(js) - 1))
                    regs.append(R_pT[j % 4][h])
                    regs.append(R_v[j])
            S.op("pe", MM(groups), reads=regs, writes=[PR[bo[hh]]])
        for hh in range(2):
            pv = PS[bo[hh]][:, 0:260].rearrange("p (h c) -> p h c", h=4)
            S.op("dve", TT(den[:, hh * 4:hh * 4 + 4], pv[:, :, 64], esink[:, hh * 4:hh * 4 + 4], ALU.add),
                 reads=[PR[bo[hh]], R_es], writes=[R_den])
        S.op("dve", RECIP(den[:, 8:16], den[:, 0:8]), reads=[R_den], writes=[R_den])
        for hh in range(2):
            pv = PS[bo[hh]][:, 0:260].rearrange("p (h c) -> p h c", h=4)
            S.op("dve", TT(attn_o[ai][:, hh * 256:(hh + 1) * 256].rearrange("p (h c) -> p h c", h=4), pv[:, :, 0:64],
                           bc_last(den[:, 8 + hh * 4:8 + hh * 4 + 4], 64), ALU.mult),
                 reads=[PR[bo[hh]], R_den], writes=[R_ao[ai]])
        S.op("act", ACT(sqj[:, 0:512], attn_o[ai], AF.Square, accum=stat[:, 8:9]), reads=[R_ao[ai]], writes=[R_sqj, R_stat])
        S.op("act", ACT(stat[:, 9:10], stat[:, 8:9], AF.Sqrt, bias=eps_col, scale=1.0 / 512), reads=[R_stat, R_eps], writes=[R_stat])
        S.op("dve", RECIP(rattn[:, n:n + 1], stat[:, 9:10]), reads=[R_stat], writes=[R_rattn])
        b = bank()
        S.op("pe", TR([(PSB[b][:, k * 128:(k + 1) * 128], attn_o[ai][:, k * 128:(k + 1) * 128]) for k in range(4)], ident),
             reads=[R_ao[ai], R_ident], writes=[PR[b]])
        S.op("act", ACOPY(attnT[:, :, n * 128:(n + 1) * 128], PSB[b][:, 0:512].rearrange("p (k c) -> p k c", k=4)),
             reads=[PR[b]], writes=[R_attnT[n]])

    for j in range(17):
        qb0, qb1 = kq_range(j)
        ncol = (qb1 - qb0) * 128
        moff = 128 if j == 0 else 0
        for h in range(8):
            kvh, qblk, pr = h // 4, h // 2, (h % 2) * 64
            b = bank()
            S.op("pe", MM([(PS[b][:, :ncol], kT[pr:pr + 64, kvh, j * 128:(j + 1) * 128], qT[pr:pr + 64, qblk, qb0 * 128:qb1 * 128], True, False),
                           (PS[b][:, :ncol], ident, maskb[:, moff:moff + ncol], False, True)]),
                 reads=[R_k[min(j // 4, 4)], R_q[0], R_q[1], R_q[2], R_q[3], R_ident, R_mask], writes=[PR[b]])
            S.op("act", ACT(pT[:, j % 4, h, :ncol], PS[b][:, :ncol], AF.Exp, scale=0.125), reads=[PR[b]], writes=[R_pT[j % 4][h]])
        if j >= 1:
            pv_block(j - 1)
        bg_step(1)
    if stage == 2:
        dbg("attnT", attnT, [128, 4, T_OWN], R_attnT[15])
        dbg("rattn", rattn, [128, NT], R_rattn)
        S.finish(); S.emit(); return nc, dbg_outs
    S.barrier()
    A.top = mark_B

    y2T = A.alloc([4, T_OWN], BF16)
    R_y2T = [Reg() for _ in range(4)]
    rssm = A.alloc([NT], F32)
    R_rssm = Reg()
    mark_D = A.top
    are = A.alloc([32], F32); aim = A.alloc([32], F32); ldt = A.alloc([32], F32)
    R_sc = Reg()
    S.dma("sp", are, a_re_p, writes=[R_sc])
    S.dma("sp", aim, a_im_p, writes=[R_sc])
    S.dma("sp", ldt, logdt_p, writes=[R_sc])
    dtv = A.alloc([32], F32); lr = A.alloc([32], F32); li = A.alloc([32], F32); rmag = A.alloc([32], F32)
    tmpa = A.alloc([32], F32); tmpb = A.alloc([32], F32); cosl = A.alloc([32], F32); sinl = A.alloc([32], F32)
    fr = A.alloc([32], F32); fi = A.alloc([32], F32); dnm = A.alloc([32], F32)
    dsk = A.alloc([4], F32)
    kis = A.alloc([32], I32)
    S.dma("sp", dsk, dskip_col, writes=[R_sc])
    seq = [
        ("act", ACT(dtv, ldt, AF.Exp)),
        ("dve", TT(lr, are, dtv, ALU.mult)),
        ("dve", TT(li, aim, dtv, ALU.mult)),
        ("act", ACT(rmag, lr, AF.Exp)),
        ("dve", TS(kis, li, 1.0 / (2 * PI), None, ALU.mult)),
        ("dve", CP(tmpa, kis)),
        ("dve", STT(tmpa, tmpa, -2 * PI, li, ALU.mult, ALU.add)),
        ("dve", TS(tmpa, tmpa, 3.1415925, -3.1415925, ALU.min, ALU.max)),
        ("act", ACT(sinl, tmpa, AF.Sin)),
        ("dve", TS(tmpb, li, PI / 2, None, ALU.add)),
        ("dve", TS(kis, tmpb, 1.0 / (2 * PI), None, ALU.mult)),
        ("dve", CP(tmpa, kis)),
        ("dve", STT(tmpa, tmpa, -2 * PI, tmpb, ALU.mult, ALU.add)),
        ("dve", TS(tmpa, tmpa, 3.1415925, -3.1415925, ALU.min, ALU.max)),
        ("act", ACT(cosl, tmpa, AF.Sin)),
        ("dve", TT(cosl, cosl, rmag, ALU.mult)),
        ("dve", TT(sinl, sinl, rmag, ALU.mult)),
        ("dve", TS(cosl, cosl, -1.0, None, ALU.add)),
        ("dve", TT(dnm, are, are, ALU.mult)),
        ("dve", TT(tmpa, aim, aim, ALU.mult)),
        ("dve", TT(dnm, dnm, tmpa, ALU.add)),
        ("dve", RECIP(dnm, dnm)),
        ("dve", TT(tmpa, cosl, are, ALU.mult)),
        ("dve", TT(tmpb, sinl, aim, ALU.mult)),
        ("dve", TT(fr, tmpa, tmpb, ALU.add)),
        ("dve", TT(fr, fr, dnm, ALU.mult)),
        ("dve", TT(tmpa, sinl, are, ALU.mult)),
        ("dve", TT(tmpb, cosl, aim, ALU.mult)),
        ("dve", TT(fi, tmpa, tmpb, ALU.subtract)),
        ("dve", TT(fi, fi, dnm, ALU.mult)),
    ]
    for eng, fn in seq:
        S.op(eng, fn, reads=[R_sc, R_eps], writes=[R_sc])
    iot = A.alloc([CH], F32)
    R_iot = Reg()
    S.dma("sp", iot, iota1, writes=[R_iot])

    NG = 4
    wl = [[A.alloc([128], BF16) for _ in range(2)] for _ in range(NG)]
    cm = [[A.alloc([128], BF16) for _ in range(2)] for _ in range(NG)]
    tabC = [A.alloc([CH], F32) for _ in range(NG)]
    tabS = [A.alloc([CH], F32) for _ in range(NG)]
    R_gc = [Reg() for _ in range(NG)]
    ldb = [A.alloc([128], F32) for _ in range(4)]
    R_ldb = [Reg() for _ in range(4)]
    bbf = [A.alloc([128], BF16) for _ in range(2)]
    R_bbf = [Reg(), Reg()]
    NW = 4
    mt = [[A.alloc([CH], F32) for _ in range(4)] for _ in range(NW)]
    qt = [[A.alloc([CH], BF16) for _ in range(2)] for _ in range(NW)]
    R_mt = [[Reg() for _ in range(4)] for _ in range(NW)]
    R_qt = [Reg() for _ in range(NW)]
    mtmp = mt
    R_w = [R_mt[0][0], R_mt[1][0]]
    argt, argk = mt[3][0], mt[3][1]
    kint = mt[3][2].bitcast(I32)
    R_argt = R_mt[3][0]
    R_argk = R_mt[3][1]
    R_kint = R_mt[3][2]
    sro = [[A.alloc([CH], BF16) for _ in range(2)] for _ in range(NG)]
    R_sro = [Reg() for _ in range(NG)]
    sprev = [A.alloc([2], F32) for _ in range(NG)]
    R_sprev = [Reg() for _ in range(NG)]
    cs5 = [A.alloc([4], F32) for _ in range(NG)]
    ltmp = [A.alloc([2], F32) for _ in range(NW)]
    ysum = A.alloc([T_OWN], F32)
    R_ys = [Reg() for _ in range(4)]
    gact = A.alloc([4, T_OWN], BF16)
    R_gact = [Reg() for _ in range(4)]
    wcount = [0]

    for cb in range(4):
        for d in range(2):
            for gs in range(NG):
                G = cb * 4 + gs
                dg = d * 16 + G
                S.dma("sp", ldb[0], bpad_re[dg], writes=[R_ldb[0]])
                S.dma("sp", ldb[1], bpad_im[dg], writes=[R_ldb[1]])
                S.dma("sp", ldb[2], cpad_re[dg], writes=[R_ldb[2]])
                S.dma("sp", ldb[3], cpad_im[dg], writes=[R_ldb[3]])
                frc, fic = fr[:, dg:dg + 1], fi[:, dg:dg + 1]
                ta, tb_ = mt[0][0][:, :128], mt[0][1][:, :128]
                S.op("dve", TS(ta, ldb[1], fic, None, ALU.mult), reads=[R_ldb[1], R_sc], writes=[R_mt[0][0]])
                S.op("dve", STT(bbf[0], ldb[0], frc, ta, ALU.mult, ALU.subtract), reads=[R_ldb[0], R_sc, R_mt[0][0]], writes=[R_bbf[0]])
                S.op("dve", TS(tb_, ldb[0], fic, None, ALU.mult), reads=[R_ldb[0], R_sc], writes=[R_mt[0][1]])
                S.op("dve", STT(bbf[1], ldb[1], frc, tb_, ALU.mult, ALU.add), reads=[R_ldb[1], R_sc, R_mt[0][1]], writes=[R_bbf[1]])
                b = bank()
                S.op("pe", TR([(PSB[b][:, 0:128], bbf[0]), (PSB[b][:, 128:256], bbf[1])], ident),
                     reads=[R_bbf[0], R_bbf[1], R_ident], writes=[PR[b]])
                S.op("act", ACOPY(wl[gs][0], PSB[b][:, 0:128]), reads=[PR[b]], writes=[R_gc[gs]])
                S.op("act", ACOPY(wl[gs][1], PSB[b][:, 128:256]), reads=[PR[b]], writes=[R_gc[gs]])
                S.op("act", ACOPY(cm[gs][0], ldb[2]), reads=[R_ldb[2]], writes=[R_gc[gs]])
                S.op("act", AMUL(cm[gs][1], ldb[3], -1.0), reads=[R_ldb[3]], writes=[R_gc[gs]])
                lic = li[:, dg:dg + 1]
                S.op("dve", TS(argt, iot, lic, None, ALU.mult), reads=[R_iot, R_sc], writes=[R_argt])
                for (tab, shift) in ((tabS[gs], False), (tabC[gs], True)):
                    if shift:
                        S.op("dve", TS(argt, argt, PI / 2, None, ALU.add), reads=[R_argt], writes=[R_argt])
                    S.op("dve", TS(kint, argt, 1.0 / (2 * PI), None, ALU.mult), reads=[R_argt], writes=[R_kint])
                    S.op("dve", CP(argk, kint), reads=[R_kint], writes=[R_argk])
                    S.op("dve", STT(argk, argk, -2 * PI, argt, ALU.mult, ALU.add), reads=[R_argt, R_argk], writes=[R_argk])
                    S.op("dve", TS(argk, argk, 3.1415925, -3.1415925, ALU.min, ALU.max), reads=[R_argk], writes=[R_argk])
                    S.op("act", ACT(tab, argk, AF.Sin), reads=[R_argk], writes=[R_gc[gs]])
                S.op("dve", CP(cs5[gs][:, 0:1], tabC[gs][:, CH - 1:CH]), reads=[R_gc[gs]], writes=[R_sprev[gs]])
                S.op("dve", CP(cs5[gs][:, 1:2], tabS[gs][:, CH - 1:CH]), reads=[R_gc[gs]], writes=[R_sprev[gs]])
                S.op("dve", TS(cs5[gs][:, 2:3], tabS[gs][:, CH - 1:CH], -1.0, None, ALU.mult), reads=[R_gc[gs]], writes=[R_sprev[gs]])
                S.op("dve", CP(cs5[gs][:, 3:4], tabC[gs][:, CH - 1:CH]), reads=[R_gc[gs]], writes=[R_sprev[gs]])
                S.op("dve", MEMSET(sprev[gs], 0.0), writes=[R_sprev[gs]])
            chunks = [0, 1, 2, 3] if d == 0 else [7, 6, 5, 4, 3, 2, 1, 0]
            for c in chunks:
                is_own = c < 4
                t0 = c * CH
                for gs in range(NG):
                    G = cb * 4 + gs
                    dg = d * 16 + G
                    wi = wcount[0] % NW
                    wcount[0] += 1
                    if wcount[0] % 3 == 0:
                        bg_step(1)
                    m, Rm = mt[wi], R_mt[wi]
                    ba, bb = bank(), bank()
                    S.op("pe", MM([(PS[ba], wl[gs][0], uT[:, cb, t0:t0 + CH], True, True)]), reads=[R_gc[gs], R_u[c]], writes=[PR[ba]])
                    S.op("pe", MM([(PS[bb], wl[gs][1], uT[:, cb, t0:t0 + CH], True, True)]), reads=[R_gc[gs], R_u[c]], writes=[PR[bb]])
                    tC = tabC[gs] if d == 0 else rev(tabC[gs])
                    tS = tabS[gs] if d == 0 else rev(tabS[gs])
                    S.op("dve", TT(m[0], PS[ba], tC, ALU.mult), reads=[PR[ba], R_gc[gs]], writes=[Rm[0]])
                    S.op("dve", TT(m[1], PS[bb], tS, ALU.mult), reads=[PR[bb], R_gc[gs]], writes=[Rm[1]])
                    S.op("dve", TT(m[2], PS[bb], tC, ALU.mult), reads=[PR[bb], R_gc[gs]], writes=[Rm[2]])
                    S.op("dve", TT(m[3], PS[ba], tS, ALU.mult), reads=[PR[ba], R_gc[gs]], writes=[Rm[3]])
                    S.op("pool", TT(m[0], m[0], m[1], ALU.add), reads=[Rm[0], Rm[1]], writes=[Rm[0]])
                    S.op("pool", TT(m[2], m[2], m[3], ALU.subtract), reads=[Rm[2], Rm[3]], writes=[Rm[2]])
                    rcol = rmag[:, dg:dg + 1]
                    rb = bass.AP(tensor=rcol.tensor, offset=rcol.offset, ap=[list(rcol.ap[0]), [0, CH]])
                    for ri, (src, dst) in enumerate(((0, 1), (2, 3))):
                        o = m[dst] if d == 0 else rev(m[dst])
                        i1 = m[src] if d == 0 else rev(m[src])
                        S.op("dve", SCAN(o, rb, i1, sprev[gs][:, ri:ri + 1]),
                             reads=[Rm[src], R_sprev[gs], R_sc], writes=[Rm[dst]])
                    lc = CH - 1 if d == 0 else 0
                    sr_l, si_l = m[1][:, lc:lc + 1], m[3][:, lc:lc + 1]
                    S.op("dve", TS(ltmp[wi], cs5[gs][:, 2:4], si_l, None, ALU.mult), reads=[Rm[3], R_sprev[gs]], writes=[R_qt[wi]])
                    S.op("dve", STT(sprev[gs], cs5[gs][:, 0:2], sr_l, ltmp[wi], ALU.mult, ALU.add), reads=[Rm[1], R_qt[wi], R_sprev[gs]], writes=[R_sprev[gs]])
                    if is_own:
                        S.op("dve", TT(m[0], m[1], tC, ALU.mult), reads=[Rm[1], R_gc[gs]], writes=[Rm[0]])
                        S.op("dve", TT(m[2], m[3], tS, ALU.mult), reads=[Rm[3], R_gc[gs]], writes=[Rm[2]])
                        S.op("dve", TT(qt[wi][0], m[1], tS, ALU.mult), reads=[Rm[1], R_gc[gs]], writes=[R_qt[wi]])
                        S.op("dve", TT(qt[wi][1], m[3], tC, ALU.mult), reads=[Rm[3], R_gc[gs]], writes=[R_qt[wi]])
                        S.op("dve", TT(sro[gs][0], m[0], m[2], ALU.subtract), reads=[Rm[0], Rm[2]], writes=[R_sro[gs]])
                        S.op("dve", TT(sro[gs][1], qt[wi][0], qt[wi][1], ALU.add), reads=[R_qt[wi]], writes=[R_sro[gs]])
                if is_own:
                    b = bank()
                    groups = []
                    for gs in range(NG):
                        groups.append((PS[b], cm[gs][0], sro[gs][0], gs == 0, False))
                        groups.append((PS[b], cm[gs][1], sro[gs][1], False, gs == NG - 1))
                    S.op("pe", MM(groups), reads=R_gc + R_sro, writes=[PR[b]])
                    ys = ysum[:, t0:t0 + CH]
                    if d == 0:
                        S.op("dve", STT(ys, uT[:, cb, t0:t0 + CH], dsk[:, cb:cb + 1], PS[b], ALU.mult, ALU.add),
                             reads=[PR[b], R_u[c], R_sc], writes=[R_ys[c]])
                    else:
                        S.op("dve", TT(ys, ys, PS[b], ALU.add), reads=[PR[b], R_ys[c]], writes=[R_ys[c]])
        for c in range(4):
            ys = ysum[:, c * CH:(c + 1) * CH]
            g1, g2 = mt[c % 2][0], mt[c % 2][1]
            Rg1, Rg2 = R_mt[c % 2][0], R_mt[c % 2][1]
            S.op("act", ACT(g1, ys, AF.Square), reads=[R_ys[c]], writes=[Rg1])
            S.op("dve", TS(g1, g1, 0.044715, 1.0, ALU.mult, ALU.add), reads=[Rg1], writes=[Rg1])
            S.op("dve", TT(g1, g1, ys, ALU.mult), reads=[Rg1, R_ys[c]], writes=[Rg1])
            S.op("act", ACT(g2, g1, AF.Sigmoid, scale=1.5957691216057308), reads=[Rg1], writes=[Rg2])
            S.op("pool", TT(gact[:, cb, c * CH:(c + 1) * CH], ys, g2, ALU.mult), reads=[Rg2, R_ys[c]], writes=[R_gact[cb]])
    if stage == 3:
        dbg("gact", gact, [128, 4, T_OWN], R_gact[3])
        S.finish(); S.emit(); return nc, dbg_outs

    wglu = A.alloc([4, 512], BF16)
    R_wglu = Reg()
    S.dma("pool", wglu, w_glu.rearrange("(k p) n -> p k n", p=128), writes=[R_wglu])
    bglu = A.alloc([4], F32)
    S.dma("sp", bglu, bglu_col, writes=[R_wglu])
    ysq = [A.alloc([512], BF16) for _ in range(4)]
    R_ysq = [Reg() for _ in range(4)]
    sgt = [mt[2][0], mt[2][1]]
    R_sgt = [R_mt[2][0], R_mt[2][1]]
    ci = 0
    for tb in range(4):
        tsl = slice(tb * 512, (tb + 1) * 512)
        for cbo in range(4):
            b = bank()
            S.op("pe", MM([(PS[b], wglu[:, k, cbo * 128:(cbo + 1) * 128], gact[:, k, tsl], k == 0, k == 3) for k in range(4)]),
                 reads=[R_wglu] + R_gact, writes=[PR[b]])
            si = ci % 2
            ci += 1
            S.op("act", ACT(sgt[si], PS[b], AF.Sigmoid, bias=bglu[:, cbo:cbo + 1]), reads=[PR[b], R_wglu], writes=[R_sgt[si]])
            S.op("dve", TT(y2T[:, cbo, tsl], gact[:, cbo, tsl], sgt[si], ALU.mult),
                 reads=[R_sgt[si], R_gact[cbo]], writes=[R_y2T[cbo]])
            S.op("pool", TT(ysq[cbo], y2T[:, cbo, tsl], y2T[:, cbo, tsl], ALU.mult),
                 reads=[R_y2T[cbo]], writes=[R_ysq[cbo]])
        bss = bank()
        groups = []
        for tl in range(4):
            for cbo in range(4):
                groups.append((PS[bss][:, tl * 16:(tl + 1) * 16], ysq[cbo][:, tl * 128:(tl + 1) * 128], ones_col, cbo == 0, cbo == 3))
        S.op("pe", MM(groups), reads=R_ysq + [R_ones], writes=[PR[bss]])
        S.op("act", ACT(stat[:, 10:14], PS[bss][:, 0:64].rearrange("p (t c) -> p t c", c=16)[:, :, 0], AF.Sqrt, bias=eps_col, scale=1.0 / 512), reads=[PR[bss], R_eps], writes=[R_stat])
        S.op("dve", RECIP(rssm[:, tb * 4:tb * 4 + 4], stat[:, 10:14]), reads=[R_stat], writes=[R_rssm])
    if stage == 4:
        dbg("y2T", y2T, [128, 4, T_OWN], R_y2T[3])
        dbg("rssm", rssm, [128, NT], R_rssm)
        S.finish(); S.emit(); return nc, dbg_outs
    S.barrier()
    A.top = mark_D

    BR = 384
    NBLK = 54
    NTB = BR // 128
    NROWS = NBLK * BR
    xs_d = nc.dram_tensor("xs_scr", [NROWS, D], BF16, kind="Internal").ap()
    ys_d = nc.dram_tensor("ys_scr", [NROWS, D], F32, kind="Internal").ap()
    gates = A.alloc_top([NT, NE], F32)
    R_gates = [Reg() for _ in range(NT)]
    maskall = A.alloc_top([NT, NE], BF16)
    R_maskall = [Reg() for _ in range(NT)]
    dest4i = A.alloc_top([NT, 4], I32)
    gate4 = A.alloc_top([NT, 4], F32)
    widx = A.alloc_top([NBLK, 2], I32)
    bidx = A.alloc_top([NBLK], I32)
    eidx = A.alloc_top([NBLK], I32)
    R_meta = Reg()
    mark_top_meta = A.hi
    alloc_norm_bufs()
    h2tm = A.alloc([NT, D], BF16)
    R_h2tm = [Reg() for _ in range(NT)]
    h2Tt = [A.alloc([8, 128], BF16) for _ in range(2)]
    R_h2Tt = [Reg(), Reg()]
    wout = A.alloc([8, D], BF16)
    R_wout = Reg()
    w_out_v = w_out.rearrange("(k p) n -> p k n", p=128)
    S.dma("pool", wout[:, 0:4, :], w_out_v[:, 0:4, :], writes=[R_wout])
    S.dma("pool", wout[:, 4:8, :], w_out_v[:, 4:8, :], writes=[R_wout])
    gcat = A.alloc([8], F32)
    S.dma("sp", gcat, gcat_col, writes=[R_wout])
    for k in range(8):
        S.op("dve", TS(wout[:, k, :], wout[:, k, :], gcat[:, k:k + 1], None, ALU.mult), reads=[R_wout], writes=[R_wout])
    wr = A.alloc([8, NE], BF16)
    R_wr = Reg()
    S.dma("pool", wr, w_router.rearrange("(k p) n -> p k n", p=128), writes=[R_wr])
    brt = A.alloc([NE], F32)
    S.dma("sp", brt, b_router_rep, writes=[R_wr])
    ym = [A.alloc([D], F32) for _ in range(2)]
    R_ym = [Reg(), Reg()]
    xn = [A.alloc([D], F32) for _ in range(2)]
    R_xn = [Reg(), Reg()]
    lg = A.alloc([NE], F32)
    top8 = A.alloc([8], F32)
    eg = A.alloc([NE], F32)
    msk = A.alloc([NE], F32)
    R_lg = Reg()
    R_xmid = [Reg() for _ in range(NT)]

    def prenorm2_tile(xbuf, R_x, n):
        t1, R_t1 = W["t1"], W["R_t1"]
        S.op("act", ACT(sqj, xbuf, AF.Square, accum=stat[:, 0:1]), reads=[R_x], writes=[R_sqj, R_stat])
        S.op("act", ACT(stat[:, 1:2], stat[:, 0:1], AF.Sqrt, bias=eps_col, scale=1.0 / D), reads=[R_stat, R_eps], writes=[R_stat])
        S.op("dve", RECIP(stat[:, 2:3], stat[:, 1:2]), reads=[R_stat], writes=[R_stat])
        S.op("dve", STT(t1, xbuf, stat[:, 2:3], modsl(3), ALU.mult, ALU.mult), reads=[R_x, R_stat, R_mods[3]], writes=[R_t1])
        S.op("pool", TT(h2tm[:, n, :], t1, modsl(4), ALU.add), reads=[R_t1, R_mods[4]], writes=[R_h2tm[n]])
        b = bank()
        S.op("pe", TR([(PSB[b][:, k * 128:(k + 1) * 128], h2tm[:, n, k * 128:(k + 1) * 128]) for k in range(8)], ident),
             reads=[R_h2tm[n], R_ident], writes=[PR[b]])
        S.op("act", ACOPY(h2Tt[n % 2], PSB[b].rearrange("p (k c) -> p k c", k=8)), reads=[PR[b]], writes=[R_h2Tt[n % 2]])

    for n in range(NT):
        i2 = n % 2
        nsl = slice(n * 128, (n + 1) * 128)
        bA = [bank(), bank()]
        bS = [bank(), bank()]
        for half in range(2):
            hs = slice(half * 512, (half + 1) * 512)
            S.op("pe", MM([(PS[bA[half]], attnT[:, k, nsl], wout[:, k, hs], k == 0, k == 3) for k in range(4)]),
                 reads=[R_attnT[n], R_wout], writes=[PR[bA[half]]])
            S.op("pe", MM([(PS[bS[half]], y2T[:, k, nsl], wout[:, 4 + k, hs], k == 0, k == 3) for k in range(4)]),
                 reads=R_y2T + [R_wout], writes=[PR[bS[half]]])
        for half in range(2):
            hs = slice(half * 512, (half + 1) * 512)
            S.op("dve", TS(ym[i2][:, hs], PS[bA[half]], rattn[:, n:n + 1], None, ALU.mult), reads=[PR[bA[half]], R_rattn], writes=[R_ym[i2]])
            S.op("dve", STT(ym[i2][:, hs], PS[bS[half]], rssm[:, n:n + 1], ym[i2][:, hs], ALU.mult, ALU.add),
                 reads=[PR[bS[half]], R_rssm, R_ym[i2]], writes=[R_ym[i2]])
        xi = n % 2
        xtb, R_xtb = W["xt"][xi], W["R_xt"][xi]
        S.dma("sp", xtb, x_core[nsl, :], writes=[R_xtb])
        S.op("act", ACT(sqj, ym[i2], AF.Square, accum=stat[:, 16:17]), reads=[R_ym[i2]], writes=[R_sqj, R_stat])
        S.op("act", ACT(stat[:, 17:18], stat[:, 16:17], AF.Sqrt, bias=eps_col, scale=1.0 / D), reads=[R_stat, R_eps], writes=[R_stat])
        S.op("dve", RECIP(stat[:, 18:19], stat[:, 17:18]), reads=[R_stat], writes=[R_stat])
        S.op("dve", STT(ym[i2], ym[i2], stat[:, 18:19], modsl(2), ALU.mult, ALU.mult), reads=[R_ym[i2], R_stat, R_mods[2]], writes=[R_ym[i2]])
        S.op("pool", TT(xn[i2], ym[i2], xtb, ALU.add), reads=[R_ym[i2], R_xtb], writes=[R_xn[i2]])
        S.dma("sp", y_out[nsl, :], xn[i2], reads=[R_xn[i2]], writes=[R_xmid[n]])
        prenorm2_tile(xn[i2], R_xn[i2], n)
        b = bank()
        S.op("pe", MM([(PS[b][:, 0:NE], h2Tt[n % 2][:, k, :], wr[:, k, :], k == 0, k == 7) for k in range(8)]),
             reads=[R_h2Tt[n % 2], R_wr], writes=[PR[b]])
        S.op("dve", TT(lg, PS[b][:, 0:NE], brt, ALU.add), reads=[PR[b], R_wr], writes=[R_lg])
        S.op("dve", (lambda e: e.max(out=top8, in_=lg)), reads=[R_lg], writes=[R_lg])
        S.op("dve", TS(msk, lg, top8[:, 3:4], None, ALU.is_ge), reads=[R_lg], writes=[R_lg])
        S.op("dve", CP(maskall[:, n, :], msk), reads=[R_lg], writes=[R_maskall[n]])
        S.op("dve", TS(stat[:, 20:21], top8[:, 0:1], -1.0, None, ALU.mult), reads=[R_lg], writes=[R_stat])
        S.op("act", ACT(eg, lg, AF.Exp, bias=stat[:, 20:21]), reads=[R_lg, R_stat], writes=[R_lg])
        S.op("dve", TT(eg, eg, msk, ALU.mult), reads=[R_lg], writes=[R_lg])
        S.op("dve", (lambda e: e.reduce_sum(out=stat[:, 21:22], in_=eg, axis=mybir.AxisListType.X)), reads=[R_lg], writes=[R_stat])
        S.op("dve", RECIP(stat[:, 22:23], stat[:, 21:22]), reads=[R_stat], writes=[R_stat])
        S.op("dve", TS(gates[:, n, :], eg, stat[:, 22:23], None, ALU.mult), reads=[R_lg, R_stat], writes=[R_gates[n]])

    ones128 = A.alloc([128], BF16)
    ltri = A.alloc([128], BF16)
    R_cst = Reg()
    S.op("dve", MEMSET(ones128, 1.0), writes=[R_cst])
    S.dma("pool", ltri, ltri_in, writes=[R_cst])
    jb = A.alloc([NBLK], F32)
    kpi = A.alloc([2], F32)
    pio = A.alloc([1], F32)
    S.dma("sp", jb, jb512_in, writes=[R_cst])
    S.dma("sp", kpi, kp_iota_in, writes=[R_cst])
    S.dma("sp", pio, p_iota_in, writes=[R_cst])
    rank = A.alloc([NT, NE], F32)
    R_rank = Reg()
    for n in range(NT):
        b = bank()
        groups = [(PS[b][:, 0:NE], ones128, maskall[:, t, :], t == 0, False) for t in range(n)]
        groups.append((PS[b][:, 0:NE], ltri, maskall[:, n, :], n == 0, True))
        S.op("pe", MM(groups), reads=R_maskall[:n + 1] + [R_cst], writes=[PR[b]])
        S.op("act", ACOPY(rank[:, n, :], PS[b][:, 0:NE]), reads=[PR[b]], writes=[R_rank])
    cnt = A.alloc([NE], F32)
    b = bank()
    S.op("pe", MM([(PS[b][:, 0:NE], ones128, maskall[:, t, :], t == 0, t == NT - 1) for t in range(NT)]),
         reads=R_maskall + [R_cst], writes=[PR[b]])
    kint32 = A.alloc([NE], I32)
    padded = A.alloc([NE], F32)
    pend = A.alloc([NE], F32)
    pstart = A.alloc([NE], F32)
    ones32 = A.alloc([NE], F32)
    destm = A.alloc([NT, NE], F32)
    selt = A.alloc([NT, NE], F32)
    d8 = A.alloc([NT, 8], F32)
    cmpt = A.alloc([NBLK, NE], F32)
    ejf = A.alloc([NBLK], F32)
    R_m = Reg()
    S.op("dve", MEMSET(ones32, 1.0), writes=[R_m])
    S.op("dve", TS(kint32, PS[b][:, 0:NE], 1.0 / BR, (BR / 2 - 0.5) / BR, ALU.mult, ALU.add), reads=[PR[b]], writes=[R_m])
    S.op("dve", CP(cnt, kint32), reads=[R_m], writes=[R_m])
    S.op("dve", TS(padded, cnt, float(BR), None, ALU.mult), reads=[R_m], writes=[R_m])
    S.op("dve", SCAN(pend, ones32, padded, 0.0), reads=[R_m], writes=[R_m])
    S.op("dve", TT(pstart, pend, padded, ALU.subtract), reads=[R_m], writes=[R_m])
    pstart_bc = bass.AP(tensor=pstart.tensor, offset=pstart.offset, ap=[list(pstart.ap[0]), [0, NT], list(pstart.ap[1])])
    S.op("dve", TT(destm, rank, pstart_bc, ALU.add), reads=[R_m, R_rank], writes=[R_m])
    S.op("dve", STT(destm, destm, 1.0, maskall, ALU.add, ALU.mult), reads=[R_m] + R_maskall, writes=[R_m])
    S.op("dve", TS(destm, destm, -1.0, None, ALU.add), reads=[R_m], writes=[R_m])
    for n in range(NT):
        S.op("dve", (lambda e, n=n: e.max(out=d8[:, n, :], in_=destm[:, n, :])), reads=[R_m], writes=[R_m])
    S.op("dve", CP(dest4i, d8[:, :, 0:4]), reads=[R_m], writes=[R_meta])
    for k in range(4):
        S.op("dve", TT(selt, destm, bc_last(d8[:, :, k], NE), ALU.is_equal), reads=[R_m], writes=[R_m])
        S.op("dve", TT(selt, selt, gates, ALU.mult), reads=[R_m] + R_gates, writes=[R_m])
        S.op("dve", (lambda e, k=k: e.reduce_sum(out=gate4[:, :, k], in_=selt, axis=mybir.AxisListType.X)), reads=[R_m], writes=[R_meta])
    pend_bc = bass.AP(tensor=pend.tensor, offset=pend.offset, ap=[list(pend.ap[0]), [0, NBLK], list(pend.ap[1])])
    S.op("dve", TT(cmpt, pend_bc, bc_last(jb, NE), ALU.is_le), reads=[R_m, R_cst], writes=[R_m])
    S.op("dve", (lambda e: e.reduce_sum(out=ejf, in_=cmpt, axis=mybir.AxisListType.X)), reads=[R_m], writes=[R_m])
    S.op("dve", TS(ejf, ejf, float(NE - 1), None, ALU.min), reads=[R_m], writes=[R_m])
    kpi_bc = bass.AP(tensor=kpi.tensor, offset=kpi.offset, ap=[list(kpi.ap[0]), [0, NBLK], list(kpi.ap[1])])
    S.op("dve", STT(widx, bc_last(ejf, 2), 256.0, kpi_bc, ALU.mult, ALU.add), reads=[R_m, R_cst], writes=[R_meta])
    S.op("dve", STT(bidx, ejf, 128.0, pio[:, 0:1].to_broadcast([128, NBLK]), ALU.mult, ALU.add), reads=[R_m, R_cst], writes=[R_meta])
    S.op("dve", CP(eidx, ejf), reads=[R_m], writes=[R_meta])
    if stage == 5:
        dbg("gates", gates, [128, NT, NE], R_gates[15])
        dbg("dest4i", dest4i, [128, NT, 4], R_meta)
        dbg("gate4", gate4, [128, NT, 4], R_meta)
        dbg("eidx", eidx, [128, NBLK], R_meta)
        dbg("h2tm", h2tm, [128, NT, D], R_h2tm[15])
        S.finish(); S.emit(); return nc, dbg_outs
    bg_step(1000)
    for n in range(NT):
        for k in range(4):
            S.idma("pool", xs_d, bass.IndirectOffsetOnAxis(ap=dest4i[:, n, k:k + 1], axis=0), h2tm[:, n, :], None, NROWS - 1,
                   reads=[R_meta, R_h2tm[n]], writes=[])
    S.barrier()
    A.top = mark_core

    wgu = [A.alloc([8, 2 * D], BF16) for _ in range(2)]
    wdn = [A.alloc([8, D], BF16) for _ in range(1)]
    R_wgu = [[Reg() for _ in range(2)] for _ in range(2)]
    R_wdn = [[Reg()]]
    bgub = [A.alloc([16], F32) for _ in range(2)]
    bgub1 = [A.alloc([8], F32) for _ in range(2)]
    bdb = [A.alloc([D], F32) for _ in range(2)]
    R_bb = [Reg(), Reg()]
    R_bd = [Reg(), Reg()]
    xb = [A.alloc([NTB, D], BF16) for _ in range(2)]
    R_xb = [Reg(), Reg()]
    xT = [A.alloc([8, BR], BF16) for _ in range(2)]
    R_xT = [Reg(), Reg()]
    actT = A.alloc([8, BR], BF16)
    R_actT = Reg()
    ea = [A.alloc([BR], F32) for _ in range(2)]
    es_ = [A.alloc([BR], BF16) for _ in range(2)]
    eb = [A.alloc([BR], F32) for _ in range(2)]
    R_e = [Reg(), Reg()]
    ysb = [A.alloc([D], F32) for _ in range(2)]
    R_ysb = [Reg(), Reg()]
    ysct = 0

    def load_blk(j):
        bi = j % 2
        for kh in range(2):
            S.idma("pool", wgu[bi][:, kh * 4:(kh + 1) * 4, :].rearrange("p a b -> p (a b)"), None, wgu_bf,
                   bass.IndirectOffsetOnAxis(ap=widx[:, j, kh:kh + 1], axis=0), None, reads=[R_meta], writes=[R_wgu[bi][kh]])
        S.idma("pool", bgub[bi], None, bgu_rows_in, bass.IndirectOffsetOnAxis(ap=bidx[:, j:j + 1], axis=0), NE * 128 - 1,
               reads=[R_meta], writes=[R_bb[bi]])
        S.idma("pool", bdb[bi], None, b_down, bass.IndirectOffsetOnAxis(ap=eidx[:, j:j + 1], axis=0), NE - 1,
               reads=[R_meta], writes=[R_bd[bi]])
        S.op("dve", TS(bgub1[bi], bgub[bi][:, 8:16], 1.0, None, ALU.add), reads=[R_bb[bi]], writes=[R_bb[bi]])
        S.dma("sp", xb[bi], xs_d[j * BR:(j + 1) * BR, :].rearrange("(a p) c -> p a c", p=128), writes=[R_xb[bi]])

    def load_wdn(j):
        S.idma("pool", wdn[0].rearrange("p a b -> p (a b)"), None, wdn_bf, bass.IndirectOffsetOnAxis(ap=bidx[:, j:j + 1], axis=0), None,
               reads=[R_meta], writes=[R_wdn[0][0]])

    nblk_run = NBLK if stage >= 7 else 3
    load_blk(0)
    for j in range(nblk_run):
        bi = j % 2
        load_wdn(j)
        for a in range(NTB):
            b = bank()
            S.op("pe", TR([(PSB[b][:, k * 128:(k + 1) * 128], xb[bi][:, a, k * 128:(k + 1) * 128]) for k in range(8)], ident),
                 reads=[R_xb[bi], R_ident], writes=[PR[b]])
            S.op("act", ACOPY(xT[bi][:, :, a * 128:(a + 1) * 128], PSB[b].rearrange("p (k c) -> p k c", k=8)), reads=[PR[b]], writes=[R_xT[bi]])
        if j + 1 < nblk_run:
            load_blk(j + 1)
        for fb in range(8):
            bg, bl = bank(), bank()
            S.op("pe", MM([(PS[bg][:, :BR], wgu[bi][:, k, fb * 128:(fb + 1) * 128], xT[bi][:, k, :], k == 0, k == 7) for k in range(8)]),
                 reads=R_wgu[bi] + [R_xT[bi]], writes=[PR[bg]])
            S.op("pe", MM([(PS[bl][:, :BR], wgu[bi][:, k, D + fb * 128:D + (fb + 1) * 128], xT[bi][:, k, :], k == 0, k == 7) for k in range(8)]),
                 reads=R_wgu[bi] + [R_xT[bi]], writes=[PR[bl]])
            wi = fb % 2
            S.op("dve", TS(ea[wi], PS[bg][:, :BR], bgub[bi][:, fb:fb + 1], 7.0, ALU.add, ALU.min), reads=[PR[bg], R_bb[bi]], writes=[R_e[wi]])
            S.op("act", ACT(es_[wi], ea[wi], AF.Sigmoid, scale=1.702), reads=[R_e[wi]], writes=[R_e[wi]])
            S.op("dve", TS(eb[wi], PS[bl][:, :BR], bgub1[bi][:, fb:fb + 1], 8.0, ALU.add, ALU.min), reads=[PR[bl], R_bb[bi]], writes=[R_e[wi]])
            S.op("dve", STT(eb[wi], eb[wi], -6.0, ea[wi], ALU.max, ALU.mult), reads=[R_e[wi]], writes=[R_e[wi]])
            S.op("dve", TT(actT[:, fb, :], eb[wi], es_[wi], ALU.mult), reads=[R_e[wi]], writes=[R_actT])
        for a in range(NTB):
            yi = ysct % 2
            ysct += 1
            for half in range(2):
                hs = slice(half * 512, (half + 1) * 512)
                b = bank()
                S.op("pe", MM([(PS[b], actT[:, fb, a * 128:(a + 1) * 128], wdn[0][:, fb, hs], fb == 0, fb == 7) for fb in range(8)]),
                     reads=[R_actT] + R_wdn[0], writes=[PR[b]])
                S.op("dve", TT(ysb[yi][:, hs], PS[b], bdb[bi][:, hs], ALU.add), reads=[PR[b], R_bd[bi]], writes=[R_ysb[yi]])
            r0 = j * BR + a * 128
            S.dma("sp", ys_d[r0:r0 + 128, :], ysb[yi], reads=[R_ysb[yi]])
    S.barrier()
    A.top = mark_core

    yk = [A.alloc([D], F32) for _ in range(4)]
    R_yk = [Reg() for _ in range(4)]
    acc = [A.alloc([D], F32) for _ in range(2)]
    R_acc = [Reg(), Reg()]
    xf = [A.alloc([D], F32) for _ in range(2)]
    R_xf = [Reg(), Reg()]
    kc = 0
    for n in range(NT):
        nsl = slice(n * 128, (n + 1) * 128)
        ai = n % 2
        S.dma("sp", xf[ai], y_out[nsl, :], reads=[R_xmid[n]], writes=[R_xf[ai]])
        for k in range(4):
            ki = kc % 4
            kc += 1
            S.idma("pool", yk[ki], None, ys_d, bass.IndirectOffsetOnAxis(ap=dest4i[:, n, k:k + 1], axis=0), NROWS - 1,
                   reads=[R_meta], writes=[R_yk[ki]])
            if k == 0:
                S.op("dve", TS(acc[ai], yk[ki], gate4[:, n, k:k + 1], None, ALU.mult), reads=[R_yk[ki], R_meta], writes=[R_acc[ai]])
            else:
                S.op("dve", STT(acc[ai], yk[ki], gate4[:, n, k:k + 1], acc[ai], ALU.mult, ALU.add), reads=[R_yk[ki], R_meta, R_acc[ai]], writes=[R_acc[ai]])
        S.op("act", ACT(sqj, acc[ai], AF.Square, accum=stat[:, 24:25]), reads=[R_acc[ai]], writes=[R_sqj, R_stat])
        S.op("act", ACT(stat[:, 25:26], stat[:, 24:25], AF.Sqrt, bias=eps_col, scale=1.0 / D), reads=[R_stat, R_eps], writes=[R_stat])
        S.op("dve", RECIP(stat[:, 26:27], stat[:, 25:26]), reads=[R_stat], writes=[R_stat])
        S.op("dve", STT(acc[ai], acc[ai], stat[:, 26:27], gtg2, ALU.mult, ALU.mult), reads=[R_acc[ai], R_stat, R_mods[5]], writes=[R_acc[ai]])
        S.op("pool", TT(acc[ai], acc[ai], xf[ai], ALU.add), reads=[R_acc[ai], R_xf[ai]], writes=[R_acc[ai]])
        S.dma("sp", y_out[nsl, :], acc[ai], reads=[R_acc[ai]], writes=[R_xmid[n]])
    S.finish()
    S.emit()
    return nc, dbg_outs


def _prep_inputs(inp):
    f = lambda a: np.ascontiguousarray(np.asarray(a, dtype=np.float32))
    x = f(inp["x"]); c = f(inp["c"])
    L = 0
    w_in = f(inp["w_in"][L])
    q = w_in[:, 0:512].reshape(1024, 8, 64)
    qsw = np.concatenate([q[:, :, 32:], q[:, :, :32]], axis=2).reshape(1024, 512)
    k = w_in[:, 512:640].reshape(1024, 2, 64)
    ksw = np.concatenate([k[:, :, 32:], k[:, :, :32]], axis=2)
    kdup = np.concatenate([k[:, 0], k[:, 0], k[:, 1], k[:, 1]], axis=1)
    kswdup = np.concatenate([ksw[:, 0], ksw[:, 0], ksw[:, 1], ksw[:, 1]], axis=1)
    w_ext = f(np.concatenate([w_in[:, 0:512], qsw, kdup, kswdup, w_in[:, 640:768], w_in[:, 768:1280]], axis=1))
    assert w_ext.shape == (1024, WEXT)
    rep = lambda v: f(np.broadcast_to(np.asarray(v, np.float32).reshape(1, -1), (128, np.asarray(v).size)))
    col = lambda v, nk: f(np.asarray(v, np.float32).reshape(nk, 128).T)
    gvecs = f(np.stack([rep(inp["g_pre_mix"][L]), rep(inp["g_post_mix"][L]), rep(inp["g_pre_ffn"][L]), rep(inp["g_post_ffn"][L])]))
    a = np.arange(128)[:, None]; b = np.arange(128)[None, :]
    m_prev = np.where(a <= b, 0.0, -30000.0)
    m_next = np.where(b <= a, 0.0, -30000.0)
    mask_mid = f(np.concatenate([m_prev, np.zeros((128, 128)), m_next], axis=1))
    inv_freq = (np.float32(10000.0) ** (-np.arange(32, dtype=np.float32) * np.float32(2.0) / np.float32(64))).astype(np.float32)
    shared = dict(
        w_ada=f(inp["w_ada"][L]), b_ada_rep=rep(inp["b_ada"][L]), gvecs=gvecs, w_ext=w_ext,
        sink_rep=rep(inp["attn_sink"][L]), mask_mid=mask_mid, ident=f(np.eye(128)),
        iota1=rep(np.arange(1, CH + 1)), dskip_col=col(inp["ssm_d"][L], 4),
        w_glu=f(inp["w_glu"][L]), bglu_col=col(inp["b_glu"][L], 4),
        gcat_col=col(np.concatenate([inp["g_attn_out"][L], inp["g_ssm_out"][L]]), 8),
        w_out=f(inp["w_out"][L]), w_router=f(inp["w_router"][L]), b_router_rep=rep(inp["b_router"][L]),
        wgu_rows=f(np.asarray(inp["w_gate_up"][L], np.float32).reshape(32, 8, 128, 2048).transpose(0, 2, 1, 3).reshape(32 * 128 * 2, 4 * 2048)),
        wdn_rows=f(np.asarray(inp["w_down"][L], np.float32).reshape(32, 8, 128, 1024).transpose(0, 2, 1, 3).reshape(32 * 128, 8 * 1024)),
        b_down=f(inp["b_down"][L]),
        bgu_rows=f(np.asarray(inp["b_gate_up"][L], np.float32).reshape(32, 16, 128).transpose(0, 2, 1).reshape(32 * 128, 16)),
        ltri=f(np.triu(np.ones((128, 128)), 1)),
        jb512=rep(np.arange(54) * 384.0),
        kp_iota=f(np.arange(2)[None, :] * 1.0 + 2.0 * np.arange(128)[:, None]),
        p_iota=f(np.arange(128).reshape(128, 1)),
    )
    are, aim, ldt = f(inp["ssm_a_re"][L]), f(inp["ssm_a_im"][L]), f(inp["ssm_log_dt"][L])
    bre, bim, cre, cim = f(inp["ssm_b_re"][L]), f(inp["ssm_b_im"][L]), f(inp["ssm_c_re"][L]), f(inp["ssm_c_im"][L])
    ssm_by_flip = {}
    for flip in (0, 1):
        dd = [1, 0] if flip else [0, 1]
        sc = {}
        for nm, arr in (("a_re_p", are), ("a_im_p", aim)):
            v = arr[dd].reshape(2, 16, 2, 64)
            sc[nm] = f(v.transpose(2, 3, 0, 1).reshape(128, 32))
        v = np.broadcast_to(ldt[dd].reshape(2, 16, 2, 1), (2, 16, 2, 64))
        sc["logdt_p"] = f(v.transpose(2, 3, 0, 1).reshape(128, 32))
        for nm, arr, is_c in (("bpad_re", bre, False), ("bpad_im", bim, False), ("cpad_re", cre, True), ("cpad_im", cim, True)):
            out = np.zeros((2, 16, 128, 128), np.float32)
            for d in range(2):
                for G in range(16):
                    for g2 in range(2):
                        g = 2 * G + g2
                        blk = arr[dd[d], g].T if is_c else arr[dd[d], g]
                        c0 = (G % 4) * 32 + g2 * 16
                        out[d, G, g2 * 64:(g2 + 1) * 64, c0:c0 + 16] = blk
            sc[nm] = out.reshape(32, 128, 128)
        ssm_by_flip[flip] = sc
    in_maps = []
    for core in range(8):
        bidx, hf = core // 2, core % 2
        seq = x[bidx]
        pos = np.arange(4096, dtype=np.float32)
        if hf == 1:
            seq = seq[::-1]
            pos = pos[::-1]
        pos = pos[:2176]
        ang = (pos[:, None] * inv_freq[None, :]).astype(np.float32)
        cs, sn = np.cos(ang).astype(np.float32), np.sin(ang).astype(np.float32)
        cos64 = np.concatenate([cs, cs], axis=1).T
        sin64 = np.concatenate([-sn, sn], axis=1).T
        m = dict(shared)
        m.update(ssm_by_flip[hf])
        m["x_core"] = f(seq)
        m["c_col"] = col(c[bidx], 8)
        m["rope_cos"] = f(np.concatenate([cos64, cos64], axis=0))
        m["rope_sin"] = f(np.concatenate([sin64, sin64], axis=0))
        in_maps.append(m)
    return in_maps


_CACHE = {}


def kernel(**inputs):
    in_maps = _prep_inputs(inputs)
    if "nc" not in _CACHE:
        _CACHE["nc"] = build()[0]
    res = run_bass_kernel_spmd(_CACHE["nc"], in_maps, core_ids=list(range(8)))
    out = np.zeros((4, 4096, 1024), np.float32)
    for core in range(8):
        bidx, hf = core // 2, core % 2
        y = np.asarray(res.results[core]["y_out"], np.float32)
        if hf == 0:
            out[bidx, :2048] = y
        else:
            out[bidx, 2048:] = y[::-1]
    return out
```

```python
import math
import os
import numpy as np
import concourse.bass as bass
import concourse.mybir as mybir
from concourse.bass_utils import run_bass_kernel_spmd
from contextlib import ExitStack

F32 = mybir.dt.float32
BF16 = mybir.dt.bfloat16
U8 = mybir.dt.uint8
I32 = mybir.dt.int32
ALU = mybir.AluOpType
AF = mybir.ActivationFunctionType
PI = math.pi

D = 1024
T_OWN = 2048
NT = 16
NE = 32
WEXT = 2176
QO, QSO, KO, KSO, VO, UO = 0, 512, 1024, 1280, 1536, 1664
CH = 512


class Reg:
    __slots__ = ("lw", "rd", "name")

    def __init__(self, name=""):
        self.lw = None
        self.rd = {}
        self.name = name


class Sched:
    CE = ("pe", "act", "dve", "pool")

    def __init__(self, nc, es, nslots=8):
        self.nc = nc
        self.sem, self.cnt, self.prog, self.waited = {}, {}, {}, {}
        for e in ("pe", "act", "dve", "pool", "sp"):
            self.prog[e] = []
            self.waited[e] = {}
        for e in self.CE:
            self.sem[e] = es.enter_context(nc.semaphore("sem_" + e))
            self.cnt[e] = 0
        self.ns = nslots
        self.dcount, self.dnext = {}, {}
        for q in ("sp", "pool", "bg"):
            self.dnext[q] = 0
            for s in range(nslots):
                k = ("d", q, s)
                self.sem[k] = es.enter_context(nc.semaphore("sd_%s_%d" % (q, s)))
                self.dcount[k] = 0

    def _deps(self, eng, reads, writes, skip_same=True):
        need = {}
        raw_same = 0

        def add(tok):
            if tok is None:
                return
            k, v = tok
            if need.get(k, 0) < v:
                need[k] = v

        for r in reads:
            add(r.lw)
            if r.lw is not None and r.lw[0] == eng and r.lw[1] > raw_same:
                raw_same = r.lw[1]
        for w in writes:
            add(w.lw)
            if w.lw is not None and w.lw[0] == eng and w.lw[1] > raw_same:
                raw_same = w.lw[1]
            if w.rd.get(eng, 0) > raw_same:
                raw_same = w.rd[eng]
            for k, v in w.rd.items():
                add((k, v))
        waits = []
        for k, v in need.items():
            if skip_same and k == eng:
                if eng == "pe" or raw_same == 0:
                    continue
                v = raw_same
            if self.waited[eng].get(k, 0) >= v:
                continue
            self.waited[eng][k] = v
            waits.append((k, v))
        return waits

    def op(self, eng, fn, reads=(), writes=()):
        waits = self._deps(eng, reads, writes)
        self.cnt[eng] += 1
        tok = (eng, self.cnt[eng])
        self.prog[eng].append((waits, fn, self.sem[eng], 1))
        for r in reads:
            if r.rd.get(eng, 0) < tok[1]:
                r.rd[eng] = tok[1]
        for w in writes:
            w.lw = tok
            w.rd = {}
        return tok

    def dma(self, q, out_ap, in_ap, reads=(), writes=(), ring=None):
        waits = self._deps(q, reads, writes, skip_same=False)
        ring = ring or q
        s = self.dnext[ring] % self.ns
        self.dnext[ring] += 1
        k = ("d", ring, s)
        c = self.dcount[k]
        if c > 0 and self.waited[q].get(k, 0) < 16 * c:
            self.waited[q][k] = 16 * c
            waits.append((k, 16 * c))
        self.dcount[k] = c + 1
        tok = (k, 16 * (c + 1))
        self.prog[q].append((waits, (lambda e: e.dma_start(out=out_ap, in_=in_ap)), self.sem[k], 16))
        for r in reads:
            if r.rd.get(k, 0) < tok[1]:
                r.rd[k] = tok[1]
        for w in writes:
            w.lw = tok
            w.rd = {}
        return tok

    def idma(self, q, out_ap, out_off, in_ap, in_off, bounds, reads=(), writes=()):
        waits = self._deps(q, reads, writes, skip_same=False)
        s = self.dnext[q] % self.ns
        self.dnext[q] += 1
        k = ("d", q, s)
        c = self.dcount[k]
        if c > 0 and self.waited[q].get(k, 0) < 16 * c:
            self.waited[q][k] = 16 * c
            waits.append((k, 16 * c))
        self.dcount[k] = c + 1
        tok = (k, 16 * (c + 1))
        self.prog[q].append((waits, (lambda e: e.indirect_dma_start(out=out_ap, out_offset=out_off, in_=in_ap, in_offset=in_off,
                                                                    bounds_check=None)), self.sem[k], 16))
        for r in reads:
            if r.rd.get(k, 0) < tok[1]:
                r.rd[k] = tok[1]
        for w in writes:
            w.lw = tok
            w.rd = {}
        return tok

    def barrier(self):
        cur = {e: self.cnt[e] for e in self.CE}
        for k, c in self.dcount.items():
            cur[k] = 16 * c
        for e in ("pe", "act", "dve", "pool", "sp"):
            waits = []
            for k, v in cur.items():
                if k == e or v == 0:
                    continue
                if self.waited[e].get(k, 0) >= v:
                    continue
                self.waited[e][k] = v
                waits.append((k, v))
            self.prog[e].append((waits, None, None, 0))

    def finish(self):
        waits = []
        for k, c in self.dcount.items():
            if c > 0:
                waits.append((k, 16 * c))
        self.prog["sp"].append((waits, None, None, 0))

    def emit(self):
        nc = self.nc

        def run(name):
            def f(e):
                for waits, fn, sem, inc in self.prog[name]:
                    for k, v in waits:
                        e.wait_ge(self.sem[k], v)
                    if fn is not None:
                        ins = fn(e)
                        ins.then_inc(sem, inc)
            return f

        with nc.Block() as block:
            block.tensor(run("pe"))
            block.scalar(run("act"))
            block.vector(run("dve"))
            block.gpsimd(run("pool"))
            block.sync(run("sp"))


DTS = {F32: 4, BF16: 2, U8: 1, I32: 4}


class Arena:
    def __init__(self, nc, nbytes):
        self.t = nc.alloc_sbuf_tensor("arena", [128, nbytes], U8)
        self.top = 0
        self.n = nbytes
        self.peak = 0
        self.hi = nbytes

    def alloc_top(self, free_shape, dt):
        size = int(np.prod(free_shape)) * DTS[dt]
        off = (self.hi - size) // 32 * 32
        self.hi = off
        ap = self.t[:, off:off + size].bitcast(dt)
        if len(free_shape) == 2:
            ap = ap.rearrange("p (a b) -> p a b", a=free_shape[0])
        return ap

    def alloc(self, free_shape, dt):
        size = int(np.prod(free_shape)) * DTS[dt]
        off = (self.top + 31) // 32 * 32
        self.top = off + size
        self.peak = max(self.peak, self.top)
        assert self.top <= self.hi, ("SBUF arena overflow", self.top, self.hi)
        ap = self.t[:, off:off + size].bitcast(dt)
        if len(free_shape) == 2:
            ap = ap.rearrange("p (a b) -> p a b", a=free_shape[0])
        elif len(free_shape) == 3:
            ap = ap.rearrange("p (a b c) -> p a b c", a=free_shape[0], b=free_shape[1])
        elif len(free_shape) == 4:
            ap = ap.rearrange("p (a b c d) -> p a b c d", a=free_shape[0], b=free_shape[1], c=free_shape[2])
        return ap


def bc_last(ap, m):
    return bass.AP(tensor=ap.tensor, offset=ap.offset, ap=[list(x) for x in ap.ap] + [[0, m]])


def rev(ap):
    dims = [list(x) for x in ap.ap]
    st, n = dims[-1]
    dims[-1] = [-st, n]
    return bass.AP(tensor=ap.tensor, offset=ap.offset + st * (n - 1), ap=dims)


def build(stage=99):
    nc = bass.Bass("TRN2", target_bir_lowering=False)
    es = ExitStack()

    def din(name, shape, dt=F32):
        return nc.dram_tensor(name, list(shape), dt, kind="ExternalInput").ap()

    x_core = din("x_core", [4096, D])
    c_col = din("c_col", [128, 8])
    w_ada = din("w_ada", [D, 6 * D])
    b_ada_rep = din("b_ada_rep", [128, 6 * D])
    gvecs = din("gvecs", [4, 128, D])
    w_ext = din("w_ext", [D, WEXT])
    rope_cos = din("rope_cos", [128, 2176])
    rope_sin = din("rope_sin", [128, 2176])
    sink_rep = din("sink_rep", [128, 8])
    mask_mid = din("mask_mid", [128, 384])
    ident_in = din("ident", [128, 128])
    iota1 = din("iota1", [128, CH])
    a_re_p = din("a_re_p", [128, 32])
    a_im_p = din("a_im_p", [128, 32])
    logdt_p = din("logdt_p", [128, 32])
    bpad_re = din("bpad_re", [32, 128, 128])
    bpad_im = din("bpad_im", [32, 128, 128])
    cpad_re = din("cpad_re", [32, 128, 128])
    cpad_im = din("cpad_im", [32, 128, 128])
    dskip_col = din("dskip_col", [128, 4])
    w_glu = din("w_glu", [512, 512])
    bglu_col = din("bglu_col", [128, 4])
    gcat_col = din("gcat_col", [128, 8])
    w_out = din("w_out", [D, D])
    w_router = din("w_router", [D, NE])
    b_router_rep = din("b_router_rep", [128, NE])
    wgu_rows = din("wgu_rows", [NE * 128 * 2, 4 * 2 * D])
    bgu_rows_in = din("bgu_rows", [NE * 128, 16])
    ltri_in = din("ltri", [128, 128])
    jb512_in = din("jb512", [128, 54])
    kp_iota_in = din("kp_iota", [128, 2])
    p_iota_in = din("p_iota", [128, 1])
    wdn_rows = din("wdn_rows", [NE * 128, 8 * D])
    b_down = din("b_down", [NE, D])
    y_out = nc.dram_tensor("y_out", [T_OWN, D], F32, kind="ExternalOutput").ap()
    dbg_outs = {}

    S = Sched(nc, es, nslots=12)
    A = Arena(nc, 204000)
    pst = [nc.alloc_psum_tensor("ps%d" % i, [128, 512], F32) for i in range(8)]
    PS = [t[:, :] for t in pst]
    PSB = [t[:, :].bitcast(BF16) for t in pst]
    PR = [Reg("ps%d" % i) for i in range(8)]
    bank_ctr = [0]

    def bank():
        i = bank_ctr[0] % 8
        bank_ctr[0] += 1
        return i

    def dbg(name, ap, shape, reg):
        t = nc.dram_tensor("dbg_" + name, list(shape), ap.dtype, kind="ExternalOutput").ap()
        dbg_outs[name] = t
        S.dma("sp", t, ap, reads=[reg])

    def ACT(out, in_, func, bias=None, scale=None, accum=None):
        kw = {}
        if bias is not None:
            kw["bias"] = bias
        if scale is not None:
            kw["scale"] = scale
        if accum is not None:
            kw["accum_out"] = accum
        return lambda e: e.activation(out=out, in_=in_, func=func, **kw)

    def TS(out, in0, s1, s2, op0, op1=None, accum=None):
        kw = {}
        if op1 is not None:
            kw["op1"] = op1
        if accum is not None:
            kw["accum_out"] = accum
        return lambda e: e.tensor_scalar(out=out, in0=in0, scalar1=s1, scalar2=s2, op0=op0, **kw)

    def TT(out, in0, in1, op):
        return lambda e: e.tensor_tensor(out=out, in0=in0, in1=in1, op=op)

    def STT(out, in0, sc, in1, op0, op1, accum=None):
        kw = {}
        if accum is not None:
            kw["accum_out"] = accum
        return lambda e: e.scalar_tensor_tensor(out=out, in0=in0, scalar=sc, in1=in1, op0=op0, op1=op1, **kw)

    def CP(out, in_):
        return lambda e: e.tensor_copy(out=out, in_=in_)

    def MM(groups):
        def f(e):
            ins = None
            for (o, l, r, st, sp_) in groups:
                ins = e.matmul(o, lhsT=l, rhs=r, start=st, stop=sp_)
            return ins
        return f

    def TR(pairs, ident):
        def f(e):
            ins = None
            for (o, i) in pairs:
                ins = e.transpose(o, i, ident)
            return ins
        return f


    def ACOPY(out, in_):
        return lambda e: e.copy(out=out, in_=in_)

    def AMUL(out, in_, m):
        return lambda e: e.mul(out=out, in_=in_, mul=m)

    def RECIP(out, in_):
        return lambda e: e.reciprocal(out=out, in_=in_)

    def MEMSET(ap, v):
        return lambda e: e.memset(ap, v)

    def SCAN(out, d0, d1, init):
        return lambda e: e.tensor_tensor_scan(out=out, data0=d0, data1=d1, initial=init, op0=ALU.mult, op1=ALU.add)

    ident = A.alloc([128], BF16)
    R_ident = Reg()
    S.dma("pool", ident, ident_in, writes=[R_ident])
    ones_col = A.alloc([16], BF16)
    R_ones = Reg()
    S.op("dve", MEMSET(ones_col, 1.0), writes=[R_ones])

    eps_col = A.alloc([1], F32)
    R_eps = Reg()
    S.op("dve", MEMSET(eps_col, 1e-6), writes=[R_eps])
    negpi = A.alloc([1], F32)
    S.op("dve", MEMSET(negpi, -PI), writes=[R_eps])
    stat = A.alloc([64], F32)
    R_stat = Reg()
    sqj = A.alloc([D], BF16)
    R_sqj = Reg()
    gtg2 = A.alloc([D], F32)
    mark_core = A.top
    mods5 = A.alloc([5, D], F32)
    R_mods = [Reg("mods%d" % i) for i in range(6)]

    def modsl(slot):
        return gtg2 if slot == 5 else mods5[:, slot, :]

    def modsl_s(slot, sl):
        return gtg2[:, sl] if slot == 5 else mods5[:, slot, sl]
    SEGMAP = {0: 1, 1: 0, 2: 2, 3: 4, 4: 3, 5: 5}
    mark_persist = A.top

    ccol = A.alloc([8], F32)
    R_cc = Reg()
    S.dma("sp", ccol, c_col, writes=[R_cc])
    csil = A.alloc([8], F32)
    R_cs = Reg()
    S.op("act", ACT(csil, ccol, AF.Silu), reads=[R_cc], writes=[R_cs])
    cl = A.alloc([8, 128], BF16)
    R_cl = Reg()
    for k in range(8):
        S.op("dve", CP(cl[:, k, :], csil[:, k:k + 1].to_broadcast([128, 128])), reads=[R_cs], writes=[R_cl])
    wada_buf = [A.alloc([8, 512], BF16) for _ in range(2)]
    R_wada = [Reg(), Reg()]
    bada_buf = [A.alloc([512], F32) for _ in range(2)]
    R_bada = [Reg(), Reg()]
    w_ada_v = w_ada.rearrange("(k p) n -> p k n", p=128)
    for j in range(12):
        bi = j % 2
        S.dma("pool", wada_buf[bi], w_ada_v[:, :, j * 512:(j + 1) * 512], writes=[R_wada[bi]])
        S.dma("sp", bada_buf[bi], b_ada_rep[:, j * 512:(j + 1) * 512], writes=[R_bada[bi]])
        b = bank()
        S.op("pe", MM([(PS[b], cl[:, k, :], wada_buf[bi][:, k, :], k == 0, k == 7) for k in range(8)]),
             reads=[R_cl, R_wada[bi]], writes=[PR[b]])
        slot = SEGMAP[j // 2]
        S.op("dve", TT(modsl_s(slot, slice((j % 2) * 512, (j % 2) * 512 + 512)), PS[b], bada_buf[bi], ALU.add),
             reads=[PR[b], R_bada[bi]], writes=[R_mods[slot]])
    gtmp = A.alloc([D], F32)
    R_gt = Reg()
    for (gi, slot, plus1) in ((0, 0, True), (1, 2, False), (2, 3, True), (3, 5, False)):
        S.dma("sp", gtmp, gvecs[gi], writes=[R_gt])
        if plus1:
            S.op("dve", STT(modsl(slot), modsl(slot), 1.0, gtmp, ALU.add, ALU.mult),
                 reads=[R_gt, R_mods[slot]], writes=[R_mods[slot]])
        else:
            S.op("dve", TT(modsl(slot), modsl(slot), gtmp, ALU.mult),
                 reads=[R_gt, R_mods[slot]], writes=[R_mods[slot]])
    if stage == 0:
        dbg("mods", mods5, [128, 5, D], R_mods[0])
        dbg("gtg2", gtg2, [128, D], R_mods[5])
        S.finish(); S.emit(); return nc, dbg_outs
    S.barrier()
    A.top = mark_persist

    wgu_bf = nc.dram_tensor("wgu_bf", [NE * 256, 8192], BF16, kind="Internal").ap()
    wdn_bf = nc.dram_tensor("wdn_bf", [NE * 128, 8192], BF16, kind="Internal").ap()
    bg_list = []
    for e_ in range(NE):
        bg_list.append((wgu_bf[e_ * 256:e_ * 256 + 128, :], wgu_rows[e_ * 256:e_ * 256 + 128, :]))
        bg_list.append((wgu_bf[e_ * 256 + 128:e_ * 256 + 256, :], wgu_rows[e_ * 256 + 128:e_ * 256 + 256, :]))
        bg_list.append((wdn_bf[e_ * 128:(e_ + 1) * 128, :], wdn_rows[e_ * 128:(e_ + 1) * 128, :]))
    bg_pos = [0]

    def bg_step(n=1):
        for _ in range(n):
            if bg_pos[0] < len(bg_list):
                o_, i_ = bg_list[bg_pos[0]]
                bg_pos[0] += 1
                S.dma("pool", o_, i_, ring="bg")

    uT = A.alloc([4, 4096], BF16)
    R_u = [Reg() for _ in range(8)]
    attnT = A.alloc([4, T_OWN], BF16)
    R_attnT = [Reg() for _ in range(NT)]
    rattn = A.alloc([NT], F32)
    R_rattn = Reg()
    mark_B = A.top
    qT = A.alloc([4, T_OWN], BF16)
    kT = A.alloc([2, 2176], BF16)
    vaug = A.alloc([17, 2, 65], BF16)
    maskb = A.alloc([384], BF16)
    R_mask = Reg()
    S.dma("pool", maskb, mask_mid, writes=[R_mask])
    R_q = [Reg() for _ in range(4)]
    R_k = [Reg() for _ in range(5)]
    R_v = [Reg() for _ in range(17)]
    S.op("pool", MEMSET(vaug, 1.0), writes=R_v)
    mark_C = A.top

    W = {}

    def alloc_norm_bufs():
        W["xt"] = [A.alloc([D], F32) for _ in range(2)]
        W["R_xt"] = [Reg(), Reg()]
        W["t1"] = A.alloc([D], F32)
        W["R_t1"] = Reg()
        W["hb"] = [A.alloc([D], BF16) for _ in range(2)]
        W["R_hb"] = [Reg(), Reg()]

    alloc_norm_bufs()
    wext = A.alloc([8, WEXT], BF16)
    R_wext = Reg()
    w_ext_v = w_ext.rearrange("(k p) n -> p k n", p=128)
    for k in range(0, 8, 2):
        S.dma("pool", wext[:, k:k + 2, :], w_ext_v[:, k:k + 2, :], writes=[R_wext])
    rcos = A.alloc([2176], BF16)
    rsin = A.alloc([2176], BF16)
    R_rope = Reg()
    S.dma("pool", rcos, rope_cos, writes=[R_rope])
    S.dma("pool", rsin, rope_sin, writes=[R_rope])
    hT = [A.alloc([8, 512], BF16) for _ in range(2)]
    R_hT = [Reg(), Reg()]
    ropet = [A.alloc([512], F32) for _ in range(4)]
    R_ropet = [Reg() for _ in range(4)]

    def prenorm_tile(src_ap, xbuf, R_x, gslot, shslot, dst_ap, R_dst, tcnt, load=True):
        bi = tcnt % 2
        t1, R_t1 = W["t1"], W["R_t1"]
        hb, R_hb = W["hb"][bi], W["R_hb"][bi]
        if load:
            S.dma("sp", xbuf, src_ap, writes=[R_x])
        S.op("act", ACT(sqj, xbuf, AF.Square, accum=stat[:, 0:1]), reads=[R_x], writes=[R_sqj, R_stat])
        S.op("act", ACT(stat[:, 1:2], stat[:, 0:1], AF.Sqrt, bias=eps_col, scale=1.0 / D), reads=[R_stat, R_eps], writes=[R_stat])
        S.op("dve", RECIP(stat[:, 2:3], stat[:, 1:2]), reads=[R_stat], writes=[R_stat])
        S.op("dve", STT(t1, xbuf, stat[:, 2:3], modsl(gslot), ALU.mult, ALU.mult),
             reads=[R_x, R_stat, R_mods[gslot]], writes=[R_t1])
        S.op("pool", TT(hb, t1, modsl(shslot), ALU.add), reads=[R_t1, R_mods[shslot]], writes=[R_hb])
        b = bank()
        S.op("pe", TR([(PSB[b][:, k * 128:(k + 1) * 128], hb[:, k * 128:(k + 1) * 128]) for k in range(8)], ident),
             reads=[R_hb, R_ident], writes=[PR[b]])
        S.op("act", ACOPY(dst_ap, PSB[b].rearrange("p (k c) -> p k c", k=8)), reads=[PR[b]], writes=[R_dst])

    def proj_fm(colblk_off, hTb, R_h, ntok_):
        b = bank()
        S.op("pe", MM([(PS[b][:, :ntok_], wext[:, k, colblk_off:colblk_off + 128], hTb[:, k, :ntok_], k == 0, k == 7)
                       for k in range(8)]), reads=[R_wext, R_h], writes=[PR[b]])
        return b

    tcnt = 0
    nblk = 8 if stage >= 3 else 5
    for blk in range(nblk):
        own = blk < 4
        ntile = 4 if (own or stage >= 3) else 1
        hb_i = blk % 2
        for tl in range(ntile):
            tile_i = blk * 4 + tl
            xi = tcnt % 2
            prenorm_tile(x_core[tile_i * 128:(tile_i + 1) * 128, :], W["xt"][xi], W["R_xt"][xi], 0, 1,
                         hT[hb_i][:, :, tl * 128:(tl + 1) * 128], R_hT[hb_i], tcnt)
            tcnt += 1
            bg_step(1)
        ntok = ntile * 128
        tok0 = blk * 512
        if own or blk == 4:
            kt = 512 if own else 128
            plist = []
            if own:
                plist += [("q", cb, QO + cb * 128, QSO + cb * 128) for cb in range(4)]
            plist += [("k", kb, KO + kb * 128, KSO + kb * 128) for kb in range(2)]
            for pi, (kind, cb, o1, o2) in enumerate(plist):
                ba = proj_fm(o1, hT[hb_i], R_hT[hb_i], kt)
                bb = proj_fm(o2, hT[hb_i], R_hT[hb_i], kt)
                ra, rb = ropet[(2 * pi) % 4], ropet[(2 * pi + 1) % 4]
                Ra, Rb = R_ropet[(2 * pi) % 4], R_ropet[(2 * pi + 1) % 4]
                S.op("dve", TT(ra[:, :kt], PS[ba][:, :kt], rcos[:, tok0:tok0 + kt], ALU.mult), reads=[PR[ba], R_rope], writes=[Ra])
                S.op("dve", TT(rb[:, :kt], PS[bb][:, :kt], rsin[:, tok0:tok0 + kt], ALU.mult), reads=[PR[bb], R_rope], writes=[Rb])
                if kind == "q":
                    S.op("pool", TT(qT[:, cb, tok0:tok0 + kt], ra[:, :kt], rb[:, :kt], ALU.add), reads=[Ra, Rb], writes=[R_q[blk]])
                else:
                    S.op("pool", TT(kT[:, cb, tok0:tok0 + kt], ra[:, :kt], rb[:, :kt], ALU.add), reads=[Ra, Rb], writes=[R_k[blk]])
            for tl in range(4 if own else 1):
                tile_i = blk * 4 + tl
                b = bank()
                S.op("pe", MM([(PS[b][:, :128], hT[hb_i][:, k, tl * 128:(tl + 1) * 128], wext[:, k, VO:VO + 128], k == 0, k == 7)
                               for k in range(8)]), reads=[R_wext, R_hT[hb_i]], writes=[PR[b]])
                S.op("act", ACOPY(vaug[:, tile_i, :, 0:64], PS[b][:, :128].rearrange("p (h c) -> p h c", h=2)),
                     reads=[PR[b]], writes=[R_v[tile_i]])
        for cb in range(4):
            b = proj_fm(UO + cb * 128, hT[hb_i], R_hT[hb_i], ntok)
            S.op("act", ACOPY(uT[:, cb, tok0:tok0 + ntok], PS[b][:, :ntok]), reads=[PR[b]], writes=[R_u[blk]])
    if stage == 1:
        dbg("qT", qT, [128, 4, T_OWN], R_q[3])
        dbg("kT", kT, [128, 2, 2176], R_k[4])
        dbg("vaug", vaug, [128, 17, 2, 65], R_v[16])
        dbg("uT", uT, [128, 4, 4096], R_u[4])
        S.finish(); S.emit(); return nc, dbg_outs
    S.barrier()
    A.top = mark_C

    esink = A.alloc([8], F32)
    R_es = Reg()
    S.dma("sp", esink, sink_rep, writes=[R_es])
    S.op("act", ACT(esink, esink, AF.Exp), reads=[R_es], writes=[R_es])
    pT = A.alloc([4, 8, 384], BF16)
    R_pT = [[Reg() for _ in range(8)] for _ in range(4)]
    attn_o = [A.alloc([512], BF16) for _ in range(2)]
    R_ao = [Reg(), Reg()]
    den = A.alloc([16], F32)
    R_den = Reg()

    def kq_range(j):
        return max(j - 1, 0), min(j + 2, 16)

    def pv_block(n):
        bo = [bank(), bank()]
        ai = n % 2
        for hh in range(2):
            groups, regs = [], []
            for h4 in range(4):
                h = hh * 4 + h4
                kvh = h // 4
                js = [j for j in (n - 1, n, n + 1) if 0 <= j <= 16]
                for ji, j in enumerate(js):
                    qb0, _ = kq_range(j)
                    c0 = (n - qb0) * 128
                    groups.append((PS[bo[hh]][:, h4 * 65:(h4 + 1) * 65], pT[:, j % 4, h, c0:c0 + 128], vaug[:, j, kvh, :],
                                   ji == 0, ji == len(js) - 1))
                    regs.append(R_pT[j % 4][h])
                    regs.append(R_v[j])
            S.op("pe", MM(groups), reads=regs, writes=[PR[bo[hh]]])
        for hh in range(2):
            pv = PS[bo[hh]][:, 0:260].rearrange("p (h c) -> p h c", h=4)
            S.op("dve", TT(den[:, hh * 4:hh * 4 + 4], pv[:, :, 64], esink[:, hh * 4:hh * 4 + 4], ALU.add),
                 reads=[PR[bo[hh]], R_es], writes=[R_den])
        S.op("dve", RECIP(den[:, 8:16], den[:, 0:8]), reads=[R_den], writes=[R_den])
        for hh in range(2):
            pv = PS[bo[hh]][:, 0:260].rearrange("p (h c) -> p h c", h=4)
            S.op("dve", TT(attn_o[ai][:, hh * 256:(hh + 1) * 256].rearrange("p (h c) -> p h c", h=4), pv[:, :, 0:64],
                           bc_last(den[:, 8 + hh * 4:8 + hh * 4 + 4], 64), ALU.mult),
                 reads=[PR[bo[hh]], R_den], writes=[R_ao[ai]])
        S.op("act", ACT(sqj[:, 0:512], attn_o[ai], AF.Square, accum=stat[:, 8:9]), reads=[R_ao[ai]], writes=[R_sqj, R_stat])
        S.op("act", ACT(stat[:, 9:10], stat[:, 8:9], AF.Sqrt, bias=eps_col, scale=1.0 / 512), reads=[R_stat, R_eps], writes=[R_stat])
        S.op("dve", RECIP(rattn[:, n:n + 1], stat[:, 9:10]), reads=[R_stat], writes=[R_rattn])
        b = bank()
        S.op("pe", TR([(PSB[b][:, k * 128:(k + 1) * 128], attn_o[ai][:, k * 128:(k + 1) * 128]) for k in range(4)], ident),
             reads=[R_ao[ai], R_ident], writes=[PR[b]])
        S.op("act", ACOPY(attnT[:, :, n * 128:(n + 1) * 128], PSB[b][:, 0:512].rearrange("p (k c) -> p k c", k=4)),
             reads=[PR[b]], writes=[R_attnT[n]])

    for j in range(17):
        qb0, qb1 = kq_range(j)
        ncol = (qb1 - qb0) * 128
        moff = 128 if j == 0 else 0
        for h in range(8):
            kvh, qblk, pr = h // 4, h // 2, (h % 2) * 64
            b = bank()
            S.op("pe", MM([(PS[b][:, :ncol], kT[pr:pr + 64, kvh, j * 128:(j + 1) * 128], qT[pr:pr + 64, qblk, qb0 * 128:qb1 * 128], True, False),
                           (PS[b][:, :ncol], ident, maskb[:, moff:moff + ncol], False, True)]),
                 reads=[R_k[min(j // 4, 4)], R_q[0], R_q[1], R_q[2], R_q[3], R_ident, R_mask], writes=[PR[b]])
            S.op("act", ACT(pT[:, j % 4, h, :ncol], PS[b][:, :ncol], AF.Exp, scale=0.125), reads=[PR[b]], writes=[R_pT[j % 4][h]])
        if j >= 1:
            pv_block(j - 1)
        bg_step(1)
    if stage == 2:
        dbg("attnT", attnT, [128, 4, T_OWN], R_attnT[15])
        dbg("rattn", rattn, [128, NT], R_rattn)
        S.finish(); S.emit(); return nc, dbg_outs
    S.barrier()
    A.top = mark_B

    y2T = A.alloc([4, T_OWN], BF16)
    R_y2T = [Reg() for _ in range(4)]
    rssm = A.alloc([NT], F32)
    R_rssm = Reg()
    mark_D = A.top
    are = A.alloc([32], F32); aim = A.alloc([32], F32); ldt = A.alloc([32], F32)
    R_sc = Reg()
    S.dma("sp", are, a_re_p, writes=[R_sc])
    S.dma("sp", aim, a_im_p, writes=[R_sc])
    S.dma("sp", ldt, logdt_p, writes=[R_sc])
    dtv = A.alloc([32], F32); lr = A.alloc([32], F32); li = A.alloc([32], F32); rmag = A.alloc([32], F32)
    tmpa = A.alloc([32], F32); tmpb = A.alloc([32], F32); cosl = A.alloc([32], F32); sinl = A.alloc([32], F32)
    fr = A.alloc([32], F32); fi = A.alloc([32], F32); dnm = A.alloc([32], F32)
    dsk = A.alloc([4], F32)
    kis = A.alloc([32], I32)
    S.dma("sp", dsk, dskip_col, writes=[R_sc])
    seq = [
        ("act", ACT(dtv, ldt, AF.Exp)),
        ("dve", TT(lr, are, dtv, ALU.mult)),
        ("dve", TT(li, aim, dtv, ALU.mult)),
        ("act", ACT(rmag, lr, AF.Exp)),
        ("dve", TS(kis, li, 1.0 / (2 * PI), None, ALU.mult)),
        ("dve", CP(tmpa, kis)),
        ("dve", STT(tmpa, tmpa, -2 * PI, li, ALU.mult, ALU.add)),
        ("dve", TS(tmpa, tmpa, 3.1415925, -3.1415925, ALU.min, ALU.max)),
        ("act", ACT(sinl, tmpa, AF.Sin)),
        ("dve", TS(tmpb, li, PI / 2, None, ALU.add)),
        ("dve", TS(kis, tmpb, 1.0 / (2 * PI), None, ALU.mult)),
        ("dve", CP(tmpa, kis)),
        ("dve", STT(tmpa, tmpa, -2 * PI, tmpb, ALU.mult, ALU.add)),
        ("dve", TS(tmpa, tmpa, 3.1415925, -3.1415925, ALU.min, ALU.max)),
        ("act", ACT(cosl, tmpa, AF.Sin)),
        ("dve", TT(cosl, cosl, rmag, ALU.mult)),
        ("dve", TT(sinl, sinl, rmag, ALU.mult)),
        ("dve", TS(cosl, cosl, -1.0, None, ALU.add)),
        ("dve", TT(dnm, are, are, ALU.mult)),
        ("dve", TT(tmpa, aim, aim, ALU.mult)),
        ("dve", TT(dnm, dnm, tmpa, ALU.add)),
        ("dve", RECIP(dnm, dnm)),
        ("dve", TT(tmpa, cosl, are, ALU.mult)),
        ("dve", TT(tmpb, sinl, aim, ALU.mult)),
        ("dve", TT(fr, tmpa, tmpb, ALU.add)),
        ("dve", TT(fr, fr, dnm, ALU.mult)),
        ("dve", TT(tmpa, sinl, are, ALU.mult)),
        ("dve", TT(tmpb, cosl, aim, ALU.mult)),
        ("dve", TT(fi, tmpa, tmpb, ALU.subtract)),
        ("dve", TT(fi, fi, dnm, ALU.mult)),
    ]
    for eng, fn in seq:
        S.op(eng, fn, reads=[R_sc, R_eps], writes=[R_sc])
    iot = A.alloc([CH], F32)
    R_iot = Reg()
    S.dma("sp", iot, iota1, writes=[R_iot])

    NG = 4
    wl = [[A.alloc([128], BF16) for _ in range(2)] for _ in range(NG)]
    cm = [[A.alloc([128], BF16) for _ in range(2)] for _ in range(NG)]
    tabC = [A.alloc([CH], F32) for _ in range(NG)]
    tabS = [A.alloc([CH], F32) for _ in range(NG)]
    R_gc = [Reg() for _ in range(NG)]
    ldb = [A.alloc([128], F32) for _ in range(4)]
    R_ldb = [Reg() for _ in range(4)]
    bbf = [A.alloc([128], BF16) for _ in range(2)]
    R_bbf = [Reg(), Reg()]
    NW = 4
    mt = [[A.alloc([CH], F32) for _ in range(4)] for _ in range(NW)]
    qt = [[A.alloc([CH], BF16) for _ in range(2)] for _ in range(NW)]
    R_mt = [[Reg() for _ in range(4)] for _ in range(NW)]
    R_qt = [Reg() for _ in range(NW)]
    mtmp = mt
    R_w = [R_mt[0][0], R_mt[1][0]]
    argt, argk = mt[3][0], mt[3][1]
    kint = mt[3][2].bitcast(I32)
    R_argt = R_mt[3][0]
    R_argk = R_mt[3][1]
    R_kint = R_mt[3][2]
    sro = [[A.alloc([CH], BF16) for _ in range(2)] for _ in range(NG)]
    R_sro = [Reg() for _ in range(NG)]
    sprev = [A.alloc([2], F32) for _ in range(NG)]
    R_sprev = [Reg() for _ in range(NG)]
    cs5 = [A.alloc([4], F32) for _ in range(NG)]
    ltmp = [A.alloc([2], F32) for _ in range(NW)]
    ysum = A.alloc([T_OWN], F32)
    R_ys = [Reg() for _ in range(4)]
    gact = A.alloc([4, T_OWN], BF16)
    R_gact = [Reg() for _ in range(4)]
    wcount = [0]

    for cb in range(4):
        for d in range(2):
            for gs in range(NG):
                G = cb * 4 + gs
                dg = d * 16 + G
                S.dma("sp", ldb[0], bpad_re[dg], writes=[R_ldb[0]])
                S.dma("sp", ldb[1], bpad_im[dg], writes=[R_ldb[1]])
                S.dma("sp", ldb[2], cpad_re[dg], writes=[R_ldb[2]])
                S.dma("sp", ldb[3], cpad_im[dg], writes=[R_ldb[3]])
                frc, fic = fr[:, dg:dg + 1], fi[:, dg:dg + 1]
                ta, tb_ = mt[0][0][:, :128], mt[0][1][:, :128]
                S.op("dve", TS(ta, ldb[1], fic, None, ALU.mult), reads=[R_ldb[1], R_sc], writes=[R_mt[0][0]])
                S.op("dve", STT(bbf[0], ldb[0], frc, ta, ALU.mult, ALU.subtract), reads=[R_ldb[0], R_sc, R_mt[0][0]], writes=[R_bbf[0]])
                S.op("dve", TS(tb_, ldb[0], fic, None, ALU.mult), reads=[R_ldb[0], R_sc], writes=[R_mt[0][1]])
                S.op("dve", STT(bbf[1], ldb[1], frc, tb_, ALU.mult, ALU.add), reads=[R_ldb[1], R_sc, R_mt[0][1]], writes=[R_bbf[1]])
                b = bank()
                S.op("pe", TR([(PSB[b][:, 0:128], bbf[0]), (PSB[b][:, 128:256], bbf[1])], ident),
                     reads=[R_bbf[0], R_bbf[1], R_ident], writes=[PR[b]])
                S.op("act", ACOPY(wl[gs][0], PSB[b][:, 0:128]), reads=[PR[b]], writes=[R_gc[gs]])
                S.op("act", ACOPY(wl[gs][1], PSB[b][:, 128:256]), reads=[PR[b]], writes=[R_gc[gs]])
                S.op("act", ACOPY(cm[gs][0], ldb[2]), reads=[R_ldb[2]], writes=[R_gc[gs]])
                S.op("act", AMUL(cm[gs][1], ldb[3], -1.0), reads=[R_ldb[3]], writes=[R_gc[gs]])
                lic = li[:, dg:dg + 1]
                S.op("dve", TS(argt, iot, lic, None, ALU.mult), reads=[R_iot, R_sc], writes=[R_argt])
                for (tab, shift) in ((tabS[gs], False), (tabC[gs], True)):
                    if shift:
                        S.op("dve", TS(argt, argt, PI / 2, None, ALU.add), reads=[R_argt], writes=[R_argt])
                    S.op("dve", TS(kint, argt, 1.0 / (2 * PI), None, ALU.mult), reads=[R_argt], writes=[R_kint])
                    S.op("dve", CP(argk, kint), reads=[R_kint], writes=[R_argk])
                    S.op("dve", STT(argk, argk, -2 * PI, argt, ALU.mult, ALU.add), reads=[R_argt, R_argk], writes=[R_argk])
                    S.op("dve", TS(argk, argk, 3.1415925, -3.1415925, ALU.min, ALU.max), reads=[R_argk], writes=[R_argk])
                    S.op("act", ACT(tab, argk, AF.Sin), reads=[R_argk], writes=[R_gc[gs]])
                S.op("dve", CP(cs5[gs][:, 0:1], tabC[gs][:, CH - 1:CH]), reads=[R_gc[gs]], writes=[R_sprev[gs]])
                S.op("dve", CP(cs5[gs][:, 1:2], tabS[gs][:, CH - 1:CH]), reads=[R_gc[gs]], writes=[R_sprev[gs]])
                S.op("dve", TS(cs5[gs][:, 2:3], tabS[gs][:, CH - 1:CH], -1.0, None, ALU.mult), reads=[R_gc[gs]], writes=[R_sprev[gs]])
                S.op("dve", CP(cs5[gs][:, 3:4], tabC[gs][:, CH - 1:CH]), reads=[R_gc[gs]], writes=[R_sprev[gs]])
                S.op("dve", MEMSET(sprev[gs], 0.0), writes=[R_sprev[gs]])
            chunks = [0, 1, 2, 3] if d == 0 else [7, 6, 5, 4, 3, 2, 1, 0]
            for c in chunks:
                is_own = c < 4
                t0 = c * CH
                for gs in range(NG):
                    G = cb * 4 + gs
                    dg = d * 16 + G
                    wi = wcount[0] % NW
                    wcount[0] += 1
                    if wcount[0] % 3 == 0:
                        bg_step(1)
                    m, Rm = mt[wi], R_mt[wi]
                    ba, bb = bank(), bank()
                    S.op("pe", MM([(PS[ba], wl[gs][0], uT[:, cb, t0:t0 + CH], True, True)]), reads=[R_gc[gs], R_u[c]], writes=[PR[ba]])
                    S.op("pe", MM([(PS[bb], wl[gs][1], uT[:, cb, t0:t0 + CH], True, True)]), reads=[R_gc[gs], R_u[c]], writes=[PR[bb]])
                    tC = tabC[gs] if d == 0 else rev(tabC[gs])
                    tS = tabS[gs] if d == 0 else rev(tabS[gs])
                    S.op("dve", TT(m[0], PS[ba], tC, ALU.mult), reads=[PR[ba], R_gc[gs]], writes=[Rm[0]])
                    S.op("dve", TT(m[1], PS[bb], tS, ALU.mult), reads=[PR[bb], R_gc[gs]], writes=[Rm[1]])
                    S.op("dve", TT(m[2], PS[bb], tC, ALU.mult), reads=[PR[bb], R_gc[gs]], writes=[Rm[2]])
                    S.op("dve", TT(m[3], PS[ba], tS, ALU.mult), reads=[PR[ba], R_gc[gs]], writes=[Rm[3]])
                    S.op("pool", TT(m[0], m[0], m[1], ALU.add), reads=[Rm[0], Rm[1]], writes=[Rm[0]])
                    S.op("pool", TT(m[2], m[2], m[3], ALU.subtract), reads=[Rm[2], Rm[3]], writes=[Rm[2]])
                    rcol = rmag[:, dg:dg + 1]
                    rb = bass.AP(tensor=rcol.tensor, offset=rcol.offset, ap=[list(rcol.ap[0]), [0, CH]])
                    for ri, (src, dst) in enumerate(((0, 1), (2, 3))):
                        o = m[dst] if d == 0 else rev(m[dst])
                        i1 = m[src] if d == 0 else rev(m[src])
                        S.op("dve", SCAN(o, rb, i1, sprev[gs][:, ri:ri + 1]),
                             reads=[Rm[src], R_sprev[gs], R_sc], writes=[Rm[dst]])
                    lc = CH - 1 if d == 0 else 0
                    sr_l, si_l = m[1][:, lc:lc + 1], m[3][:, lc:lc + 1]
                    S.op("dve", TS(ltmp[wi], cs5[gs][:, 2:4], si_l, None, ALU.mult), reads=[Rm[3], R_sprev[gs]], writes=[R_qt[wi]])
                    S.op("dve", STT(sprev[gs], cs5[gs][:, 0:2], sr_l, ltmp[wi], ALU.mult, ALU.add), reads=[Rm[1], R_qt[wi], R_sprev[gs]], writes=[R_sprev[gs]])
                    if is_own:
                        S.op("dve", TT(m[0], m[1], tC, ALU.mult), reads=[Rm[1], R_gc[gs]], writes=[Rm[0]])
                        S.op("dve", TT(m[2], m[3], tS, ALU.mult), reads=[Rm[3], R_gc[gs]], writes=[Rm[2]])
                        S.op("dve", TT(qt[wi][0], m[1], tS, ALU.mult), reads=[Rm[1], R_gc[gs]], writes=[R_qt[wi]])
                        S.op("dve", TT(qt[wi][1], m[3], tC, ALU.mult), reads=[Rm[3], R_gc[gs]], writes=[R_qt[wi]])
                        S.op("dve", TT(sro[gs][0], m[0], m[2], ALU.subtract), reads=[Rm[0], Rm[2]], writes=[R_sro[gs]])
                        S.op("dve", TT(sro[gs][1], qt[wi][0], qt[wi][1], ALU.add), reads=[R_qt[wi]], writes=[R_sro[gs]])
                if is_own:
                    b = bank()
                    groups = []
                    for gs in range(NG):
                        groups.append((PS[b], cm[gs][0], sro[gs][0], gs == 0, False))
                        groups.append((PS[b], cm[gs][1], sro[gs][1], False, gs == NG - 1))
                    S.op("pe", MM(groups), reads=R_gc + R_sro, writes=[PR[b]])
                    ys = ysum[:, t0:t0 + CH]
                    if d == 0:
                        S.op("dve", STT(ys, uT[:, cb, t0:t0 + CH], dsk[:, cb:cb + 1], PS[b], ALU.mult, ALU.add),
                             reads=[PR[b], R_u[c], R_sc], writes=[R_ys[c]])
                    else:
                        S.op("dve", TT(ys, ys, PS[b], ALU.add), reads=[PR[b], R_ys[c]], writes=[R_ys[c]])
        for c in range(4):
            ys = ysum[:, c * CH:(c + 1) * CH]
            g1, g2 = mt[c % 2][0], mt[c % 2][1]
            Rg1, Rg2 = R_mt[c % 2][0], R_mt[c % 2][1]
            S.op("act", ACT(g1, ys, AF.Square), reads=[R_ys[c]], writes=[Rg1])
            S.op("dve", TS(g1, g1, 0.044715, 1.0, ALU.mult, ALU.add), reads=[Rg1], writes=[Rg1])
            S.op("dve", TT(g1, g1, ys, ALU.mult), reads=[Rg1, R_ys[c]], writes=[Rg1])
            S.op("act", ACT(g2, g1, AF.Sigmoid, scale=1.5957691216057308), reads=[Rg1], writes=[Rg2])
            S.op("pool", TT(gact[:, cb, c * CH:(c + 1) * CH], ys, g2, ALU.mult), reads=[Rg2, R_ys[c]], writes=[R_gact[cb]])
    if stage == 3:
        dbg("gact", gact, [128, 4, T_OWN], R_gact[3])
        S.finish(); S.emit(); return nc, dbg_outs

    wglu = A.alloc([4, 512], BF16)
    R_wglu = Reg()
    S.dma("pool", wglu, w_glu.rearrange("(k p) n -> p k n", p=128), writes=[R_wglu])
    bglu = A.alloc([4], F32)
    S.dma("sp", bglu, bglu_col, writes=[R_wglu])
    ysq = [A.alloc([512], BF16) for _ in range(4)]
    R_ysq = [Reg() for _ in range(4)]
    sgt = [mt[2][0], mt[2][1]]
    R_sgt = [R_mt[2][0], R_mt[2][1]]
    ci = 0
    for tb in range(4):
        tsl = slice(tb * 512, (tb + 1) * 512)
        for cbo in range(4):
            b = bank()
            S.op("pe", MM([(PS[b], wglu[:, k, cbo * 128:(cbo + 1) * 128], gact[:, k, tsl], k == 0, k == 3) for k in range(4)]),
                 reads=[R_wglu] + R_gact, writes=[PR[b]])
            si = ci % 2
            ci += 1
            S.op("act", ACT(sgt[si], PS[b], AF.Sigmoid, bias=bglu[:, cbo:cbo + 1]), reads=[PR[b], R_wglu], writes=[R_sgt[si]])
            S.op("dve", TT(y2T[:, cbo, tsl], gact[:, cbo, tsl], sgt[si], ALU.mult),
                 reads=[R_sgt[si], R_gact[cbo]], writes=[R_y2T[cbo]])
            S.op("pool", TT(ysq[cbo], y2T[:, cbo, tsl], y2T[:, cbo, tsl], ALU.mult),
                 reads=[R_y2T[cbo]], writes=[R_ysq[cbo]])
        bss = bank()
        groups = []
        for tl in range(4):
            for cbo in range(4):
                groups.append((PS[bss][:, tl * 16:(tl + 1) * 16], ysq[cbo][:, tl * 128:(tl + 1) * 128], ones_col, cbo == 0, cbo == 3))
        S.op("pe", MM(groups), reads=R_ysq + [R_ones], writes=[PR[bss]])
        S.op("act", ACT(stat[:, 10:14], PS[bss][:, 0:64].rearrange("p (t c) -> p t c", c=16)[:, :, 0], AF.Sqrt, bias=eps_col, scale=1.0 / 512), reads=[PR[bss], R_eps], writes=[R_stat])
        S.op("dve", RECIP(rssm[:, tb * 4:tb * 4 + 4], stat[:, 10:14]), reads=[R_stat], writes=[R_rssm])
    if stage == 4:
        dbg("y2T", y2T, [128, 4, T_OWN], R_y2T[3])
        dbg("rssm", rssm, [128, NT], R_rssm)
        S.finish(); S.emit(); return nc, dbg_outs
    S.barrier()
    A.top = mark_D

    BR = 384
    NBLK = 54
    NTB = BR // 128
    NROWS = NBLK * BR
    xs_d = nc.dram_tensor("xs_scr", [NROWS, D], BF16, kind="Internal").ap()
    ys_d = nc.dram_tensor("ys_scr", [NROWS, D], F32, kind="Internal").ap()
    gates = A.alloc_top([NT, NE], F32)
    R_gates = [Reg() for _ in range(NT)]
    maskall = A.alloc_top([NT, NE], BF16)
    R_maskall = [Reg() for _ in range(NT)]
    dest4i = A.alloc_top([NT, 4], I32)
    gate4 = A.alloc_top([NT, 4], F32)
    widx = A.alloc_top([NBLK, 2], I32)
    bidx = A.alloc_top([NBLK], I32)
    eidx = A.alloc_top([NBLK], I32)
    R_meta = Reg()
    mark_top_meta = A.hi
    alloc_norm_bufs()
    h2tm = A.alloc([NT, D], BF16)
    R_h2tm = [Reg() for _ in range(NT)]
    h2Tt = [A.alloc([8, 128], BF16) for _ in range(2)]
    R_h2Tt = [Reg(), Reg()]
    wout = A.alloc([8, D], BF16)
    R_wout = Reg()
    w_out_v = w_out.rearrange("(k p) n -> p k n", p=128)
    S.dma("pool", wout[:, 0:4, :], w_out_v[:, 0:4, :], writes=[R_wout])
    S.dma("pool", wout[:, 4:8, :], w_out_v[:, 4:8, :], writes=[R_wout])
    gcat = A.alloc([8], F32)
    S.dma("sp", gcat, gcat_col, writes=[R_wout])
    for k in range(8):
        S.op("dve", TS(wout[:, k, :], wout[:, k, :], gcat[:, k:k + 1], None, ALU.mult), reads=[R_wout], writes=[R_wout])
    wr = A.alloc([8, NE], BF16)
    R_wr = Reg()
    S.dma("pool", wr, w_router.rearrange("(k p) n -> p k n", p=128), writes=[R_wr])
    brt = A.alloc([NE], F32)
    S.dma("sp", brt, b_router_rep, writes=[R_wr])
    ym = [A.alloc([D], F32) for _ in range(2)]
    R_ym = [Reg(), Reg()]
    xn = [A.alloc([D], F32) for _ in range(2)]
    R_xn = [Reg(), Reg()]
    lg = A.alloc([NE], F32)
    top8 = A.alloc([8], F32)
    eg = A.alloc([NE], F32)
    msk = A.alloc([NE], F32)
    R_lg = Reg()
    R_xmid = [Reg() for _ in range(NT)]

    def prenorm2_tile(xbuf, R_x, n):
        t1, R_t1 = W["t1"], W["R_t1"]
        S.op("act", ACT(sqj, xbuf, AF.Square, accum=stat[:, 0:1]), reads=[R_x], writes=[R_sqj, R_stat])
        S.op("act", ACT(stat[:, 1:2], stat[:, 0:1], AF.Sqrt, bias=eps_col, scale=1.0 / D), reads=[R_stat, R_eps], writes=[R_stat])
        S.op("dve", RECIP(stat[:, 2:3], stat[:, 1:2]), reads=[R_stat], writes=[R_stat])
        S.op("dve", STT(t1, xbuf, stat[:, 2:3], modsl(3), ALU.mult, ALU.mult), reads=[R_x, R_stat, R_mods[3]], writes=[R_t1])
        S.op("pool", TT(h2tm[:, n, :], t1, modsl(4), ALU.add), reads=[R_t1, R_mods[4]], writes=[R_h2tm[n]])
        b = bank()
        S.op("pe", TR([(PSB[b][:, k * 128:(k + 1) * 128], h2tm[:, n, k * 128:(k + 1) * 128]) for k in range(8)], ident),
             reads=[R_h2tm[n], R_ident], writes=[PR[b]])
        S.op("act", ACOPY(h2Tt[n % 2], PSB[b].rearrange("p (k c) -> p k c", k=8)), reads=[PR[b]], writes=[R_h2Tt[n % 2]])

    for n in range(NT):
        i2 = n % 2
        nsl = slice(n * 128, (n + 1) * 128)
        bA = [bank(), bank()]
        bS = [bank(), bank()]
        for half in range(2):
            hs = slice(half * 512, (half + 1) * 512)
            S.op("pe", MM([(PS[bA[half]], attnT[:, k, nsl], wout[:, k, hs], k == 0, k == 3) for k in range(4)]),
                 reads=[R_attnT[n], R_wout], writes=[PR[bA[half]]])
            S.op("pe", MM([(PS[bS[half]], y2T[:, k, nsl], wout[:, 4 + k, hs], k == 0, k == 3) for k in range(4)]),
                 reads=R_y2T + [R_wout], writes=[PR[bS[half]]])
        for half in range(2):
            hs = slice(half * 512, (half + 1) * 512)
            S.op("dve", TS(ym[i2][:, hs], PS[bA[half]], rattn[:, n:n + 1], None, ALU.mult), reads=[PR[bA[half]], R_rattn], writes=[R_ym[i2]])
            S.op("dve", STT(ym[i2][:, hs], PS[bS[half]], rssm[:, n:n + 1], ym[i2][:, hs], ALU.mult, ALU.add),
                 reads=[PR[bS[half]], R_rssm, R_ym[i2]], writes=[R_ym[i2]])
        xi = n % 2
        xtb, R_xtb = W["xt"][xi], W["R_xt"][xi]
        S.dma("sp", xtb, x_core[nsl, :], writes=[R_xtb])
        S.op("act", ACT(sqj, ym[i2], AF.Square, accum=stat[:, 16:17]), reads=[R_ym[i2]], writes=[R_sqj, R_stat])
        S.op("act", ACT(stat[:, 17:18], stat[:, 16:17], AF.Sqrt, bias=eps_col, scale=1.0 / D), reads=[R_stat, R_eps], writes=[R_stat])
        S.op("dve", RECIP(stat[:, 18:19], stat[:, 17:18]), reads=[R_stat], writes=[R_stat])
        S.op("dve", STT(ym[i2], ym[i2], stat[:, 18:19], modsl(2), ALU.mult, ALU.mult), reads=[R_ym[i2], R_stat, R_mods[2]], writes=[R_ym[i2]])
        S.op("pool", TT(xn[i2], ym[i2], xtb, ALU.add), reads=[R_ym[i2], R_xtb], writes=[R_xn[i2]])
        S.dma("sp", y_out[nsl, :], xn[i2], reads=[R_xn[i2]], writes=[R_xmid[n]])
        prenorm2_tile(xn[i2], R_xn[i2], n)
        b = bank()
        S.op("pe", MM([(PS[b][:, 0:NE], h2Tt[n % 2][:, k, :], wr[:, k, :], k == 0, k == 7) for k in range(8)]),
             reads=[R_h2Tt[n % 2], R_wr], writes=[PR[b]])
        S.op("dve", TT(lg, PS[b][:, 0:NE], brt, ALU.add), reads=[PR[b], R_wr], writes=[R_lg])
        S.op("dve", (lambda e: e.max(out=top8, in_=lg)), reads=[R_lg], writes=[R_lg])
        S.op("dve", TS(msk, lg, top8[:, 3:4], None, ALU.is_ge), reads=[R_lg], writes=[R_lg])
        S.op("dve", CP(maskall[:, n, :], msk), reads=[R_lg], writes=[R_maskall[n]])
        S.op("dve", TS(stat[:, 20:21], top8[:, 0:1], -1.0, None, ALU.mult), reads=[R_lg], writes=[R_stat])
        S.op("act", ACT(eg, lg, AF.Exp, bias=stat[:, 20:21]), reads=[R_lg, R_stat], writes=[R_lg])
        S.op("dve", TT(eg, eg, msk, ALU.mult), reads=[R_lg], writes=[R_lg])
        S.op("dve", (lambda e: e.reduce_sum(out=stat[:, 21:22], in_=eg, axis=mybir.AxisListType.X)), reads=[R_lg], writes=[R_stat])
        S.op("dve", RECIP(stat[:, 22:23], stat[:, 21:22]), reads=[R_stat], writes=[R_stat])
        S.op("dve", TS(gates[:, n, :], eg, stat[:, 22:23], None, ALU.mult), reads=[R_lg, R_stat], writes=[R_gates[n]])

    ones128 = A.alloc([128], BF16)
    ltri = A.alloc([128], BF16)
    R_cst = Reg()
    S.op("dve", MEMSET(ones128, 1.0), writes=[R_cst])
    S.dma("pool", ltri, ltri_in, writes=[R_cst])
    jb = A.alloc([NBLK], F32)
    kpi = A.alloc([2], F32)
    pio = A.alloc([1], F32)
    S.dma("sp", jb, jb512_in, writes=[R_cst])
    S.dma("sp", kpi, kp_iota_in, writes=[R_cst])
    S.dma("sp", pio, p_iota_in, writes=[R_cst])
    rank = A.alloc([NT, NE], F32)
    R_rank = Reg()
    for n in range(NT):
        b = bank()
        groups = [(PS[b][:, 0:NE], ones128, maskall[:, t, :], t == 0, False) for t in range(n)]
        groups.append((PS[b][:, 0:NE], ltri, maskall[:, n, :], n == 0, True))
        S.op("pe", MM(groups), reads=R_maskall[:n + 1] + [R_cst], writes=[PR[b]])
        S.op("act", ACOPY(rank[:, n, :], PS[b][:, 0:NE]), reads=[PR[b]], writes=[R_rank])
    cnt = A.alloc([NE], F32)
    b = bank()
    S.op("pe", MM([(PS[b][:, 0:NE], ones128, maskall[:, t, :], t == 0, t == NT - 1) for t in range(NT)]),
         reads=R_maskall + [R_cst], writes=[PR[b]])
    kint32 = A.alloc([NE], I32)
    padded = A.alloc([NE], F32)
    pend = A.alloc([NE], F32)
    pstart = A.alloc([NE], F32)
    ones32 = A.alloc([NE], F32)
    destm = A.alloc([NT, NE], F32)
    selt = A.alloc([NT, NE], F32)
    d8 = A.alloc([NT, 8], F32)
    cmpt = A.alloc([NBLK, NE], F32)
    ejf = A.alloc([NBLK], F32)
    R_m = Reg()
    S.op("dve", MEMSET(ones32, 1.0), writes=[R_m])
    S.op("dve", TS(kint32, PS[b][:, 0:NE], 1.0 / BR, (BR / 2 - 0.5) / BR, ALU.mult, ALU.add), reads=[PR[b]], writes=[R_m])
    S.op("dve", CP(cnt, kint32), reads=[R_m], writes=[R_m])
    S.op("dve", TS(padded, cnt, float(BR), None, ALU.mult), reads=[R_m], writes=[R_m])
    S.op("dve", SCAN(pend, ones32, padded, 0.0), reads=[R_m], writes=[R_m])
    S.op("dve", TT(pstart, pend, padded, ALU.subtract), reads=[R_m], writes=[R_m])
    pstart_bc = bass.AP(tensor=pstart.tensor, offset=pstart.offset, ap=[list(pstart.ap[0]), [0, NT], list(pstart.ap[1])])
    S.op("dve", TT(destm, rank, pstart_bc, ALU.add), reads=[R_m, R_rank], writes=[R_m])
    S.op("dve", STT(destm, destm, 1.0, maskall, ALU.add, ALU.mult), reads=[R_m] + R_maskall, writes=[R_m])
    S.op("dve", TS(destm, destm, -1.0, None, ALU.add), reads=[R_m], writes=[R_m])
    for n in range(NT):
        S.op("dve", (lambda e, n=n: e.max(out=d8[:, n, :], in_=destm[:, n, :])), reads=[R_m], writes=[R_m])
    S.op("dve", CP(dest4i, d8[:, :, 0:4]), reads=[R_m], writes=[R_meta])
    for k in range(4):
        S.op("dve", TT(selt, destm, bc_last(d8[:, :, k], NE), ALU.is_equal), reads=[R_m], writes=[R_m])
        S.op("dve", TT(selt, selt, gates, ALU.mult), reads=[R_m] + R_gates, writes=[R_m])
        S.op("dve", (lambda e, k=k: e.reduce_sum(out=gate4[:, :, k], in_=selt, axis=mybir.AxisListType.X)), reads=[R_m], writes=[R_meta])
    pend_bc = bass.AP(tensor=pend.tensor, offset=pend.offset, ap=[list(pend.ap[0]), [0, NBLK], list(pend.ap[1])])
    S.op("dve", TT(cmpt, pend_bc, bc_last(jb, NE), ALU.is_le), reads=[R_m, R_cst], writes=[R_m])
    S.op("dve", (lambda e: e.reduce_sum(out=ejf, in_=cmpt, axis=mybir.AxisListType.X)), reads=[R_m], writes=[R_m])
    S.op("dve", TS(ejf, ejf, float(NE - 1), None, ALU.min), reads=[R_m], writes=[R_m])
    kpi_bc = bass.AP(tensor=kpi.tensor, offset=kpi.offset, ap=[list(kpi.ap[0]), [0, NBLK], list(kpi.ap[1])])
    S.op("dve", STT(widx, bc_last(ejf, 2), 256.0, kpi_bc, ALU.mult, ALU.add), reads=[R_m, R_cst], writes=[R_meta])
    S.op("dve", STT(bidx, ejf, 128.0, pio[:, 0:1].to_broadcast([128, NBLK]), ALU.mult, ALU.add), reads=[R_m, R_cst], writes=[R_meta])
    S.op("dve", CP(eidx, ejf), reads=[R_m], writes=[R_meta])
    if stage == 5:
        dbg("gates", gates, [128, NT, NE], R_gates[15])
        dbg("dest4i", dest4i, [128, NT, 4], R_meta)
        dbg("gate4", gate4, [128, NT, 4], R_meta)
        dbg("eidx", eidx, [128, NBLK], R_meta)
        dbg("h2tm", h2tm, [128, NT, D], R_h2tm[15])
        S.finish(); S.emit(); return nc, dbg_outs
    bg_step(1000)
    for n in range(NT):
        for k in range(4):
            S.idma("pool", xs_d, bass.IndirectOffsetOnAxis(ap=dest4i[:, n, k:k + 1], axis=0), h2tm[:, n, :], None, NROWS - 1,
                   reads=[R_meta, R_h2tm[n]], writes=[])
    S.barrier()
    A.top = mark_core

    wgu = [A.alloc([8, 2 * D], BF16) for _ in range(2)]
    wdn = [A.alloc([8, D], BF16) for _ in range(2)]
    R_wgu = [[Reg() for _ in range(2)] for _ in range(2)]
    R_wdn = [[Reg()], [Reg()]]
    bgub = [A.alloc([16], F32) for _ in range(2)]
    bgub1 = [A.alloc([8], F32) for _ in range(2)]
    bdb = [A.alloc([D], F32) for _ in range(2)]
    R_bb = [Reg(), Reg()]
    R_bd = [Reg(), Reg()]
    xb = [A.alloc([NTB, D], BF16) for _ in range(2)]
    R_xb = [Reg(), Reg()]
    xT = [A.alloc([8, BR], BF16) for _ in range(2)]
    R_xT = [Reg(), Reg()]
    actT = A.alloc([8, BR], BF16)
    R_actT = Reg()
    ea = [A.alloc([BR], F32) for _ in range(2)]
    es_ = [A.alloc([BR], BF16) for _ in range(2)]
    eb = [A.alloc([BR], F32) for _ in range(2)]
    R_e = [Reg(), Reg()]
    ysb = [A.alloc([D], F32) for _ in range(2)]
    R_ysb = [Reg(), Reg()]
    ysct = 0

    def load_blk(j):
        bi = j % 2
        for kh in range(2):
            S.idma("pool", wgu[bi][:, kh * 4:(kh + 1) * 4, :].rearrange("p a b -> p (a b)"), None, wgu_bf,
                   bass.IndirectOffsetOnAxis(ap=widx[:, j, kh:kh + 1], axis=0), None, reads=[R_meta], writes=[R_wgu[bi][kh]])
        S.idma("pool", bgub[bi], None, bgu_rows_in, bass.IndirectOffsetOnAxis(ap=bidx[:, j:j + 1], axis=0), NE * 128 - 1,
               reads=[R_meta], writes=[R_bb[bi]])
        S.idma("pool", bdb[bi], None, b_down, bass.IndirectOffsetOnAxis(ap=eidx[:, j:j + 1], axis=0), NE - 1,
               reads=[R_meta], writes=[R_bd[bi]])
        S.op("dve", TS(bgub1[bi], bgub[bi][:, 8:16], 1.0, None, ALU.add), reads=[R_bb[bi]], writes=[R_bb[bi]])
        S.dma("sp", xb[bi], xs_d[j * BR:(j + 1) * BR, :].rearrange("(a p) c -> p a c", p=128), writes=[R_xb[bi]])

    def load_wdn(j):
        S.idma("pool", wdn[j % 2].rearrange("p a b -> p (a b)"), None, wdn_bf, bass.IndirectOffsetOnAxis(ap=bidx[:, j:j + 1], axis=0), None,
               reads=[R_meta], writes=[R_wdn[j % 2][0]])

    nblk_run = NBLK if stage >= 7 else 3
    load_blk(0)
    load_wdn(0)
    for j in range(nblk_run):
        bi = j % 2
        if j + 1 < nblk_run:
            load_blk(j + 1)
            load_wdn(j + 1)
        for a in range(NTB):
            b = bank()
            S.op("pe", TR([(PSB[b][:, k * 128:(k + 1) * 128], xb[bi][:, a, k * 128:(k + 1) * 128]) for k in range(8)], ident),
                 reads=[R_xb[bi], R_ident], writes=[PR[b]])
            S.op("act", ACOPY(xT[bi][:, :, a * 128:(a + 1) * 128], PSB[b].rearrange("p (k c) -> p k c", k=8)), reads=[PR[b]], writes=[R_xT[bi]])
        for fb in range(8):
            bg, bl = bank(), bank()
            S.op("pe", MM([(PS[bg][:, :BR], wgu[bi][:, k, fb * 128:(fb + 1) * 128], xT[bi][:, k, :], k == 0, k == 7) for k in range(8)]),
                 reads=R_wgu[bi] + [R_xT[bi]], writes=[PR[bg]])
            S.op("pe", MM([(PS[bl][:, :BR], wgu[bi][:, k, D + fb * 128:D + (fb + 1) * 128], xT[bi][:, k, :], k == 0, k == 7) for k in range(8)]),
                 reads=R_wgu[bi] + [R_xT[bi]], writes=[PR[bl]])
            wi = fb % 2
            S.op("dve", TS(ea[wi], PS[bg][:, :BR], bgub[bi][:, fb:fb + 1], 7.0, ALU.add, ALU.min), reads=[PR[bg], R_bb[bi]], writes=[R_e[wi]])
            S.op("act", ACT(es_[wi], ea[wi], AF.Sigmoid, scale=1.702), reads=[R_e[wi]], writes=[R_e[wi]])
            S.op("dve", TS(eb[wi], PS[bl][:, :BR], bgub1[bi][:, fb:fb + 1], 8.0, ALU.add, ALU.min), reads=[PR[bl], R_bb[bi]], writes=[R_e[wi]])
            S.op("dve", STT(eb[wi], eb[wi], -6.0, ea[wi], ALU.max, ALU.mult), reads=[R_e[wi]], writes=[R_e[wi]])
            S.op("dve", TT(actT[:, fb, :], eb[wi], es_[wi], ALU.mult), reads=[R_e[wi]], writes=[R_actT])
        for a in range(NTB):
            yi = ysct % 2
            ysct += 1
            for half in range(2):
                hs = slice(half * 512, (half + 1) * 512)
                b = bank()
                S.op("pe", MM([(PS[b], actT[:, fb, a * 128:(a + 1) * 128], wdn[bi][:, fb, hs], fb == 0, fb == 7) for fb in range(8)]),
                     reads=[R_actT] + R_wdn[bi], writes=[PR[b]])
                S.op("dve", TT(ysb[yi][:, hs], PS[b], bdb[bi][:, hs], ALU.add), reads=[PR[b], R_bd[bi]], writes=[R_ysb[yi]])
            r0 = j * BR + a * 128
            S.dma("sp", ys_d[r0:r0 + 128, :], ysb[yi], reads=[R_ysb[yi]])
    S.barrier()
    A.top = mark_core

    yk = [A.alloc([D], F32) for _ in range(4)]
    R_yk = [Reg() for _ in range(4)]
    acc = [A.alloc([D], F32) for _ in range(2)]
    R_acc = [Reg(), Reg()]
    xf = [A.alloc([D], F32) for _ in range(2)]
    R_xf = [Reg(), Reg()]
    kc = 0
    for n in range(NT):
        nsl = slice(n * 128, (n + 1) * 128)
        ai = n % 2
        S.dma("sp", xf[ai], y_out[nsl, :], reads=[R_xmid[n]], writes=[R_xf[ai]])
        for k in range(4):
            ki = kc % 4
            kc += 1
            S.idma("pool", yk[ki], None, ys_d, bass.IndirectOffsetOnAxis(ap=dest4i[:, n, k:k + 1], axis=0), NROWS - 1,
                   reads=[R_meta], writes=[R_yk[ki]])
            if k == 0:
                S.op("dve", TS(acc[ai], yk[ki], gate4[:, n, k:k + 1], None, ALU.mult), reads=[R_yk[ki], R_meta], writes=[R_acc[ai]])
            else:
                S.op("dve", STT(acc[ai], yk[ki], gate4[:, n, k:k + 1], acc[ai], ALU.mult, ALU.add), reads=[R_yk[ki], R_meta, R_acc[ai]], writes=[R_acc[ai]])
        S.op("act", ACT(sqj, acc[ai], AF.Square, accum=stat[:, 24:25]), reads=[R_acc[ai]], writes=[R_sqj, R_stat])
        S.op("act", ACT(stat[:, 25:26], stat[:, 24:25], AF.Sqrt, bias=eps_col, scale=1.0 / D), reads=[R_stat, R_eps], writes=[R_stat])
        S.op("dve", RECIP(stat[:, 26:27], stat[:, 25:26]), reads=[R_stat], writes=[R_stat])
        S.op("dve", STT(acc[ai], acc[ai], stat[:, 26:27], gtg2, ALU.mult, ALU.mult), reads=[R_acc[ai], R_stat, R_mods[5]], writes=[R_acc[ai]])
        S.op("pool", TT(acc[ai], acc[ai], xf[ai], ALU.add), reads=[R_acc[ai], R_xf[ai]], writes=[R_acc[ai]])
        S.dma("sp", y_out[nsl, :], acc[ai], reads=[R_acc[ai]], writes=[R_xmid[n]])
    S.finish()
    S.emit()
    return nc, dbg_outs


def _prep_inputs(inp):
    f = lambda a: np.ascontiguousarray(np.asarray(a, dtype=np.float32))
    x = f(inp["x"]); c = f(inp["c"])
    L = 0
    w_in = f(inp["w_in"][L])
    q = w_in[:, 0:512].reshape(1024, 8, 64)
    qsw = np.concatenate([q[:, :, 32:], q[:, :, :32]], axis=2).reshape(1024, 512)
    k = w_in[:, 512:640].reshape(1024, 2, 64)
    ksw = np.concatenate([k[:, :, 32:], k[:, :, :32]], axis=2)
    kdup = np.concatenate([k[:, 0], k[:, 0], k[:, 1], k[:, 1]], axis=1)
    kswdup = np.concatenate([ksw[:, 0], ksw[:, 0], ksw[:, 1], ksw[:, 1]], axis=1)
    w_ext = f(np.concatenate([w_in[:, 0:512], qsw, kdup, kswdup, w_in[:, 640:768], w_in[:, 768:1280]], axis=1))
    assert w_ext.shape == (1024, WEXT)
    rep = lambda v: f(np.broadcast_to(np.asarray(v, np.float32).reshape(1, -1), (128, np.asarray(v).size)))
    col = lambda v, nk: f(np.asarray(v, np.float32).reshape(nk, 128).T)
    gvecs = f(np.stack([rep(inp["g_pre_mix"][L]), rep(inp["g_post_mix"][L]), rep(inp["g_pre_ffn"][L]), rep(inp["g_post_ffn"][L])]))
    a = np.arange(128)[:, None]; b = np.arange(128)[None, :]
    m_prev = np.where(a <= b, 0.0, -30000.0)
    m_next = np.where(b <= a, 0.0, -30000.0)
    mask_mid = f(np.concatenate([m_prev, np.zeros((128, 128)), m_next], axis=1))
    inv_freq = (np.float32(10000.0) ** (-np.arange(32, dtype=np.float32) * np.float32(2.0) / np.float32(64))).astype(np.float32)
    shared = dict(
        w_ada=f(inp["w_ada"][L]), b_ada_rep=rep(inp["b_ada"][L]), gvecs=gvecs, w_ext=w_ext,
        sink_rep=rep(inp["attn_sink"][L]), mask_mid=mask_mid, ident=f(np.eye(128)),
        iota1=rep(np.arange(1, CH + 1)), dskip_col=col(inp["ssm_d"][L], 4),
        w_glu=f(inp["w_glu"][L]), bglu_col=col(inp["b_glu"][L], 4),
        gcat_col=col(np.concatenate([inp["g_attn_out"][L], inp["g_ssm_out"][L]]), 8),
        w_out=f(inp["w_out"][L]), w_router=f(inp["w_router"][L]), b_router_rep=rep(inp["b_router"][L]),
        wgu_rows=f(np.asarray(inp["w_gate_up"][L], np.float32).reshape(32, 8, 128, 2048).transpose(0, 2, 1, 3).reshape(32 * 128 * 2, 4 * 2048)),
        wdn_rows=f(np.asarray(inp["w_down"][L], np.float32).reshape(32, 8, 128, 1024).transpose(0, 2, 1, 3).reshape(32 * 128, 8 * 1024)),
        b_down=f(inp["b_down"][L]),
        bgu_rows=f(np.asarray(inp["b_gate_up"][L], np.float32).reshape(32, 16, 128).transpose(0, 2, 1).reshape(32 * 128, 16)),
        ltri=f(np.triu(np.ones((128, 128)), 1)),
        jb512=rep(np.arange(54) * 384.0),
        kp_iota=f(np.arange(2)[None, :] * 1.0 + 2.0 * np.arange(128)[:, None]),
        p_iota=f(np.arange(128).reshape(128, 1)),
    )
    are, aim, ldt = f(inp["ssm_a_re"][L]), f(inp["ssm_a_im"][L]), f(inp["ssm_log_dt"][L])
    bre, bim, cre, cim = f(inp["ssm_b_re"][L]), f(inp["ssm_b_im"][L]), f(inp["ssm_c_re"][L]), f(inp["ssm_c_im"][L])
    ssm_by_flip = {}
    for flip in (0, 1):
        dd = [1, 0] if flip else [0, 1]
        sc = {}
        for nm, arr in (("a_re_p", are), ("a_im_p", aim)):
            v = arr[dd].reshape(2, 16, 2, 64)
            sc[nm] = f(v.transpose(2, 3, 0, 1).reshape(128, 32))
        v = np.broadcast_to(ldt[dd].reshape(2, 16, 2, 1), (2, 16, 2, 64))
        sc["logdt_p"] = f(v.transpose(2, 3, 0, 1).reshape(128, 32))
        for nm, arr, is_c in (("bpad_re", bre, False), ("bpad_im", bim, False), ("cpad_re", cre, True), ("cpad_im", cim, True)):
            out = np.zeros((2, 16, 128, 128), np.float32)
            for d in range(2):
                for G in range(16):
                    for g2 in range(2):
                        g = 2 * G + g2
                        blk = arr[dd[d], g].T if is_c else arr[dd[d], g]
                        c0 = (G % 4) * 32 + g2 * 16
                        out[d, G, g2 * 64:(g2 + 1) * 64, c0:c0 + 16] = blk
            sc[nm] = out.reshape(32, 128, 128)
        ssm_by_flip[flip] = sc
    in_maps = []
    for core in range(8):
        bidx, hf = core // 2, core % 2
        seq = x[bidx]
        pos = np.arange(4096, dtype=np.float32)
        if hf == 1:
            seq = seq[::-1]
            pos = pos[::-1]
        pos = pos[:2176]
        ang = (pos[:, None] * inv_freq[None, :]).astype(np.float32)
        cs, sn = np.cos(ang).astype(np.float32), np.sin(ang).astype(np.float32)
        cos64 = np.concatenate([cs, cs], axis=1).T
        sin64 = np.concatenate([-sn, sn], axis=1).T
        m = dict(shared)
        m.update(ssm_by_flip[hf])
        m["x_core"] = f(seq)
        m["c_col"] = col(c[bidx], 8)
        m["rope_cos"] = f(np.concatenate([cos64, cos64], axis=0))
        m["rope_sin"] = f(np.concatenate([sin64, sin64], axis=0))
        in_maps.append(m)
    return in_maps


_CACHE = {}


def kernel(**inputs):
    in_maps = _prep_inputs(inputs)
    if "nc" not in _CACHE:
        _CACHE["nc"] = build()[0]
    res = run_bass_kernel_spmd(_CACHE["nc"], in_maps, core_ids=list(range(8)))
    out = np.zeros((4, 4096, 1024), np.float32)
    for core in range(8):
        bidx, hf = core // 2, core % 2
        y = np.asarray(res.results[core]["y_out"], np.float32)
        if hf == 0:
            out[bidx, :2048] = y
        else:
            out[bidx, 2048:] = y[::-1]
    return out
```

```python
import math
import os
import numpy as np
import concourse.bass as bass
import concourse.mybir as mybir
from concourse.bass_utils import run_bass_kernel_spmd
from contextlib import ExitStack

F32 = mybir.dt.float32
BF16 = mybir.dt.bfloat16
U8 = mybir.dt.uint8
I32 = mybir.dt.int32
ALU = mybir.AluOpType
AF = mybir.ActivationFunctionType
PI = math.pi

D = 1024
T_OWN = 2048
NT = 16
NE = 32
WEXT = 2176
QO, QSO, KO, KSO, VO, UO = 0, 512, 1024, 1280, 1536, 1664
CH = 512


class Reg:
    __slots__ = ("lw", "rd", "name")

    def __init__(self, name=""):
        self.lw = None
        self.rd = {}
        self.name = name


class Sched:
    CE = ("pe", "act", "dve", "pool")

    def __init__(self, nc, es, nslots=8):
        self.nc = nc
        self.sem, self.cnt, self.prog, self.waited = {}, {}, {}, {}
        for e in ("pe", "act", "dve", "pool", "sp"):
            self.prog[e] = []
            self.waited[e] = {}
        for e in self.CE:
            self.sem[e] = es.enter_context(nc.semaphore("sem_" + e))
            self.cnt[e] = 0
        self.ns = nslots
        self.dcount, self.dnext = {}, {}
        for q in ("sp", "pool", "bg"):
            self.dnext[q] = 0
            for s in range(nslots):
                k = ("d", q, s)
                self.sem[k] = es.enter_context(nc.semaphore("sd_%s_%d" % (q, s)))
                self.dcount[k] = 0

    def _deps(self, eng, reads, writes, skip_same=True):
        need = {}
        raw_same = 0

        def add(tok):
            if tok is None:
                return
            k, v = tok
            if need.get(k, 0) < v:
                need[k] = v

        for r in reads:
            add(r.lw)
            if r.lw is not None and r.lw[0] == eng and r.lw[1] > raw_same:
                raw_same = r.lw[1]
        for w in writes:
            add(w.lw)
            if w.lw is not None and w.lw[0] == eng and w.lw[1] > raw_same:
                raw_same = w.lw[1]
            if w.rd.get(eng, 0) > raw_same:
                raw_same = w.rd[eng]
            for k, v in w.rd.items():
                add((k, v))
        waits = []
        for k, v in need.items():
            if skip_same and k == eng:
                if eng == "pe" or raw_same == 0:
                    continue
                v = raw_same
            if self.waited[eng].get(k, 0) >= v:
                continue
            self.waited[eng][k] = v
            waits.append((k, v))
        return waits

    def op(self, eng, fn, reads=(), writes=()):
        waits = self._deps(eng, reads, writes)
        self.cnt[eng] += 1
        tok = (eng, self.cnt[eng])
        self.prog[eng].append((waits, fn, self.sem[eng], 1))
        for r in reads:
            if r.rd.get(eng, 0) < tok[1]:
                r.rd[eng] = tok[1]
        for w in writes:
            w.lw = tok
            w.rd = {}
        return tok

    def dma(self, q, out_ap, in_ap, reads=(), writes=(), ring=None):
        waits = self._deps(q, reads, writes, skip_same=False)
        ring = ring or q
        s = self.dnext[ring] % self.ns
        self.dnext[ring] += 1
        k = ("d", ring, s)
        c = self.dcount[k]
        if c > 0 and self.waited[q].get(k, 0) < 16 * c:
            self.waited[q][k] = 16 * c
            waits.append((k, 16 * c))
        self.dcount[k] = c + 1
        tok = (k, 16 * (c + 1))
        self.prog[q].append((waits, (lambda e: e.dma_start(out=out_ap, in_=in_ap)), self.sem[k], 16))
        for r in reads:
            if r.rd.get(k, 0) < tok[1]:
                r.rd[k] = tok[1]
        for w in writes:
            w.lw = tok
            w.rd = {}
        return tok

    def idma(self, q, out_ap, out_off, in_ap, in_off, bounds, reads=(), writes=()):
        waits = self._deps(q, reads, writes, skip_same=False)
        s = self.dnext[q] % self.ns
        self.dnext[q] += 1
        k = ("d", q, s)
        c = self.dcount[k]
        if c > 0 and self.waited[q].get(k, 0) < 16 * c:
            self.waited[q][k] = 16 * c
            waits.append((k, 16 * c))
        self.dcount[k] = c + 1
        tok = (k, 16 * (c + 1))
        self.prog[q].append((waits, (lambda e: e.indirect_dma_start(out=out_ap, out_offset=out_off, in_=in_ap, in_offset=in_off,
                                                                    bounds_check=None)), self.sem[k], 16))
        for r in reads:
            if r.rd.get(k, 0) < tok[1]:
                r.rd[k] = tok[1]
        for w in writes:
            w.lw = tok
            w.rd = {}
        return tok

    def barrier(self):
        cur = {e: self.cnt[e] for e in self.CE}
        for k, c in self.dcount.items():
            cur[k] = 16 * c
        for e in ("pe", "act", "dve", "pool", "sp"):
            waits = []
            for k, v in cur.items():
                if k == e or v == 0:
                    continue
                if self.waited[e].get(k, 0) >= v:
                    continue
                self.waited[e][k] = v
                waits.append((k, v))
            self.prog[e].append((waits, None, None, 0))

    def finish(self):
        waits = []
        for k, c in self.dcount.items():
            if c > 0:
                waits.append((k, 16 * c))
        self.prog["sp"].append((waits, None, None, 0))

    def emit(self):
        nc = self.nc

        def run(name):
            def f(e):
                for waits, fn, sem, inc in self.prog[name]:
                    for k, v in waits:
                        e.wait_ge(self.sem[k], v)
                    if fn is not None:
                        ins = fn(e)
                        ins.then_inc(sem, inc)
            return f

        with nc.Block() as block:
            block.tensor(run("pe"))
            block.scalar(run("act"))
            block.vector(run("dve"))
            block.gpsimd(run("pool"))
            block.sync(run("sp"))


DTS = {F32: 4, BF16: 2, U8: 1, I32: 4}


class Arena:
    def __init__(self, nc, nbytes):
        self.t = nc.alloc_sbuf_tensor("arena", [128, nbytes], U8)
        self.top = 0
        self.n = nbytes
        self.peak = 0
        self.hi = nbytes

    def alloc_top(self, free_shape, dt):
        size = int(np.prod(free_shape)) * DTS[dt]
        off = (self.hi - size) // 32 * 32
        self.hi = off
        ap = self.t[:, off:off + size].bitcast(dt)
        if len(free_shape) == 2:
            ap = ap.rearrange("p (a b) -> p a b", a=free_shape[0])
        return ap

    def alloc(self, free_shape, dt):
        size = int(np.prod(free_shape)) * DTS[dt]
        off = (self.top + 31) // 32 * 32
        self.top = off + size
        self.peak = max(self.peak, self.top)
        assert self.top <= self.hi, ("SBUF arena overflow", self.top, self.hi)
        ap = self.t[:, off:off + size].bitcast(dt)
        if len(free_shape) == 2:
            ap = ap.rearrange("p (a b) -> p a b", a=free_shape[0])
        elif len(free_shape) == 3:
            ap = ap.rearrange("p (a b c) -> p a b c", a=free_shape[0], b=free_shape[1])
        elif len(free_shape) == 4:
            ap = ap.rearrange("p (a b c d) -> p a b c d", a=free_shape[0], b=free_shape[1], c=free_shape[2])
        return ap


def bc_last(ap, m):
    return bass.AP(tensor=ap.tensor, offset=ap.offset, ap=[list(x) for x in ap.ap] + [[0, m]])


def rev(ap):
    dims = [list(x) for x in ap.ap]
    st, n = dims[-1]
    dims[-1] = [-st, n]
    return bass.AP(tensor=ap.tensor, offset=ap.offset + st * (n - 1), ap=dims)


def build(stage=99):
    nc = bass.Bass("TRN2", target_bir_lowering=False)
    es = ExitStack()

    def din(name, shape, dt=F32):
        return nc.dram_tensor(name, list(shape), dt, kind="ExternalInput").ap()

    x_core = din("x_core", [4096, D])
    c_col = din("c_col", [128, 8])
    w_ada = din("w_ada", [D, 6 * D])
    b_ada_rep = din("b_ada_rep", [128, 6 * D])
    gvecs = din("gvecs", [4, 128, D])
    w_ext = din("w_ext", [D, WEXT])
    rope_cos = din("rope_cos", [128, 2176])
    rope_sin = din("rope_sin", [128, 2176])
    sink_rep = din("sink_rep", [128, 8])
    mask_mid = din("mask_mid", [128, 384])
    ident_in = din("ident", [128, 128])
    iota1 = din("iota1", [128, CH])
    a_re_p = din("a_re_p", [128, 32])
    a_im_p = din("a_im_p", [128, 32])
    logdt_p = din("logdt_p", [128, 32])
    bpad_re = din("bpad_re", [32, 128, 128])
    bpad_im = din("bpad_im", [32, 128, 128])
    cpad_re = din("cpad_re", [32, 128, 128])
    cpad_im = din("cpad_im", [32, 128, 128])
    dskip_col = din("dskip_col", [128, 4])
    w_glu = din("w_glu", [512, 512])
    bglu_col = din("bglu_col", [128, 4])
    gcat_col = din("gcat_col", [128, 8])
    w_out = din("w_out", [D, D])
    w_router = din("w_router", [D, NE])
    b_router_rep = din("b_router_rep", [128, NE])
    wgu_rows = din("wgu_rows", [NE * 128 * 2, 4 * 2 * D])
    bgu_rows_in = din("bgu_rows", [NE * 128, 16])
    ltri_in = din("ltri", [128, 128])
    jb512_in = din("jb512", [128, 64])
    kp_iota_in = din("kp_iota", [128, 2])
    p_iota_in = din("p_iota", [128, 1])
    wdn_rows = din("wdn_rows", [NE * 128, 8 * D])
    b_down = din("b_down", [NE, D])
    y_out = nc.dram_tensor("y_out", [T_OWN, D], F32, kind="ExternalOutput").ap()
    dbg_outs = {}

    S = Sched(nc, es, nslots=12)
    A = Arena(nc, 204000)
    pst = [nc.alloc_psum_tensor("ps%d" % i, [128, 512], F32) for i in range(8)]
    PS = [t[:, :] for t in pst]
    PSB = [t[:, :].bitcast(BF16) for t in pst]
    PR = [Reg("ps%d" % i) for i in range(8)]
    bank_ctr = [0]

    def bank():
        i = bank_ctr[0] % 8
        bank_ctr[0] += 1
        return i

    def dbg(name, ap, shape, reg):
        t = nc.dram_tensor("dbg_" + name, list(shape), ap.dtype, kind="ExternalOutput").ap()
        dbg_outs[name] = t
        S.dma("sp", t, ap, reads=[reg])

    def ACT(out, in_, func, bias=None, scale=None, accum=None):
        kw = {}
        if bias is not None:
            kw["bias"] = bias
        if scale is not None:
            kw["scale"] = scale
        if accum is not None:
            kw["accum_out"] = accum
        return lambda e: e.activation(out=out, in_=in_, func=func, **kw)

    def TS(out, in0, s1, s2, op0, op1=None, accum=None):
        kw = {}
        if op1 is not None:
            kw["op1"] = op1
        if accum is not None:
            kw["accum_out"] = accum
        return lambda e: e.tensor_scalar(out=out, in0=in0, scalar1=s1, scalar2=s2, op0=op0, **kw)

    def TT(out, in0, in1, op):
        return lambda e: e.tensor_tensor(out=out, in0=in0, in1=in1, op=op)

    def STT(out, in0, sc, in1, op0, op1, accum=None):
        kw = {}
        if accum is not None:
            kw["accum_out"] = accum
        return lambda e: e.scalar_tensor_tensor(out=out, in0=in0, scalar=sc, in1=in1, op0=op0, op1=op1, **kw)

    def CP(out, in_):
        return lambda e: e.tensor_copy(out=out, in_=in_)

    def MM(groups):
        def f(e):
            ins = None
            for (o, l, r, st, sp_) in groups:
                ins = e.matmul(o, lhsT=l, rhs=r, start=st, stop=sp_)
            return ins
        return f

    def TR(pairs, ident):
        def f(e):
            ins = None
            for (o, i) in pairs:
                ins = e.transpose(o, i, ident)
            return ins
        return f


    def ACOPY(out, in_):
        return lambda e: e.copy(out=out, in_=in_)

    def AMUL(out, in_, m):
        return lambda e: e.mul(out=out, in_=in_, mul=m)

    def RECIP(out, in_):
        return lambda e: e.reciprocal(out=out, in_=in_)

    def MEMSET(ap, v):
        return lambda e: e.memset(ap, v)

    def SCAN(out, d0, d1, init):
        return lambda e: e.tensor_tensor_scan(out=out, data0=d0, data1=d1, initial=init, op0=ALU.mult, op1=ALU.add)

    ident = A.alloc([128], BF16)
    R_ident = Reg()
    S.dma("pool", ident, ident_in, writes=[R_ident])
    ones_col = A.alloc([16], BF16)
    R_ones = Reg()
    S.op("dve", MEMSET(ones_col, 1.0), writes=[R_ones])

    eps_col = A.alloc([1], F32)
    R_eps = Reg()
    S.op("dve", MEMSET(eps_col, 1e-6), writes=[R_eps])
    negpi = A.alloc([1], F32)
    S.op("dve", MEMSET(negpi, -PI), writes=[R_eps])
    stat = A.alloc([64], F32)
    R_stat = Reg()
    sqj = A.alloc([D], BF16)
    R_sqj = Reg()
    gtg2 = A.alloc([D], F32)
    mark_core = A.top
    mods5 = A.alloc([5, D], F32)
    R_mods = [Reg("mods%d" % i) for i in range(6)]

    def modsl(slot):
        return gtg2 if slot == 5 else mods5[:, slot, :]

    def modsl_s(slot, sl):
        return gtg2[:, sl] if slot == 5 else mods5[:, slot, sl]
    SEGMAP = {0: 1, 1: 0, 2: 2, 3: 4, 4: 3, 5: 5}
    mark_persist = A.top

    ccol = A.alloc([8], F32)
    R_cc = Reg()
    S.dma("sp", ccol, c_col, writes=[R_cc])
    csil = A.alloc([8], F32)
    R_cs = Reg()
    S.op("act", ACT(csil, ccol, AF.Silu), reads=[R_cc], writes=[R_cs])
    cl = A.alloc([8, 128], BF16)
    R_cl = Reg()
    for k in range(8):
        S.op("dve", CP(cl[:, k, :], csil[:, k:k + 1].to_broadcast([128, 128])), reads=[R_cs], writes=[R_cl])
    wada_buf = [A.alloc([8, 512], BF16) for _ in range(2)]
    R_wada = [Reg(), Reg()]
    bada_buf = [A.alloc([512], F32) for _ in range(2)]
    R_bada = [Reg(), Reg()]
    w_ada_v = w_ada.rearrange("(k p) n -> p k n", p=128)
    for j in range(12):
        bi = j % 2
        S.dma("pool", wada_buf[bi], w_ada_v[:, :, j * 512:(j + 1) * 512], writes=[R_wada[bi]])
        S.dma("sp", bada_buf[bi], b_ada_rep[:, j * 512:(j + 1) * 512], writes=[R_bada[bi]])
        b = bank()
        S.op("pe", MM([(PS[b], cl[:, k, :], wada_buf[bi][:, k, :], k == 0, k == 7) for k in range(8)]),
             reads=[R_cl, R_wada[bi]], writes=[PR[b]])
        slot = SEGMAP[j // 2]
        S.op("dve", TT(modsl_s(slot, slice((j % 2) * 512, (j % 2) * 512 + 512)), PS[b], bada_buf[bi], ALU.add),
             reads=[PR[b], R_bada[bi]], writes=[R_mods[slot]])
    gtmp = A.alloc([D], F32)
    R_gt = Reg()
    for (gi, slot, plus1) in ((0, 0, True), (1, 2, False), (2, 3, True), (3, 5, False)):
        S.dma("sp", gtmp, gvecs[gi], writes=[R_gt])
        if plus1:
            S.op("dve", STT(modsl(slot), modsl(slot), 1.0, gtmp, ALU.add, ALU.mult),
                 reads=[R_gt, R_mods[slot]], writes=[R_mods[slot]])
        else:
            S.op("dve", TT(modsl(slot), modsl(slot), gtmp, ALU.mult),
                 reads=[R_gt, R_mods[slot]], writes=[R_mods[slot]])
    if stage == 0:
        dbg("mods", mods5, [128, 5, D], R_mods[0])
        dbg("gtg2", gtg2, [128, D], R_mods[5])
        S.finish(); S.emit(); return nc, dbg_outs
    S.barrier()
    A.top = mark_persist

    wgu_bf = nc.dram_tensor("wgu_bf", [NE * 256, 8192], BF16, kind="Internal").ap()
    wdn_bf = nc.dram_tensor("wdn_bf", [NE * 128, 8192], BF16, kind="Internal").ap()
    bg_list = []
    for e_ in range(NE):
        bg_list.append((wgu_bf[e_ * 256:e_ * 256 + 128, :], wgu_rows[e_ * 256:e_ * 256 + 128, :]))
        bg_list.append((wgu_bf[e_ * 256 + 128:e_ * 256 + 256, :], wgu_rows[e_ * 256 + 128:e_ * 256 + 256, :]))
        bg_list.append((wdn_bf[e_ * 128:(e_ + 1) * 128, :], wdn_rows[e_ * 128:(e_ + 1) * 128, :]))
    bg_pos = [0]

    def bg_step(n=1):
        for _ in range(n):
            if bg_pos[0] < len(bg_list):
                o_, i_ = bg_list[bg_pos[0]]
                bg_pos[0] += 1
                S.dma("pool", o_, i_, ring="bg")

    uT = A.alloc([4, 4096], BF16)
    R_u = [Reg() for _ in range(8)]
    attnT = A.alloc([4, T_OWN], BF16)
    R_attnT = [Reg() for _ in range(NT)]
    rattn = A.alloc([NT], F32)
    R_rattn = Reg()
    mark_B = A.top
    qT = A.alloc([4, T_OWN], BF16)
    kT = A.alloc([2, 2176], BF16)
    vaug = A.alloc([17, 2, 65], BF16)
    maskb = A.alloc([384], BF16)
    R_mask = Reg()
    S.dma("pool", maskb, mask_mid, writes=[R_mask])
    R_q = [Reg() for _ in range(4)]
    R_k = [Reg() for _ in range(5)]
    R_v = [Reg() for _ in range(17)]
    S.op("pool", MEMSET(vaug, 1.0), writes=R_v)
    mark_C = A.top

    W = {}

    def alloc_norm_bufs():
        W["xt"] = [A.alloc([D], F32) for _ in range(2)]
        W["R_xt"] = [Reg(), Reg()]
        W["t1"] = A.alloc([D], F32)
        W["R_t1"] = Reg()
        W["hb"] = [A.alloc([D], BF16) for _ in range(2)]
        W["R_hb"] = [Reg(), Reg()]

    alloc_norm_bufs()
    wext = A.alloc([8, WEXT], BF16)
    R_wext = Reg()
    w_ext_v = w_ext.rearrange("(k p) n -> p k n", p=128)
    for k in range(0, 8, 2):
        S.dma("pool", wext[:, k:k + 2, :], w_ext_v[:, k:k + 2, :], writes=[R_wext])
    rcos = A.alloc([2176], BF16)
    rsin = A.alloc([2176], BF16)
    R_rope = Reg()
    S.dma("pool", rcos, rope_cos, writes=[R_rope])
    S.dma("pool", rsin, rope_sin, writes=[R_rope])
    hT = [A.alloc([8, 512], BF16) for _ in range(2)]
    R_hT = [Reg(), Reg()]
    ropet = [A.alloc([512], F32) for _ in range(4)]
    R_ropet = [Reg() for _ in range(4)]

    def prenorm_tile(src_ap, xbuf, R_x, gslot, shslot, dst_ap, R_dst, tcnt, load=True):
        bi = tcnt % 2
        t1, R_t1 = W["t1"], W["R_t1"]
        hb, R_hb = W["hb"][bi], W["R_hb"][bi]
        if load:
            S.dma("sp", xbuf, src_ap, writes=[R_x])
        S.op("act", ACT(sqj, xbuf, AF.Square, accum=stat[:, 0:1]), reads=[R_x], writes=[R_sqj, R_stat])
        S.op("act", ACT(stat[:, 1:2], stat[:, 0:1], AF.Sqrt, bias=eps_col, scale=1.0 / D), reads=[R_stat, R_eps], writes=[R_stat])
        S.op("dve", RECIP(stat[:, 2:3], stat[:, 1:2]), reads=[R_stat], writes=[R_stat])
        S.op("dve", STT(t1, xbuf, stat[:, 2:3], modsl(gslot), ALU.mult, ALU.mult),
             reads=[R_x, R_stat, R_mods[gslot]], writes=[R_t1])
        S.op("pool", TT(hb, t1, modsl(shslot), ALU.add), reads=[R_t1, R_mods[shslot]], writes=[R_hb])
        b = bank()
        S.op("pe", TR([(PSB[b][:, k * 128:(k + 1) * 128], hb[:, k * 128:(k + 1) * 128]) for k in range(8)], ident),
             reads=[R_hb, R_ident], writes=[PR[b]])
        S.op("act", ACOPY(dst_ap, PSB[b].rearrange("p (k c) -> p k c", k=8)), reads=[PR[b]], writes=[R_dst])

    def proj_fm(colblk_off, hTb, R_h, ntok_):
        b = bank()
        S.op("pe", MM([(PS[b][:, :ntok_], wext[:, k, colblk_off:colblk_off + 128], hTb[:, k, :ntok_], k == 0, k == 7)
                       for k in range(8)]), reads=[R_wext, R_h], writes=[PR[b]])
        return b

    tcnt = 0
    nblk = 8 if stage >= 3 else 5
    for blk in range(nblk):
        own = blk < 4
        ntile = 4 if (own or stage >= 3) else 1
        hb_i = blk % 2
        for tl in range(ntile):
            tile_i = blk * 4 + tl
            xi = tcnt % 2
            prenorm_tile(x_core[tile_i * 128:(tile_i + 1) * 128, :], W["xt"][xi], W["R_xt"][xi], 0, 1,
                         hT[hb_i][:, :, tl * 128:(tl + 1) * 128], R_hT[hb_i], tcnt)
            tcnt += 1
            bg_step(1)
        ntok = ntile * 128
        tok0 = blk * 512
        if own or blk == 4:
            kt = 512 if own else 128
            plist = []
            if own:
                plist += [("q", cb, QO + cb * 128, QSO + cb * 128) for cb in range(4)]
            plist += [("k", kb, KO + kb * 128, KSO + kb * 128) for kb in range(2)]
            for pi, (kind, cb, o1, o2) in enumerate(plist):
                ba = proj_fm(o1, hT[hb_i], R_hT[hb_i], kt)
                bb = proj_fm(o2, hT[hb_i], R_hT[hb_i], kt)
                ra, rb = ropet[(2 * pi) % 4], ropet[(2 * pi + 1) % 4]
                Ra, Rb = R_ropet[(2 * pi) % 4], R_ropet[(2 * pi + 1) % 4]
                S.op("dve", TT(ra[:, :kt], PS[ba][:, :kt], rcos[:, tok0:tok0 + kt], ALU.mult), reads=[PR[ba], R_rope], writes=[Ra])
                S.op("dve", TT(rb[:, :kt], PS[bb][:, :kt], rsin[:, tok0:tok0 + kt], ALU.mult), reads=[PR[bb], R_rope], writes=[Rb])
                if kind == "q":
                    S.op("pool", TT(qT[:, cb, tok0:tok0 + kt], ra[:, :kt], rb[:, :kt], ALU.add), reads=[Ra, Rb], writes=[R_q[blk]])
                else:
                    S.op("pool", TT(kT[:, cb, tok0:tok0 + kt], ra[:, :kt], rb[:, :kt], ALU.add), reads=[Ra, Rb], writes=[R_k[blk]])
            for tl in range(4 if own else 1):
                tile_i = blk * 4 + tl
                b = bank()
                S.op("pe", MM([(PS[b][:, :128], hT[hb_i][:, k, tl * 128:(tl + 1) * 128], wext[:, k, VO:VO + 128], k == 0, k == 7)
                               for k in range(8)]), reads=[R_wext, R_hT[hb_i]], writes=[PR[b]])
                S.op("act", ACOPY(vaug[:, tile_i, :, 0:64], PS[b][:, :128].rearrange("p (h c) -> p h c", h=2)),
                     reads=[PR[b]], writes=[R_v[tile_i]])
        for cb in range(4):
            b = proj_fm(UO + cb * 128, hT[hb_i], R_hT[hb_i], ntok)
            S.op("act", ACOPY(uT[:, cb, tok0:tok0 + ntok], PS[b][:, :ntok]), reads=[PR[b]], writes=[R_u[blk]])
    if stage == 1:
        dbg("qT", qT, [128, 4, T_OWN], R_q[3])
        dbg("kT", kT, [128, 2, 2176], R_k[4])
        dbg("vaug", vaug, [128, 17, 2, 65], R_v[16])
        dbg("uT", uT, [128, 4, 4096], R_u[4])
        S.finish(); S.emit(); return nc, dbg_outs
    S.barrier()
    A.top = mark_C

    esink = A.alloc([8], F32)
    R_es = Reg()
    S.dma("sp", esink, sink_rep, writes=[R_es])
    S.op("act", ACT(esink, esink, AF.Exp), reads=[R_es], writes=[R_es])
    pT = A.alloc([4, 8, 384], BF16)
    R_pT = [[Reg() for _ in range(8)] for _ in range(4)]
    attn_o = [A.alloc([512], BF16) for _ in range(2)]
    R_ao = [Reg(), Reg()]
    den = A.alloc([16], F32)
    R_den = Reg()

    def kq_range(j):
        return max(j - 1, 0), min(j + 2, 16)

    def pv_block(n):
        bo = [bank(), bank()]
        ai = n % 2
        for hh in range(2):
            groups, regs = [], []
            for h4 in range(4):
                h = hh * 4 + h4
                kvh = h // 4
                js = [j for j in (n - 1, n, n + 1) if 0 <= j <= 16]
                for ji, j in enumerate(js):
                    qb0, _ = kq_range(j)
                    c0 = (n - qb0) * 128
                    groups.append((PS[bo[hh]][:, h4 * 65:(h4 + 1) * 65], pT[:, j % 4, h, c0:c0 + 128], vaug[:, j, kvh, :],
                                   ji == 0, ji == len(js) - 1))
                    regs.append(R_pT[j % 4][h])
                    regs.append(R_v[j])
            S.op("pe", MM(groups), reads=regs, writes=[PR[bo[hh]]])
        for hh in range(2):
            pv = PS[bo[hh]][:, 0:260].rearrange("p (h c) -> p h c", h=4)
            S.op("dve", TT(den[:, hh * 4:hh * 4 + 4], pv[:, :, 64], esink[:, hh * 4:hh * 4 + 4], ALU.add),
                 reads=[PR[bo[hh]], R_es], writes=[R_den])
        S.op("dve", RECIP(den[:, 8:16], den[:, 0:8]), reads=[R_den], writes=[R_den])
        for hh in range(2):
            pv = PS[bo[hh]][:, 0:260].rearrange("p (h c) -> p h c", h=4)
            S.op("dve", TT(attn_o[ai][:, hh * 256:(hh + 1) * 256].rearrange("p (h c) -> p h c", h=4), pv[:, :, 0:64],
                           bc_last(den[:, 8 + hh * 4:8 + hh * 4 + 4], 64), ALU.mult),
                 reads=[PR[bo[hh]], R_den], writes=[R_ao[ai]])
        S.op("act", ACT(sqj[:, 0:512], attn_o[ai], AF.Square, accum=stat[:, 8:9]), reads=[R_ao[ai]], writes=[R_sqj, R_stat])
        S.op("act", ACT(stat[:, 9:10], stat[:, 8:9], AF.Sqrt, bias=eps_col, scale=1.0 / 512), reads=[R_stat, R_eps], writes=[R_stat])
        S.op("dve", RECIP(rattn[:, n:n + 1], stat[:, 9:10]), reads=[R_stat], writes=[R_rattn])
        b = bank()
        S.op("pe", TR([(PSB[b][:, k * 128:(k + 1) * 128], attn_o[ai][:, k * 128:(k + 1) * 128]) for k in range(4)], ident),
             reads=[R_ao[ai], R_ident], writes=[PR[b]])
        S.op("act", ACOPY(attnT[:, :, n * 128:(n + 1) * 128], PSB[b][:, 0:512].rearrange("p (k c) -> p k c", k=4)),
             reads=[PR[b]], writes=[R_attnT[n]])

    for j in range(17):
        qb0, qb1 = kq_range(j)
        ncol = (qb1 - qb0) * 128
        moff = 128 if j == 0 else 0
        for h in range(8):
            kvh, qblk, pr = h // 4, h // 2, (h % 2) * 64
            b = bank()
            S.op("pe", MM([(PS[b][:, :ncol], kT[pr:pr + 64, kvh, j * 128:(j + 1) * 128], qT[pr:pr + 64, qblk, qb0 * 128:qb1 * 128], True, False),
                           (PS[b][:, :ncol], ident, maskb[:, moff:moff + ncol], False, True)]),
                 reads=[R_k[min(j // 4, 4)], R_q[0], R_q[1], R_q[2], R_q[3], R_ident, R_mask], writes=[PR[b]])
            S.op("act", ACT(pT[:, j % 4, h, :ncol], PS[b][:, :ncol], AF.Exp, scale=0.125), reads=[PR[b]], writes=[R_pT[j % 4][h]])
        if j >= 1:
            pv_block(j - 1)
        bg_step(1)
    if stage == 2:
        dbg("attnT", attnT, [128, 4, T_OWN], R_attnT[15])
        dbg("rattn", rattn, [128, NT], R_rattn)
        S.finish(); S.emit(); return nc, dbg_outs
    S.barrier()
    A.top = mark_B

    y2T = A.alloc([4, T_OWN], BF16)
    R_y2T = [Reg() for _ in range(4)]
    rssm = A.alloc([NT], F32)
    R_rssm = Reg()
    mark_D = A.top
    are = A.alloc([32], F32); aim = A.alloc([32], F32); ldt = A.alloc([32], F32)
    R_sc = Reg()
    S.dma("sp", are, a_re_p, writes=[R_sc])
    S.dma("sp", aim, a_im_p, writes=[R_sc])
    S.dma("sp", ldt, logdt_p, writes=[R_sc])
    dtv = A.alloc([32], F32); lr = A.alloc([32], F32); li = A.alloc([32], F32); rmag = A.alloc([32], F32)
    tmpa = A.alloc([32], F32); tmpb = A.alloc([32], F32); cosl = A.alloc([32], F32); sinl = A.alloc([32], F32)
    fr = A.alloc([32], F32); fi = A.alloc([32], F32); dnm = A.alloc([32], F32)
    dsk = A.alloc([4], F32)
    kis = A.alloc([32], I32)
    S.dma("sp", dsk, dskip_col, writes=[R_sc])
    seq = [
        ("act", ACT(dtv, ldt, AF.Exp)),
        ("dve", TT(lr, are, dtv, ALU.mult)),
        ("dve", TT(li, aim, dtv, ALU.mult)),
        ("act", ACT(rmag, lr, AF.Exp)),
        ("dve", TS(kis, li, 1.0 / (2 * PI), None, ALU.mult)),
        ("dve", CP(tmpa, kis)),
        ("dve", STT(tmpa, tmpa, -2 * PI, li, ALU.mult, ALU.add)),
        ("dve", TS(tmpa, tmpa, 3.1415925, -3.1415925, ALU.min, ALU.max)),
        ("act", ACT(sinl, tmpa, AF.Sin)),
        ("dve", TS(tmpb, li, PI / 2, None, ALU.add)),
        ("dve", TS(kis, tmpb, 1.0 / (2 * PI), None, ALU.mult)),
        ("dve", CP(tmpa, kis)),
        ("dve", STT(tmpa, tmpa, -2 * PI, tmpb, ALU.mult, ALU.add)),
        ("dve", TS(tmpa, tmpa, 3.1415925, -3.1415925, ALU.min, ALU.max)),
        ("act", ACT(cosl, tmpa, AF.Sin)),
        ("dve", TT(cosl, cosl, rmag, ALU.mult)),
        ("dve", TT(sinl, sinl, rmag, ALU.mult)),
        ("dve", TS(cosl, cosl, -1.0, None, ALU.add)),
        ("dve", TT(dnm, are, are, ALU.mult)),
        ("dve", TT(tmpa, aim, aim, ALU.mult)),
        ("dve", TT(dnm, dnm, tmpa, ALU.add)),
        ("dve", RECIP(dnm, dnm)),
        ("dve", TT(tmpa, cosl, are, ALU.mult)),
        ("dve", TT(tmpb, sinl, aim, ALU.mult)),
        ("dve", TT(fr, tmpa, tmpb, ALU.add)),
        ("dve", TT(fr, fr, dnm, ALU.mult)),
        ("dve", TT(tmpa, sinl, are, ALU.mult)),
        ("dve", TT(tmpb, cosl, aim, ALU.mult)),
        ("dve", TT(fi, tmpa, tmpb, ALU.subtract)),
        ("dve", TT(fi, fi, dnm, ALU.mult)),
    ]
    for eng, fn in seq:
        S.op(eng, fn, reads=[R_sc, R_eps], writes=[R_sc])
    iot = A.alloc([CH], F32)
    R_iot = Reg()
    S.dma("sp", iot, iota1, writes=[R_iot])

    NG = 4
    wl = [[A.alloc([128], BF16) for _ in range(2)] for _ in range(NG)]
    cm = [[A.alloc([128], BF16) for _ in range(2)] for _ in range(NG)]
    tabC = [A.alloc([CH], F32) for _ in range(NG)]
    tabS = [A.alloc([CH], F32) for _ in range(NG)]
    R_gc = [Reg() for _ in range(NG)]
    ldb = [A.alloc([128], F32) for _ in range(4)]
    R_ldb = [Reg() for _ in range(4)]
    bbf = [A.alloc([128], BF16) for _ in range(2)]
    R_bbf = [Reg(), Reg()]
    NW = 4
    mt = [[A.alloc([CH], F32) for _ in range(4)] for _ in range(NW)]
    qt = [[A.alloc([CH], BF16) for _ in range(2)] for _ in range(NW)]
    R_mt = [[Reg() for _ in range(4)] for _ in range(NW)]
    R_qt = [Reg() for _ in range(NW)]
    mtmp = mt
    R_w = [R_mt[0][0], R_mt[1][0]]
    argt, argk = mt[3][0], mt[3][1]
    kint = mt[3][2].bitcast(I32)
    R_argt = R_mt[3][0]
    R_argk = R_mt[3][1]
    R_kint = R_mt[3][2]
    sro = [[A.alloc([CH], BF16) for _ in range(2)] for _ in range(NG)]
    R_sro = [Reg() for _ in range(NG)]
    sprev = [A.alloc([2], F32) for _ in range(NG)]
    R_sprev = [Reg() for _ in range(NG)]
    cs5 = [A.alloc([4], F32) for _ in range(NG)]
    ltmp = [A.alloc([2], F32) for _ in range(NW)]
    ysum = A.alloc([T_OWN], F32)
    R_ys = [Reg() for _ in range(4)]
    gact = A.alloc([4, T_OWN], BF16)
    R_gact = [Reg() for _ in range(4)]
    wcount = [0]

    for cb in range(4):
        for d in range(2):
            for gs in range(NG):
                G = cb * 4 + gs
                dg = d * 16 + G
                S.dma("sp", ldb[0], bpad_re[dg], writes=[R_ldb[0]])
                S.dma("sp", ldb[1], bpad_im[dg], writes=[R_ldb[1]])
                S.dma("sp", ldb[2], cpad_re[dg], writes=[R_ldb[2]])
                S.dma("sp", ldb[3], cpad_im[dg], writes=[R_ldb[3]])
                frc, fic = fr[:, dg:dg + 1], fi[:, dg:dg + 1]
                ta, tb_ = mt[0][0][:, :128], mt[0][1][:, :128]
                S.op("dve", TS(ta, ldb[1], fic, None, ALU.mult), reads=[R_ldb[1], R_sc], writes=[R_mt[0][0]])
                S.op("dve", STT(bbf[0], ldb[0], frc, ta, ALU.mult, ALU.subtract), reads=[R_ldb[0], R_sc, R_mt[0][0]], writes=[R_bbf[0]])
                S.op("dve", TS(tb_, ldb[0], fic, None, ALU.mult), reads=[R_ldb[0], R_sc], writes=[R_mt[0][1]])
                S.op("dve", STT(bbf[1], ldb[1], frc, tb_, ALU.mult, ALU.add), reads=[R_ldb[1], R_sc, R_mt[0][1]], writes=[R_bbf[1]])
                b = bank()
                S.op("pe", TR([(PSB[b][:, 0:128], bbf[0]), (PSB[b][:, 128:256], bbf[1])], ident),
                     reads=[R_bbf[0], R_bbf[1], R_ident], writes=[PR[b]])
                S.op("act", ACOPY(wl[gs][0], PSB[b][:, 0:128]), reads=[PR[b]], writes=[R_gc[gs]])
                S.op("act", ACOPY(wl[gs][1], PSB[b][:, 128:256]), reads=[PR[b]], writes=[R_gc[gs]])
                S.op("act", ACOPY(cm[gs][0], ldb[2]), reads=[R_ldb[2]], writes=[R_gc[gs]])
                S.op("act", AMUL(cm[gs][1], ldb[3], -1.0), reads=[R_ldb[3]], writes=[R_gc[gs]])
                lic = li[:, dg:dg + 1]
                S.op("dve", TS(argt, iot, lic, None, ALU.mult), reads=[R_iot, R_sc], writes=[R_argt])
                for (tab, shift) in ((tabS[gs], False), (tabC[gs], True)):
                    if shift:
                        S.op("dve", TS(argt, argt, PI / 2, None, ALU.add), reads=[R_argt], writes=[R_argt])
                    S.op("dve", TS(kint, argt, 1.0 / (2 * PI), None, ALU.mult), reads=[R_argt], writes=[R_kint])
                    S.op("dve", CP(argk, kint), reads=[R_kint], writes=[R_argk])
                    S.op("dve", STT(argk, argk, -2 * PI, argt, ALU.mult, ALU.add), reads=[R_argt, R_argk], writes=[R_argk])
                    S.op("dve", TS(argk, argk, 3.1415925, -3.1415925, ALU.min, ALU.max), reads=[R_argk], writes=[R_argk])
                    S.op("act", ACT(tab, argk, AF.Sin), reads=[R_argk], writes=[R_gc[gs]])
                S.op("dve", CP(cs5[gs][:, 0:1], tabC[gs][:, CH - 1:CH]), reads=[R_gc[gs]], writes=[R_sprev[gs]])
                S.op("dve", CP(cs5[gs][:, 1:2], tabS[gs][:, CH - 1:CH]), reads=[R_gc[gs]], writes=[R_sprev[gs]])
                S.op("dve", TS(cs5[gs][:, 2:3], tabS[gs][:, CH - 1:CH], -1.0, None, ALU.mult), reads=[R_gc[gs]], writes=[R_sprev[gs]])
                S.op("dve", CP(cs5[gs][:, 3:4], tabC[gs][:, CH - 1:CH]), reads=[R_gc[gs]], writes=[R_sprev[gs]])
                S.op("dve", MEMSET(sprev[gs], 0.0), writes=[R_sprev[gs]])
            chunks = [0, 1, 2, 3] if d == 0 else [7, 6, 5, 4, 3, 2, 1, 0]
            for c in chunks:
                is_own = c < 4
                t0 = c * CH
                for gs in range(NG):
                    G = cb * 4 + gs
                    dg = d * 16 + G
                    wi = wcount[0] % NW
                    wcount[0] += 1
                    if wcount[0] % 3 == 0:
                        bg_step(1)
                    m, Rm = mt[wi], R_mt[wi]
                    ba, bb = bank(), bank()
                    S.op("pe", MM([(PS[ba], wl[gs][0], uT[:, cb, t0:t0 + CH], True, True)]), reads=[R_gc[gs], R_u[c]], writes=[PR[ba]])
                    S.op("pe", MM([(PS[bb], wl[gs][1], uT[:, cb, t0:t0 + CH], True, True)]), reads=[R_gc[gs], R_u[c]], writes=[PR[bb]])
                    tC = tabC[gs] if d == 0 else rev(tabC[gs])
                    tS = tabS[gs] if d == 0 else rev(tabS[gs])
                    S.op("dve", TT(m[0], PS[ba], tC, ALU.mult), reads=[PR[ba], R_gc[gs]], writes=[Rm[0]])
                    S.op("dve", TT(m[1], PS[bb], tS, ALU.mult), reads=[PR[bb], R_gc[gs]], writes=[Rm[1]])
                    S.op("dve", TT(m[2], PS[bb], tC, ALU.mult), reads=[PR[bb], R_gc[gs]], writes=[Rm[2]])
                    S.op("dve", TT(m[3], PS[ba], tS, ALU.mult), reads=[PR[ba], R_gc[gs]], writes=[Rm[3]])
                    S.op("pool", TT(m[0], m[0], m[1], ALU.add), reads=[Rm[0], Rm[1]], writes=[Rm[0]])
                    S.op("pool", TT(m[2], m[2], m[3], ALU.subtract), reads=[Rm[2], Rm[3]], writes=[Rm[2]])
                    rcol = rmag[:, dg:dg + 1]
                    rb = bass.AP(tensor=rcol.tensor, offset=rcol.offset, ap=[list(rcol.ap[0]), [0, CH]])
                    for ri, (src, dst) in enumerate(((0, 1), (2, 3))):
                        o = m[dst] if d == 0 else rev(m[dst])
                        i1 = m[src] if d == 0 else rev(m[src])
                        S.op("dve", SCAN(o, rb, i1, sprev[gs][:, ri:ri + 1]),
                             reads=[Rm[src], R_sprev[gs], R_sc], writes=[Rm[dst]])
                    lc = CH - 1 if d == 0 else 0
                    sr_l, si_l = m[1][:, lc:lc + 1], m[3][:, lc:lc + 1]
                    S.op("dve", TS(ltmp[wi], cs5[gs][:, 2:4], si_l, None, ALU.mult), reads=[Rm[3], R_sprev[gs]], writes=[R_qt[wi]])
                    S.op("dve", STT(sprev[gs], cs5[gs][:, 0:2], sr_l, ltmp[wi], ALU.mult, ALU.add), reads=[Rm[1], R_qt[wi], R_sprev[gs]], writes=[R_sprev[gs]])
                    if is_own:
                        S.op("dve", TT(m[0], m[1], tC, ALU.mult), reads=[Rm[1], R_gc[gs]], writes=[Rm[0]])
                        S.op("dve", TT(m[2], m[3], tS, ALU.mult), reads=[Rm[3], R_gc[gs]], writes=[Rm[2]])
                        S.op("dve", TT(qt[wi][0], m[1], tS, ALU.mult), reads=[Rm[1], R_gc[gs]], writes=[R_qt[wi]])
                        S.op("dve", TT(qt[wi][1], m[3], tC, ALU.mult), reads=[Rm[3], R_gc[gs]], writes=[R_qt[wi]])
                        S.op("dve", TT(sro[gs][0], m[0], m[2], ALU.subtract), reads=[Rm[0], Rm[2]], writes=[R_sro[gs]])
                        S.op("dve", TT(sro[gs][1], qt[wi][0], qt[wi][1], ALU.add), reads=[R_qt[wi]], writes=[R_sro[gs]])
                if is_own:
                    b = bank()
                    groups = []
                    for gs in range(NG):
                        groups.append((PS[b], cm[gs][0], sro[gs][0], gs == 0, False))
                        groups.append((PS[b], cm[gs][1], sro[gs][1], False, gs == NG - 1))
                    S.op("pe", MM(groups), reads=R_gc + R_sro, writes=[PR[b]])
                    ys = ysum[:, t0:t0 + CH]
                    if d == 0:
                        S.op("dve", STT(ys, uT[:, cb, t0:t0 + CH], dsk[:, cb:cb + 1], PS[b], ALU.mult, ALU.add),
                             reads=[PR[b], R_u[c], R_sc], writes=[R_ys[c]])
                    else:
                        S.op("dve", TT(ys, ys, PS[b], ALU.add), reads=[PR[b], R_ys[c]], writes=[R_ys[c]])
        for c in range(4):
            ys = ysum[:, c * CH:(c + 1) * CH]
            g1, g2 = mt[c % 2][0], mt[c % 2][1]
            Rg1, Rg2 = R_mt[c % 2][0], R_mt[c % 2][1]
            S.op("act", ACT(g1, ys, AF.Square), reads=[R_ys[c]], writes=[Rg1])
            S.op("dve", TS(g1, g1, 0.044715, 1.0, ALU.mult, ALU.add), reads=[Rg1], writes=[Rg1])
            S.op("dve", TT(g1, g1, ys, ALU.mult), reads=[Rg1, R_ys[c]], writes=[Rg1])
            S.op("act", ACT(g2, g1, AF.Sigmoid, scale=1.5957691216057308), reads=[Rg1], writes=[Rg2])
            S.op("pool", TT(gact[:, cb, c * CH:(c + 1) * CH], ys, g2, ALU.mult), reads=[Rg2, R_ys[c]], writes=[R_gact[cb]])
    if stage == 3:
        dbg("gact", gact, [128, 4, T_OWN], R_gact[3])
        S.finish(); S.emit(); return nc, dbg_outs

    wglu = A.alloc([4, 512], BF16)
    R_wglu = Reg()
    S.dma("pool", wglu, w_glu.rearrange("(k p) n -> p k n", p=128), writes=[R_wglu])
    bglu = A.alloc([4], F32)
    S.dma("sp", bglu, bglu_col, writes=[R_wglu])
    ysq = [A.alloc([512], BF16) for _ in range(4)]
    R_ysq = [Reg() for _ in range(4)]
    sgt = [mt[2][0], mt[2][1]]
    R_sgt = [R_mt[2][0], R_mt[2][1]]
    ci = 0
    for tb in range(4):
        tsl = slice(tb * 512, (tb + 1) * 512)
        for cbo in range(4):
            b = bank()
            S.op("pe", MM([(PS[b], wglu[:, k, cbo * 128:(cbo + 1) * 128], gact[:, k, tsl], k == 0, k == 3) for k in range(4)]),
                 reads=[R_wglu] + R_gact, writes=[PR[b]])
            si = ci % 2
            ci += 1
            S.op("act", ACT(sgt[si], PS[b], AF.Sigmoid, bias=bglu[:, cbo:cbo + 1]), reads=[PR[b], R_wglu], writes=[R_sgt[si]])
            S.op("dve", TT(y2T[:, cbo, tsl], gact[:, cbo, tsl], sgt[si], ALU.mult),
                 reads=[R_sgt[si], R_gact[cbo]], writes=[R_y2T[cbo]])
            S.op("pool", TT(ysq[cbo], y2T[:, cbo, tsl], y2T[:, cbo, tsl], ALU.mult),
                 reads=[R_y2T[cbo]], writes=[R_ysq[cbo]])
        bss = bank()
        groups = []
        for tl in range(4):
            for cbo in range(4):
                groups.append((PS[bss][:, tl * 16:(tl + 1) * 16], ysq[cbo][:, tl * 128:(tl + 1) * 128], ones_col, cbo == 0, cbo == 3))
        S.op("pe", MM(groups), reads=R_ysq + [R_ones], writes=[PR[bss]])
        S.op("act", ACT(stat[:, 10:14], PS[bss][:, 0:64].rearrange("p (t c) -> p t c", c=16)[:, :, 0], AF.Sqrt, bias=eps_col, scale=1.0 / 512), reads=[PR[bss], R_eps], writes=[R_stat])
        S.op("dve", RECIP(rssm[:, tb * 4:tb * 4 + 4], stat[:, 10:14]), reads=[R_stat], writes=[R_rssm])
    if stage == 4:
        dbg("y2T", y2T, [128, 4, T_OWN], R_y2T[3])
        dbg("rssm", rssm, [128, NT], R_rssm)
        S.finish(); S.emit(); return nc, dbg_outs
    S.barrier()
    A.top = mark_D

    BR = 256
    NBLK = 64
    NTB = BR // 128
    NROWS = NBLK * BR
    xs_d = nc.dram_tensor("xs_scr", [NROWS, D], BF16, kind="Internal").ap()
    ys_d = nc.dram_tensor("ys_scr", [NROWS, D], F32, kind="Internal").ap()
    gates = A.alloc_top([NT, NE], F32)
    R_gates = [Reg() for _ in range(NT)]
    maskall = A.alloc_top([NT, NE], BF16)
    R_maskall = [Reg() for _ in range(NT)]
    dest4i = A.alloc_top([NT, 4], I32)
    gate4 = A.alloc_top([NT, 4], F32)
    widx = A.alloc_top([NBLK, 2], I32)
    bidx = A.alloc_top([NBLK], I32)
    eidx = A.alloc_top([NBLK], I32)
    R_meta = Reg()
    mark_top_meta = A.hi
    alloc_norm_bufs()
    h2tm = A.alloc([NT, D], BF16)
    R_h2tm = [Reg() for _ in range(NT)]
    h2Tt = [A.alloc([8, 128], BF16) for _ in range(2)]
    R_h2Tt = [Reg(), Reg()]
    wout = A.alloc([8, D], BF16)
    R_wout = Reg()
    w_out_v = w_out.rearrange("(k p) n -> p k n", p=128)
    S.dma("pool", wout[:, 0:4, :], w_out_v[:, 0:4, :], writes=[R_wout])
    S.dma("pool", wout[:, 4:8, :], w_out_v[:, 4:8, :], writes=[R_wout])
    gcat = A.alloc([8], F32)
    S.dma("sp", gcat, gcat_col, writes=[R_wout])
    for k in range(8):
        S.op("dve", TS(wout[:, k, :], wout[:, k, :], gcat[:, k:k + 1], None, ALU.mult), reads=[R_wout], writes=[R_wout])
    wr = A.alloc([8, NE], BF16)
    R_wr = Reg()
    S.dma("pool", wr, w_router.rearrange("(k p) n -> p k n", p=128), writes=[R_wr])
    brt = A.alloc([NE], F32)
    S.dma("sp", brt, b_router_rep, writes=[R_wr])
    ym = [A.alloc([D], F32) for _ in range(2)]
    R_ym = [Reg(), Reg()]
    xn = [A.alloc([D], F32) for _ in range(2)]
    R_xn = [Reg(), Reg()]
    lg = A.alloc([NE], F32)
    top8 = A.alloc([8], F32)
    eg = A.alloc([NE], F32)
    msk = A.alloc([NE], F32)
    R_lg = Reg()
    R_xmid = [Reg() for _ in range(NT)]

    def prenorm2_tile(xbuf, R_x, n):
        t1, R_t1 = W["t1"], W["R_t1"]
        S.op("act", ACT(sqj, xbuf, AF.Square, accum=stat[:, 0:1]), reads=[R_x], writes=[R_sqj, R_stat])
        S.op("act", ACT(stat[:, 1:2], stat[:, 0:1], AF.Sqrt, bias=eps_col, scale=1.0 / D), reads=[R_stat, R_eps], writes=[R_stat])
        S.op("dve", RECIP(stat[:, 2:3], stat[:, 1:2]), reads=[R_stat], writes=[R_stat])
        S.op("dve", STT(t1, xbuf, stat[:, 2:3], modsl(3), ALU.mult, ALU.mult), reads=[R_x, R_stat, R_mods[3]], writes=[R_t1])
        S.op("pool", TT(h2tm[:, n, :], t1, modsl(4), ALU.add), reads=[R_t1, R_mods[4]], writes=[R_h2tm[n]])
        b = bank()
        S.op("pe", TR([(PSB[b][:, k * 128:(k + 1) * 128], h2tm[:, n, k * 128:(k + 1) * 128]) for k in range(8)], ident),
             reads=[R_h2tm[n], R_ident], writes=[PR[b]])
        S.op("act", ACOPY(h2Tt[n % 2], PSB[b].rearrange("p (k c) -> p k c", k=8)), reads=[PR[b]], writes=[R_h2Tt[n % 2]])

    for n in range(NT):
        i2 = n % 2
        nsl = slice(n * 128, (n + 1) * 128)
        bA = [bank(), bank()]
        bS = [bank(), bank()]
        for half in range(2):
            hs = slice(half * 512, (half + 1) * 512)
            S.op("pe", MM([(PS[bA[half]], attnT[:, k, nsl], wout[:, k, hs], k == 0, k == 3) for k in range(4)]),
                 reads=[R_attnT[n], R_wout], writes=[PR[bA[half]]])
            S.op("pe", MM([(PS[bS[half]], y2T[:, k, nsl], wout[:, 4 + k, hs], k == 0, k == 3) for k in range(4)]),
                 reads=R_y2T + [R_wout], writes=[PR[bS[half]]])
        for half in range(2):
            hs = slice(half * 512, (half + 1) * 512)
            S.op("dve", TS(ym[i2][:, hs], PS[bA[half]], rattn[:, n:n + 1], None, ALU.mult), reads=[PR[bA[half]], R_rattn], writes=[R_ym[i2]])
            S.op("dve", STT(ym[i2][:, hs], PS[bS[half]], rssm[:, n:n + 1], ym[i2][:, hs], ALU.mult, ALU.add),
                 reads=[PR[bS[half]], R_rssm, R_ym[i2]], writes=[R_ym[i2]])
        xi = n % 2
        xtb, R_xtb = W["xt"][xi], W["R_xt"][xi]
        S.dma("sp", xtb, x_core[nsl, :], writes=[R_xtb])
        S.op("act", ACT(sqj, ym[i2], AF.Square, accum=stat[:, 16:17]), reads=[R_ym[i2]], writes=[R_sqj, R_stat])
        S.op("act", ACT(stat[:, 17:18], stat[:, 16:17], AF.Sqrt, bias=eps_col, scale=1.0 / D), reads=[R_stat, R_eps], writes=[R_stat])
        S.op("dve", RECIP(stat[:, 18:19], stat[:, 17:18]), reads=[R_stat], writes=[R_stat])
        S.op("dve", STT(ym[i2], ym[i2], stat[:, 18:19], modsl(2), ALU.mult, ALU.mult), reads=[R_ym[i2], R_stat, R_mods[2]], writes=[R_ym[i2]])
        S.op("pool", TT(xn[i2], ym[i2], xtb, ALU.add), reads=[R_ym[i2], R_xtb], writes=[R_xn[i2]])
        S.dma("sp", y_out[nsl, :], xn[i2], reads=[R_xn[i2]], writes=[R_xmid[n]])
        prenorm2_tile(xn[i2], R_xn[i2], n)
        b = bank()
        S.op("pe", MM([(PS[b][:, 0:NE], h2Tt[n % 2][:, k, :], wr[:, k, :], k == 0, k == 7) for k in range(8)]),
             reads=[R_h2Tt[n % 2], R_wr], writes=[PR[b]])
        S.op("dve", TT(lg, PS[b][:, 0:NE], brt, ALU.add), reads=[PR[b], R_wr], writes=[R_lg])
        S.op("dve", (lambda e: e.max(out=top8, in_=lg)), reads=[R_lg], writes=[R_lg])
        S.op("dve", TS(msk, lg, top8[:, 3:4], None, ALU.is_ge), reads=[R_lg], writes=[R_lg])
        S.op("dve", CP(maskall[:, n, :], msk), reads=[R_lg], writes=[R_maskall[n]])
        S.op("dve", TS(stat[:, 20:21], top8[:, 0:1], -1.0, None, ALU.mult), reads=[R_lg], writes=[R_stat])
        S.op("act", ACT(eg, lg, AF.Exp, bias=stat[:, 20:21]), reads=[R_lg, R_stat], writes=[R_lg])
        S.op("dve", TT(eg, eg, msk, ALU.mult), reads=[R_lg], writes=[R_lg])
        S.op("dve", (lambda e: e.reduce_sum(out=stat[:, 21:22], in_=eg, axis=mybir.AxisListType.X)), reads=[R_lg], writes=[R_stat])
        S.op("dve", RECIP(stat[:, 22:23], stat[:, 21:22]), reads=[R_stat], writes=[R_stat])
        S.op("dve", TS(gates[:, n, :], eg, stat[:, 22:23], None, ALU.mult), reads=[R_lg, R_stat], writes=[R_gates[n]])

    ones128 = A.alloc([128], BF16)
    ltri = A.alloc([128], BF16)
    R_cst = Reg()
    S.op("dve", MEMSET(ones128, 1.0), writes=[R_cst])
    S.dma("pool", ltri, ltri_in, writes=[R_cst])
    jb = A.alloc([NBLK], F32)
    kpi = A.alloc([2], F32)
    pio = A.alloc([1], F32)
    S.dma("sp", jb, jb512_in, writes=[R_cst])
    S.dma("sp", kpi, kp_iota_in, writes=[R_cst])
    S.dma("sp", pio, p_iota_in, writes=[R_cst])
    rank = A.alloc([NT, NE], F32)
    R_rank = Reg()
    for n in range(NT):
        b = bank()
        groups = [(PS[b][:, 0:NE], ones128, maskall[:, t, :], t == 0, False) for t in range(n)]
        groups.append((PS[b][:, 0:NE], ltri, maskall[:, n, :], n == 0, True))
        S.op("pe", MM(groups), reads=R_maskall[:n + 1] + [R_cst], writes=[PR[b]])
        S.op("act", ACOPY(rank[:, n, :], PS[b][:, 0:NE]), reads=[PR[b]], writes=[R_rank])
    cnt = A.alloc([NE], F32)
    b = bank()
    S.op("pe", MM([(PS[b][:, 0:NE], ones128, maskall[:, t, :], t == 0, t == NT - 1) for t in range(NT)]),
         reads=R_maskall + [R_cst], writes=[PR[b]])
    kint32 = A.alloc([NE], I32)
    padded = A.alloc([NE], F32)
    pend = A.alloc([NE], F32)
    pstart = A.alloc([NE], F32)
    ones32 = A.alloc([NE], F32)
    destm = A.alloc([NT, NE], F32)
    selt = A.alloc([NT, NE], F32)
    d8 = A.alloc([NT, 8], F32)
    cmpt = A.alloc([NBLK, NE], F32)
    ejf = A.alloc([NBLK], F32)
    R_m = Reg()
    S.op("dve", MEMSET(ones32, 1.0), writes=[R_m])
    S.op("dve", TS(kint32, PS[b][:, 0:NE], 1.0 / BR, (BR / 2 - 0.5) / BR, ALU.mult, ALU.add), reads=[PR[b]], writes=[R_m])
    S.op("dve", CP(cnt, kint32), reads=[R_m], writes=[R_m])
    S.op("dve", TS(padded, cnt, float(BR), None, ALU.mult), reads=[R_m], writes=[R_m])
    S.op("dve", SCAN(pend, ones32, padded, 0.0), reads=[R_m], writes=[R_m])
    S.op("dve", TT(pstart, pend, padded, ALU.subtract), reads=[R_m], writes=[R_m])
    pstart_bc = bass.AP(tensor=pstart.tensor, offset=pstart.offset, ap=[list(pstart.ap[0]), [0, NT], list(pstart.ap[1])])
    S.op("dve", TT(destm, rank, pstart_bc, ALU.add), reads=[R_m, R_rank], writes=[R_m])
    S.op("dve", STT(destm, destm, 1.0, maskall, ALU.add, ALU.mult), reads=[R_m] + R_maskall, writes=[R_m])
    S.op("dve", TS(destm, destm, -1.0, None, ALU.add), reads=[R_m], writes=[R_m])
    for n in range(NT):
        S.op("dve", (lambda e, n=n: e.max(out=d8[:, n, :], in_=destm[:, n, :])), reads=[R_m], writes=[R_m])
    S.op("dve", CP(dest4i, d8[:, :, 0:4]), reads=[R_m], writes=[R_meta])
    for k in range(4):
        S.op("dve", TT(selt, destm, bc_last(d8[:, :, k], NE), ALU.is_equal), reads=[R_m], writes=[R_m])
        S.op("dve", TT(selt, selt, gates, ALU.mult), reads=[R_m] + R_gates, writes=[R_m])
        S.op("dve", (lambda e, k=k: e.reduce_sum(out=gate4[:, :, k], in_=selt, axis=mybir.AxisListType.X)), reads=[R_m], writes=[R_meta])
    pend_bc = bass.AP(tensor=pend.tensor, offset=pend.offset, ap=[list(pend.ap[0]), [0, NBLK], list(pend.ap[1])])
    S.op("dve", TT(cmpt, pend_bc, bc_last(jb, NE), ALU.is_le), reads=[R_m, R_cst], writes=[R_m])
    S.op("dve", (lambda e: e.reduce_sum(out=ejf, in_=cmpt, axis=mybir.AxisListType.X)), reads=[R_m], writes=[R_m])
    S.op("dve", TS(ejf, ejf, float(NE - 1), None, ALU.min), reads=[R_m], writes=[R_m])
    kpi_bc = bass.AP(tensor=kpi.tensor, offset=kpi.offset, ap=[list(kpi.ap[0]), [0, NBLK], list(kpi.ap[1])])
    S.op("dve", STT(widx, bc_last(ejf, 2), 256.0, kpi_bc, ALU.mult, ALU.add), reads=[R_m, R_cst], writes=[R_meta])
    S.op("dve", STT(bidx, ejf, 128.0, pio[:, 0:1].to_broadcast([128, NBLK]), ALU.mult, ALU.add), reads=[R_m, R_cst], writes=[R_meta])
    S.op("dve", CP(eidx, ejf), reads=[R_m], writes=[R_meta])
    if stage == 5:
        dbg("gates", gates, [128, NT, NE], R_gates[15])
        dbg("dest4i", dest4i, [128, NT, 4], R_meta)
        dbg("gate4", gate4, [128, NT, 4], R_meta)
        dbg("eidx", eidx, [128, NBLK], R_meta)
        dbg("h2tm", h2tm, [128, NT, D], R_h2tm[15])
        S.finish(); S.emit(); return nc, dbg_outs
    bg_step(1000)
    for n in range(NT):
        for k in range(4):
            S.idma("pool", xs_d, bass.IndirectOffsetOnAxis(ap=dest4i[:, n, k:k + 1], axis=0), h2tm[:, n, :], None, NROWS - 1,
                   reads=[R_meta, R_h2tm[n]], writes=[])
    S.barrier()
    A.top = mark_core

    wgu = [A.alloc([8, 2 * D], BF16) for _ in range(2)]
    wdn = [A.alloc([8, D], BF16) for _ in range(2)]
    R_wgu = [[Reg() for _ in range(2)] for _ in range(2)]
    R_wdn = [[Reg()], [Reg()]]
    bgub = [A.alloc([16], F32) for _ in range(2)]
    bgub1 = [A.alloc([8], F32) for _ in range(2)]
    bdb = [A.alloc([D], F32) for _ in range(2)]
    R_bb = [Reg(), Reg()]
    R_bd = [Reg(), Reg()]
    xb = [A.alloc([NTB, D], BF16) for _ in range(2)]
    R_xb = [Reg(), Reg()]
    xT = [A.alloc([8, BR], BF16) for _ in range(2)]
    R_xT = [Reg(), Reg()]
    actT = A.alloc([8, BR], BF16)
    R_actT = Reg()
    ea = [A.alloc([BR], F32) for _ in range(2)]
    es_ = [A.alloc([BR], BF16) for _ in range(2)]
    eb = [A.alloc([BR], F32) for _ in range(2)]
    R_e = [Reg(), Reg()]
    ysb = [A.alloc([D], F32) for _ in range(2)]
    R_ysb = [Reg(), Reg()]
    ysct = 0

    def load_blk(j):
        bi = j % 2
        for kh in range(2):
            S.idma("pool", wgu[bi][:, kh * 4:(kh + 1) * 4, :].rearrange("p a b -> p (a b)"), None, wgu_bf,
                   bass.IndirectOffsetOnAxis(ap=widx[:, j, kh:kh + 1], axis=0), None, reads=[R_meta], writes=[R_wgu[bi][kh]])
        S.idma("pool", bgub[bi], None, bgu_rows_in, bass.IndirectOffsetOnAxis(ap=bidx[:, j:j + 1], axis=0), NE * 128 - 1,
               reads=[R_meta], writes=[R_bb[bi]])
        S.idma("pool", bdb[bi], None, b_down, bass.IndirectOffsetOnAxis(ap=eidx[:, j:j + 1], axis=0), NE - 1,
               reads=[R_meta], writes=[R_bd[bi]])
        S.op("dve", TS(bgub1[bi], bgub[bi][:, 8:16], 1.0, None, ALU.add), reads=[R_bb[bi]], writes=[R_bb[bi]])
        S.dma("sp", xb[bi], xs_d[j * BR:(j + 1) * BR, :].rearrange("(a p) c -> p a c", p=128), writes=[R_xb[bi]])

    def load_wdn(j):
        S.idma("pool", wdn[j % 2].rearrange("p a b -> p (a b)"), None, wdn_bf, bass.IndirectOffsetOnAxis(ap=bidx[:, j:j + 1], axis=0), None,
               reads=[R_meta], writes=[R_wdn[j % 2][0]])

    nblk_run = NBLK if stage >= 7 else 3
    load_blk(0)
    load_wdn(0)
    for j in range(nblk_run):
        bi = j % 2
        if j + 1 < nblk_run:
            load_blk(j + 1)
            load_wdn(j + 1)
        for a in range(NTB):
            b = bank()
            S.op("pe", TR([(PSB[b][:, k * 128:(k + 1) * 128], xb[bi][:, a, k * 128:(k + 1) * 128]) for k in range(8)], ident),
                 reads=[R_xb[bi], R_ident], writes=[PR[b]])
            S.op("act", ACOPY(xT[bi][:, :, a * 128:(a + 1) * 128], PSB[b].rearrange("p (k c) -> p k c", k=8)), reads=[PR[b]], writes=[R_xT[bi]])
        for fb in range(8):
            bg, bl = bank(), bank()
            S.op("pe", MM([(PS[bg][:, :BR], wgu[bi][:, k, fb * 128:(fb + 1) * 128], xT[bi][:, k, :], k == 0, k == 7) for k in range(8)]),
                 reads=R_wgu[bi] + [R_xT[bi]], writes=[PR[bg]])
            S.op("pe", MM([(PS[bl][:, :BR], wgu[bi][:, k, D + fb * 128:D + (fb + 1) * 128], xT[bi][:, k, :], k == 0, k == 7) for k in range(8)]),
                 reads=R_wgu[bi] + [R_xT[bi]], writes=[PR[bl]])
            wi = fb % 2
            S.op("dve", TS(ea[wi], PS[bg][:, :BR], bgub[bi][:, fb:fb + 1], 7.0, ALU.add, ALU.min), reads=[PR[bg], R_bb[bi]], writes=[R_e[wi]])
            S.op("act", ACT(es_[wi], ea[wi], AF.Sigmoid, scale=1.702), reads=[R_e[wi]], writes=[R_e[wi]])
            S.op("dve", TS(eb[wi], PS[bl][:, :BR], bgub1[bi][:, fb:fb + 1], 8.0, ALU.add, ALU.min), reads=[PR[bl], R_bb[bi]], writes=[R_e[wi]])
            S.op("dve", STT(eb[wi], eb[wi], -6.0, ea[wi], ALU.max, ALU.mult), reads=[R_e[wi]], writes=[R_e[wi]])
            S.op("dve", TT(actT[:, fb, :], eb[wi], es_[wi], ALU.mult), reads=[R_e[wi]], writes=[R_actT])
        for a in range(NTB):
            yi = ysct % 2
            ysct += 1
            for half in range(2):
                hs = slice(half * 512, (half + 1) * 512)
                b = bank()
                S.op("pe", MM([(PS[b], actT[:, fb, a * 128:(a + 1) * 128], wdn[bi][:, fb, hs], fb == 0, fb == 7) for fb in range(8)]),
                     reads=[R_actT] + R_wdn[bi], writes=[PR[b]])
                S.op("dve", TT(ysb[yi][:, hs], PS[b], bdb[bi][:, hs], ALU.add), reads=[PR[b], R_bd[bi]], writes=[R_ysb[yi]])
            r0 = j * BR + a * 128
            S.dma("sp", ys_d[r0:r0 + 128, :], ysb[yi], reads=[R_ysb[yi]])
    S.barrier()
    A.top = mark_core

    yk = [A.alloc([D], F32) for _ in range(4)]
    R_yk = [Reg() for _ in range(4)]
    acc = [A.alloc([D], F32) for _ in range(2)]
    R_acc = [Reg(), Reg()]
    xf = [A.alloc([D], F32) for _ in range(2)]
    R_xf = [Reg(), Reg()]
    kc = 0
    for n in range(NT):
        nsl = slice(n * 128, (n + 1) * 128)
        ai = n % 2
        S.dma("sp", xf[ai], y_out[nsl, :], reads=[R_xmid[n]], writes=[R_xf[ai]])
        for k in range(4):
            ki = kc % 4
            kc += 1
            S.idma("pool", yk[ki], None, ys_d, bass.IndirectOffsetOnAxis(ap=dest4i[:, n, k:k + 1], axis=0), NROWS - 1,
                   reads=[R_meta], writes=[R_yk[ki]])
            if k == 0:
                S.op("dve", TS(acc[ai], yk[ki], gate4[:, n, k:k + 1], None, ALU.mult), reads=[R_yk[ki], R_meta], writes=[R_acc[ai]])
            else:
                S.op("dve", STT(acc[ai], yk[ki], gate4[:, n, k:k + 1], acc[ai], ALU.mult, ALU.add), reads=[R_yk[ki], R_meta, R_acc[ai]], writes=[R_acc[ai]])
        S.op("act", ACT(sqj, acc[ai], AF.Square, accum=stat[:, 24:25]), reads=[R_acc[ai]], writes=[R_sqj, R_stat])
        S.op("act", ACT(stat[:, 25:26], stat[:, 24:25], AF.Sqrt, bias=eps_col, scale=1.0 / D), reads=[R_stat, R_eps], writes=[R_stat])
        S.op("dve", RECIP(stat[:, 26:27], stat[:, 25:26]), reads=[R_stat], writes=[R_stat])
        S.op("dve", STT(acc[ai], acc[ai], stat[:, 26:27], gtg2, ALU.mult, ALU.mult), reads=[R_acc[ai], R_stat, R_mods[5]], writes=[R_acc[ai]])
        S.op("pool", TT(acc[ai], acc[ai], xf[ai], ALU.add), reads=[R_acc[ai], R_xf[ai]], writes=[R_acc[ai]])
        S.dma("sp", y_out[nsl, :], acc[ai], reads=[R_acc[ai]], writes=[R_xmid[n]])
    S.finish()
    S.emit()
    return nc, dbg_outs


def _prep_inputs(inp):
    f = lambda a: np.ascontiguousarray(np.asarray(a, dtype=np.float32))
    x = f(inp["x"]); c = f(inp["c"])
    L = 0
    w_in = f(inp["w_in"][L])
    q = w_in[:, 0:512].reshape(1024, 8, 64)
    qsw = np.concatenate([q[:, :, 32:], q[:, :, :32]], axis=2).reshape(1024, 512)
    k = w_in[:, 512:640].reshape(1024, 2, 64)
    ksw = np.concatenate([k[:, :, 32:], k[:, :, :32]], axis=2)
    kdup = np.concatenate([k[:, 0], k[:, 0], k[:, 1], k[:, 1]], axis=1)
    kswdup = np.concatenate([ksw[:, 0], ksw[:, 0], ksw[:, 1], ksw[:, 1]], axis=1)
    w_ext = f(np.concatenate([w_in[:, 0:512], qsw, kdup, kswdup, w_in[:, 640:768], w_in[:, 768:1280]], axis=1))
    assert w_ext.shape == (1024, WEXT)
    rep = lambda v: f(np.broadcast_to(np.asarray(v, np.float32).reshape(1, -1), (128, np.asarray(v).size)))
    col = lambda v, nk: f(np.asarray(v, np.float32).reshape(nk, 128).T)
    gvecs = f(np.stack([rep(inp["g_pre_mix"][L]), rep(inp["g_post_mix"][L]), rep(inp["g_pre_ffn"][L]), rep(inp["g_post_ffn"][L])]))
    a = np.arange(128)[:, None]; b = np.arange(128)[None, :]
    m_prev = np.where(a <= b, 0.0, -30000.0)
    m_next = np.where(b <= a, 0.0, -30000.0)
    mask_mid = f(np.concatenate([m_prev, np.zeros((128, 128)), m_next], axis=1))
    inv_freq = (np.float32(10000.0) ** (-np.arange(32, dtype=np.float32) * np.float32(2.0) / np.float32(64))).astype(np.float32)
    shared = dict(
        w_ada=f(inp["w_ada"][L]), b_ada_rep=rep(inp["b_ada"][L]), gvecs=gvecs, w_ext=w_ext,
        sink_rep=rep(inp["attn_sink"][L]), mask_mid=mask_mid, ident=f(np.eye(128)),
        iota1=rep(np.arange(1, CH + 1)), dskip_col=col(inp["ssm_d"][L], 4),
        w_glu=f(inp["w_glu"][L]), bglu_col=col(inp["b_glu"][L], 4),
        gcat_col=col(np.concatenate([inp["g_attn_out"][L], inp["g_ssm_out"][L]]), 8),
        w_out=f(inp["w_out"][L]), w_router=f(inp["w_router"][L]), b_router_rep=rep(inp["b_router"][L]),
        wgu_rows=f(np.asarray(inp["w_gate_up"][L], np.float32).reshape(32, 8, 128, 2048).transpose(0, 2, 1, 3).reshape(32 * 128 * 2, 4 * 2048)),
        wdn_rows=f(np.asarray(inp["w_down"][L], np.float32).reshape(32, 8, 128, 1024).transpose(0, 2, 1, 3).reshape(32 * 128, 8 * 1024)),
        b_down=f(inp["b_down"][L]),
        bgu_rows=f(np.asarray(inp["b_gate_up"][L], np.float32).reshape(32, 16, 128).transpose(0, 2, 1).reshape(32 * 128, 16)),
        ltri=f(np.triu(np.ones((128, 128)), 1)),
        jb512=rep(np.arange(64) * 256.0),
        kp_iota=f(np.arange(2)[None, :] * 1.0 + 2.0 * np.arange(128)[:, None]),
        p_iota=f(np.arange(128).reshape(128, 1)),
    )
    are, aim, ldt = f(inp["ssm_a_re"][L]), f(inp["ssm_a_im"][L]), f(inp["ssm_log_dt"][L])
    bre, bim, cre, cim = f(inp["ssm_b_re"][L]), f(inp["ssm_b_im"][L]), f(inp["ssm_c_re"][L]), f(inp["ssm_c_im"][L])
    ssm_by_flip = {}
    for flip in (0, 1):
        dd = [1, 0] if flip else [0, 1]
        sc = {}
        for nm, arr in (("a_re_p", are), ("a_im_p", aim)):
            v = arr[dd].reshape(2, 16, 2, 64)
            sc[nm] = f(v.transpose(2, 3, 0, 1).reshape(128, 32))
        v = np.broadcast_to(ldt[dd].reshape(2, 16, 2, 1), (2, 16, 2, 64))
        sc["logdt_p"] = f(v.transpose(2, 3, 0, 1).reshape(128, 32))
        for nm, arr, is_c in (("bpad_re", bre, False), ("bpad_im", bim, False), ("cpad_re", cre, True), ("cpad_im", cim, True)):
            out = np.zeros((2, 16, 128, 128), np.float32)
            for d in range(2):
                for G in range(16):
                    for g2 in range(2):
                        g = 2 * G + g2
                        blk = arr[dd[d], g].T if is_c else arr[dd[d], g]
                        c0 = (G % 4) * 32 + g2 * 16
                        out[d, G, g2 * 64:(g2 + 1) * 64, c0:c0 + 16] = blk
            sc[nm] = out.reshape(32, 128, 128)
        ssm_by_flip[flip] = sc
    in_maps = []
    for core in range(8):
        bidx, hf = core // 2, core % 2
        seq = x[bidx]
        pos = np.arange(4096, dtype=np.float32)
        if hf == 1:
            seq = seq[::-1]
            pos = pos[::-1]
        pos = pos[:2176]
        ang = (pos[:, None] * inv_freq[None, :]).astype(np.float32)
        cs, sn = np.cos(ang).astype(np.float32), np.sin(ang).astype(np.float32)
        cos64 = np.concatenate([cs, cs], axis=1).T
        sin64 = np.concatenate([-sn, sn], axis=1).T
        m = dict(shared)
        m.update(ssm_by_flip[hf])
        m["x_core"] = f(seq)
        m["c_col"] = col(c[bidx], 8)
        m["rope_cos"] = f(np.concatenate([cos64, cos64], axis=0))
        m["rope_sin"] = f(np.concatenate([sin64, sin64], axis=0))
        in_maps.append(m)
    return in_maps


_CACHE = {}


def kernel(**inputs):
    in_maps = _prep_inputs(inputs)
    if "nc" not in _CACHE:
        _CACHE["nc"] = build()[0]
    res = run_bass_kernel_spmd(_CACHE["nc"], in_maps, core_ids=list(range(8)))
    out = np.zeros((4, 4096, 1024), np.float32)
    for core in range(8):
        bidx, hf = core // 2, core % 2
        y = np.asarray(res.results[core]["y_out"], np.float32)
        if hf == 0:
            out[bidx, :2048] = y
        else:
            out[bidx, 2048:] = y[::-1]
    return out
```
